# Optimizing a Trainium2 kernel written in Bass

```python
import jax, jax.numpy as jnp
from jax import lax
import numpy as np

D_MODEL = 2048
BATCH = 2
SEQ = 4096
DEPTH = 2

HEAD_DIM = 64
HEADS_PER_GROUP = 16
DILATED_GROUPS = ((128, 1), (512, 4), (2048, 16))
N_ATT_HEADS = HEADS_PER_GROUP * len(DILATED_GROUPS)
ATT_WIDTH = N_ATT_HEADS * HEAD_DIM
ROPE_THETA = 500000.0
ROT_DIM = HEAD_DIM // 4

SG_CHUNK = 128
SG_WIDTH = 2 * D_MODEL
SG_GROUPS = 16
SG_GROUP_DIM = SG_WIDTH // SG_GROUPS

N_EXPERTS = 32
TOP_K = 4
EXPERT_FF = D_MODEL
SWIGLU_LIMIT = 7.0
SWIGLU_ALPHA = 1.702
MOE_BLOCK = 128

N_MIXERS = 2
N_MODULATIONS = 6
N_ATT_LAYERS = (DEPTH + 1) // 2
N_SG_LAYERS = DEPTH // 2
DEEPNORM_ALPHA = (2 * DEPTH) ** 0.25
DEEPNORM_BETA = (8 * DEPTH) ** -0.25
LN_EPS = 1e-5

kernel_name = 'hybrid_dilated_sgmlp_moe_deepnorm'


def layer_norm(x, g, b):
    xf = x.astype(jnp.float32)
    mu = jnp.mean(xf, axis=-1, keepdims=True)
    var = jnp.mean(jnp.square(xf - mu), axis=-1, keepdims=True)
    return ((xf - mu) * lax.rsqrt(var + LN_EPS) * g + b).astype(x.dtype)


def rotary_tables(positions):
    inv = jnp.power(jnp.float32(ROPE_THETA), -jnp.arange(0, ROT_DIM, 2, dtype=jnp.float32) / ROT_DIM)
    ang = positions.astype(jnp.float32)[..., None] * inv
    return jnp.cos(ang)[:, :, None, :], jnp.sin(ang)[:, :, None, :]


def apply_partial_rotary(t, cos, sin):
    half = ROT_DIM // 2
    t1 = t[..., :half].astype(jnp.float32)
    t2 = t[..., half:ROT_DIM].astype(jnp.float32)
    rot = jnp.concatenate([t1 * cos - t2 * sin, t2 * cos + t1 * sin], axis=-1).astype(t.dtype)
    return jnp.concatenate([rot, t[..., ROT_DIM:]], axis=-1)


def dilated_window_attention(q, k, v, window, dilation):
    B, S, H, E = q.shape
    L = S // dilation
    blk = window // dilation
    nblk = -(-L // blk)
    Lp = nblk * blk

    def strided(t):
        return t.reshape(B, L, dilation, H, E).transpose(0, 2, 1, 3, 4)

    qs = jnp.pad(strided(q), ((0, 0), (0, 0), (0, Lp - L), (0, 0), (0, 0))).reshape(B, dilation, nblk, blk, H, E)

    def key_pairs(t):
        tp = jnp.pad(strided(t), ((0, 0), (0, 0), (blk, Lp - L), (0, 0), (0, 0)))
        tp = tp.reshape(B, dilation, nblk + 1, blk, H, E)
        return jnp.concatenate([tp[:, :, :-1], tp[:, :, 1:]], axis=3)

    ks, vs = key_pairs(k), key_pairs(v)
    s = jnp.einsum('bdnqhe,bdnkhe->bdnhqk', qs, ks, preferred_element_type=jnp.float32) * (E ** -0.5)
    q_loc = jnp.arange(blk)[:, None] + blk
    k_loc = jnp.arange(2 * blk)[None, :]
    rel = q_loc - k_loc
    key_idx = jnp.arange(nblk)[:, None] * blk + jnp.arange(2 * blk)[None, :] - blk
    valid = ((rel >= 0) & (rel <= blk))[None, :, :] & (key_idx >= 0)[:, None, :]
    s = jnp.where(valid[None, None, :, None], s, -jnp.inf)
    m = jnp.max(s, axis=-1, keepdims=True)
    p = jnp.exp(s - m)
    den = jnp.sum(p, axis=-1, keepdims=True)
    o = jnp.einsum('bdnhqk,bdnkhe->bdnqhe', p / den, vs.astype(jnp.float32))
    lse = (m + jnp.log(den))[..., 0]
    o = o.reshape(B, dilation, Lp, H, E)[:, :, :L].transpose(0, 2, 1, 3, 4).reshape(B, S, H, E)
    lse = lse.transpose(0, 1, 2, 4, 3).reshape(B, dilation, Lp, H)[:, :, :L].transpose(0, 2, 1, 3).reshape(B, S, H)
    return o, lse


def dilated_attention_mixer(h, cos, sin, w_qkv, w_o):
    B, S, _ = h.shape
    qkv = (h @ w_qkv).reshape(B, S, 3, N_ATT_HEADS, HEAD_DIM)
    q = apply_partial_rotary(qkv[:, :, 0], cos, sin)
    k = apply_partial_rotary(qkv[:, :, 1], cos, sin)
    v = qkv[:, :, 2]
    outs, lses = [], []
    for g, (window, dilation) in enumerate(DILATED_GROUPS):
        sl = slice(g * HEADS_PER_GROUP, (g + 1) * HEADS_PER_GROUP)
        o, l = dilated_window_attention(q[:, :, sl], k[:, :, sl], v[:, :, sl], window, dilation)
        outs.append(o)
        lses.append(l)
    weights = jax.nn.softmax(jnp.stack(lses, axis=0), axis=0)
    mixed = jnp.concatenate([outs[g] * weights[g][..., None] for g in range(len(DILATED_GROUPS))], axis=2)
    return mixed.reshape(B, S, ATT_WIDTH).astype(h.dtype) @ w_o


def spatial_gating_mixer(h, w_in, b_in, ln_g, ln_b, w_sp, b_sp, w_out):
    B, S, _ = h.shape
    z = jax.nn.gelu(h @ w_in + b_in, approximate=False)
    u, v = z[..., :SG_WIDTH], z[..., SG_WIDTH:]
    v = layer_norm(v, ln_g, ln_b).reshape(B, S // SG_CHUNK, SG_CHUNK, SG_GROUPS, SG_GROUP_DIM)
    causal = jnp.tril(jnp.ones((SG_CHUNK, SG_CHUNK), dtype=bool))
    w_causal = jnp.where(causal[None], w_sp, 0)
    mixed = jnp.einsum('gts,bnsgc->bntgc', w_causal, v) + b_sp.T[None, None, :, :, None]
    gated = u * mixed.reshape(B, S, SG_WIDTH)
    return gated @ w_out


def clamped_swiglu(gu):
    gate = jnp.minimum(gu[..., :EXPERT_FF], SWIGLU_LIMIT)
    up = jnp.clip(gu[..., EXPERT_FF:], -SWIGLU_LIMIT, SWIGLU_LIMIT)
    return (up + 1.0) * gate * jax.nn.sigmoid(SWIGLU_ALPHA * gate)


def moe_ffn(h, router_w, router_b, w_in, b_in, w_out, b_out):
    B, S, D = h.shape
    T = B * S
    xt = h.reshape(T, D)
    logits = (xt @ router_w + router_b).astype(jnp.float32)
    top_val, top_idx = lax.top_k(logits, TOP_K)
    probs = jax.nn.softmax(top_val, axis=-1)
    flat_e = top_idx.reshape(-1)
    flat_t = jnp.repeat(jnp.arange(T, dtype=jnp.int32), TOP_K)
    flat_w = probs.reshape(-1)
    onehot = jax.nn.one_hot(flat_e, N_EXPERTS, dtype=jnp.int32)
    counts = jnp.sum(onehot, axis=0)
    rank = jnp.take_along_axis(jnp.cumsum(onehot, axis=0) - onehot, flat_e[:, None], axis=1)[:, 0]
    padded = (counts + MOE_BLOCK - 1) // MOE_BLOCK * MOE_BLOCK
    ends = jnp.cumsum(padded)
    starts = ends - padded
    dest = starts[flat_e] + rank
    n_rows = T * TOP_K + N_EXPERTS * MOE_BLOCK
    n_blocks = n_rows // MOE_BLOCK
    row_tok = jnp.zeros((n_rows,), jnp.int32).at[dest].set(flat_t)
    row_w = jnp.zeros((n_rows,), jnp.float32).at[dest].set(flat_w)
    block_start = jnp.arange(n_blocks) * MOE_BLOCK
    block_e = jnp.minimum(jnp.sum(block_start[:, None] >= ends[None, :], axis=1), N_EXPERTS - 1)

    def expert_block(args):
        tok, e = args
        xb = xt[tok]
        hid = clamped_swiglu(xb @ w_in[e] + b_in[e])
        return hid @ w_out[e] + b_out[e]

    yb = lax.map(expert_block, (row_tok.reshape(n_blocks, MOE_BLOCK), block_e))
    out = jnp.zeros((T, D), jnp.float32).at[row_tok].add(yb.reshape(n_rows, D) * row_w[:, None])
    return out.reshape(B, S, D).astype(h.dtype)


def setup_inputs(seed: int = 0) -> dict:
    key = jax.random.key(seed)
    ks = jax.random.split(key, 24)
    f32 = jnp.float32
    nrm = lambda k, shape, scale: jax.random.normal(k, shape, f32) * scale
    x = nrm(ks[0], (BATCH, SEQ, D_MODEL), 1.0)
    c = nrm(ks[1], (BATCH, D_MODEL), 1.0)
    offset = jax.random.randint(ks[2], (BATCH, 1), 0, 4096, dtype=jnp.int32)
    positions = offset + jnp.arange(SEQ, dtype=jnp.int32)[None, :]
    cond_w = nrm(ks[3], (DEPTH, D_MODEL, N_MODULATIONS * D_MODEL), 0.5 * D_MODEL ** -0.5)
    cond_b = nrm(ks[4], (DEPTH, N_MODULATIONS * D_MODEL), 0.02)
    ln_g = 1.0 + nrm(ks[5], (DEPTH, 2, D_MODEL), 0.02)
    ln_b = nrm(ks[6], (DEPTH, 2, D_MODEL), 0.02)
    attn_w_qkv = nrm(ks[7], (N_ATT_LAYERS, D_MODEL, 3 * ATT_WIDTH), D_MODEL ** -0.5)
    attn_w_o = nrm(ks[8], (N_ATT_LAYERS, ATT_WIDTH, D_MODEL), DEEPNORM_BETA * ATT_WIDTH ** -0.5)
    sg_w_in = nrm(ks[9], (N_SG_LAYERS, D_MODEL, 2 * SG_WIDTH), D_MODEL ** -0.5)
    sg_b_in = nrm(ks[10], (N_SG_LAYERS, 2 * SG_WIDTH), 0.02)
    sg_ln_g = 1.0 + nrm(ks[11], (N_SG_LAYERS, SG_WIDTH), 0.02)
    sg_ln_b = nrm(ks[12], (N_SG_LAYERS, SG_WIDTH), 0.02)
    sg_w_spatial = nrm(ks[13], (N_SG_LAYERS, SG_GROUPS, SG_CHUNK, SG_CHUNK), SG_CHUNK ** -0.5)
    sg_b_spatial = 1.0 + nrm(ks[14], (N_SG_LAYERS, SG_GROUPS, SG_CHUNK), 0.02)
    sg_w_out = nrm(ks[15], (N_SG_LAYERS, SG_WIDTH, D_MODEL), DEEPNORM_BETA * SG_WIDTH ** -0.5)
    router_w = nrm(ks[16], (DEPTH, D_MODEL, N_EXPERTS), D_MODEL ** -0.5)
    router_b = nrm(ks[17], (DEPTH, N_EXPERTS), 0.01)
    expert_w_in = nrm(ks[18], (DEPTH, N_EXPERTS, D_MODEL, 2 * EXPERT_FF), D_MODEL ** -0.5)
    expert_b_in = nrm(ks[19], (DEPTH, N_EXPERTS, 2 * EXPERT_FF), 0.02)
    expert_w_out = nrm(ks[20], (DEPTH, N_EXPERTS, EXPERT_FF, D_MODEL), DEEPNORM_BETA * EXPERT_FF ** -0.5)
    expert_b_out = nrm(ks[21], (DEPTH, N_EXPERTS, D_MODEL), 0.02)
    return {'x': x, 'c': c, 'positions': positions, 'cond_w': cond_w, 'cond_b': cond_b,
            'ln_g': ln_g, 'ln_b': ln_b, 'attn_w_qkv': attn_w_qkv, 'attn_w_o': attn_w_o,
            'sg_w_in': sg_w_in, 'sg_b_in': sg_b_in, 'sg_ln_g': sg_ln_g, 'sg_ln_b': sg_ln_b,
            'sg_w_spatial': sg_w_spatial, 'sg_b_spatial': sg_b_spatial, 'sg_w_out': sg_w_out,
            'router_w': router_w, 'router_b': router_b, 'expert_w_in': expert_w_in,
            'expert_b_in': expert_b_in, 'expert_w_out': expert_w_out, 'expert_b_out': expert_b_out}


def reference(x, c, positions, cond_w, cond_b, ln_g, ln_b, attn_w_qkv, attn_w_o,
              sg_w_in, sg_b_in, sg_ln_g, sg_ln_b, sg_w_spatial, sg_b_spatial, sg_w_out,
              router_w, router_b, expert_w_in, expert_b_in, expert_w_out, expert_b_out):
    cos, sin = rotary_tables(positions)
    c_act = jax.nn.silu(c)
    for i in range(DEPTH):
        mod = (c_act @ cond_w[i] + cond_b[i])[:, None, :]
        shift_m, scale_m, gate_m, shift_f, scale_f, gate_f = jnp.split(mod, N_MODULATIONS, axis=-1)
        h = x * (1.0 + scale_m) + shift_m
        j = i // N_MIXERS
        if i % N_MIXERS == 0:
            y = dilated_attention_mixer(h, cos, sin, attn_w_qkv[j], attn_w_o[j])
        else:
            y = spatial_gating_mixer(h, sg_w_in[j], sg_b_in[j], sg_ln_g[j], sg_ln_b[j],
                                     sg_w_spatial[j], sg_b_spatial[j], sg_w_out[j])
        x = layer_norm(DEEPNORM_ALPHA * x + (1.0 + gate_m) * y, ln_g[i, 0], ln_b[i, 0])
        h = x * (1.0 + scale_f) + shift_f
        y = moe_ffn(h, router_w[i], router_b[i], expert_w_in[i], expert_b_in[i],
                    expert_w_out[i], expert_b_out[i])
        x = layer_norm(DEEPNORM_ALPHA * x + (1.0 + gate_f) * y, ln_g[i, 1], ln_b[i, 1])
    return x
```

```python
import numpy as np
from contextlib import ExitStack
import concourse.bass as bass
import concourse.mybir as mybir
from concourse.bass_utils import run_bass_kernel_spmd

F32 = mybir.dt.float32
BF16 = mybir.dt.bfloat16
I32 = mybir.dt.int32
U32 = mybir.dt.uint32
AF = mybir.ActivationFunctionType
ALU = mybir.AluOpType
AX = mybir.AxisListType

D = 2048
NCORES = 8
TOK = 1024
NTOK = 8192
SEQ = 4096
DEPTH = 2
ALPHA = float((2 * DEPTH) ** 0.25)
LN_EPS = 1e-5
NEXP = 32
NLOC = 4
NBLK = 20
FF = 2048
BIGIDX = 1.0e6


class T:
    def __init__(self, t, name=""):
        self.t = t
        self.name = name
        self.w = {}
        self.r = {}

    def __getitem__(self, idx):
        return self.t[idx]


class Sched:
    NDMA = 20

    def __init__(self, nc, es):
        self.nc = nc
        self.es = es
        self.eng = {"pe": nc.tensor, "dve": nc.vector, "act": nc.scalar, "pool": nc.gpsimd, "sp": nc.sync}
        self.sem = {}
        self.cnt = {}
        self.known = {}
        for k in self.eng:
            self.sem[k] = es.enter_context(nc.semaphore("sem_" + k))
            self.cnt[k] = 0
            self.known[k] = {}
        self.dsem = {}
        self.dcnt = {}
        self.dptr = {}
        for q in ("sp", "pool", "act"):
            self.dsem[q] = [es.enter_context(nc.semaphore(f"dma_{q}_{i}")) for i in range(self.NDMA)]
            self.dcnt[q] = [0] * self.NDMA
            self.dptr[q] = 0
        self.ntensors = 0

    def sb(self, es, name, shape, dt):
        self.ntensors += 1
        return T(es.enter_context(self.nc.sbuf_tensor(f"{name}_{self.ntensors}", list(shape), dt)), name)

    def ps(self, es, name, shape, dt=F32):
        self.ntensors += 1
        return T(es.enter_context(self.nc.psum_tensor(f"{name}_{self.ntensors}", list(shape), dt)), name)

    def dram(self, name, shape, dt, kind="Internal"):
        h = self.nc.dram_tensor(name, list(shape), dt, kind=kind)
        return T(h.ap(), name)

    def _wait(self, e, sem, val):
        k = self.known[e]
        key = id(sem)
        if k.get(key, 0) >= val:
            return
        self.eng[e].wait_ge(sem, val)
        k[key] = val

    def _deps(self, e, reads, writes, acc):
        own = id(self.sem[e]) if e == "pe" else None
        for b in list(reads) + list(writes):
            if acc and any(b is x for x in writes):
                continue
            for key, (sem, val) in b.w.items():
                if key == own:
                    continue
                self._wait(e, sem, val)
        for b in writes:
            for key, (sem, val) in b.r.items():
                if key == own:
                    continue
                self._wait(e, sem, val)

    def _update(self, sem, val, reads, writes, acc):
        key = id(sem)
        for b in writes:
            if not acc:
                b.w = {}
                b.r = {}
            b.w[key] = (sem, val)
        for b in reads:
            if any(b is x for x in writes):
                continue
            b.r[key] = (sem, val)

    def op(self, e, fn, reads=(), writes=(), acc=False):
        self._deps(e, reads, writes, acc)
        ins = fn(self.eng[e])
        self.cnt[e] += 1
        ins.then_inc(self.sem[e], 1)
        self._update(self.sem[e], self.cnt[e], reads, writes, acc)
        return ins

    def dma(self, q, out, in_, reads=(), writes=(), acc=False, fn=None):
        i = self.dptr[q] % self.NDMA
        self.dptr[q] += 1
        sem = self.dsem[q][i]
        if self.dcnt[q][i] > 0:
            self._wait(q, sem, 16 * self.dcnt[q][i])
        self._deps(q, reads, writes, acc)
        if fn is None:
            ins = self.eng[q].dma_start(out=out, in_=in_)
        else:
            ins = fn(self.eng[q])
        ins.then_inc(sem, 16)
        self.dcnt[q][i] += 1
        self._update(sem, 16 * self.dcnt[q][i], reads, writes, acc)
        return ins

    def barrier(self):
        evs = []
        for k in self.eng:
            if self.cnt[k] > 0:
                evs.append((self.sem[k], self.cnt[k]))
        for q in self.dsem:
            for i in range(self.NDMA):
                if self.dcnt[q][i] > 0:
                    evs.append((self.dsem[q][i], 16 * self.dcnt[q][i]))
        for e in self.eng:
            for sem, val in evs:
                if e == "pe" and sem is self.sem["pe"]:
                    continue
                self._wait(e, sem, val)

    def finish(self, outs):
        for b in outs:
            for key, (sem, val) in b.w.items():
                self._wait("sp", sem, val)


class Rot:
    def __init__(self, items):
        self.items = items
        self.i = 0

    def next(self):
        x = self.items[self.i % len(self.items)]
        self.i += 1
        return x


def new_nc():
    return bass.Bass("TRN2", target_bir_lowering=False)


def make_consts(S, es):
    C = {}
    io_f_i = S.sb(es, "io_f_i", [128, 128], I32)
    io_p_i = S.sb(es, "io_p_i", [128, 1], I32)
    C["io_f"] = S.sb(es, "io_f", [128, 128], F32)
    C["io_p"] = S.sb(es, "io_p", [128, 1], F32)
    S.op("pool", lambda e: e.iota(io_f_i[:], pattern=[[1, 128]], base=0, channel_multiplier=0), writes=[io_f_i])
    S.op("pool", lambda e: e.iota(io_p_i[:], pattern=[[0, 1]], base=0, channel_multiplier=1), writes=[io_p_i])
    S.op("dve", lambda e: e.tensor_copy(out=C["io_f"][:], in_=io_f_i[:]), reads=[io_f_i], writes=[C["io_f"]])
    S.op("dve", lambda e: e.tensor_copy(out=C["io_p"][:], in_=io_p_i[:]), reads=[io_p_i], writes=[C["io_p"]])
    C["ident32"] = S.sb(es, "ident32", [128, 128], F32)
    C["ident16"] = S.sb(es, "ident16", [128, 128], BF16)
    S.op("dve", lambda e: e.tensor_scalar(out=C["ident32"][:], in0=C["io_f"][:], scalar1=C["io_p"][:, 0:1], scalar2=None, op0=ALU.is_equal),
         reads=[C["io_f"], C["io_p"]], writes=[C["ident32"]])
    S.op("dve", lambda e: e.tensor_copy(out=C["ident16"][:], in_=C["ident32"][:]), reads=[C["ident32"]], writes=[C["ident16"]])
    C["ones16"] = S.sb(es, "ones16", [128, 128], BF16)
    S.op("dve", lambda e: e.memset(C["ones16"][:], 1.0), writes=[C["ones16"]])
    C["ones32"] = S.sb(es, "ones32", [128, 128], F32)
    S.op("dve", lambda e: e.memset(C["ones32"][:], 1.0), writes=[C["ones32"]])
    return C


def transpose_chunks(S, C, src, src_ap_fn, nchunks, dst, dst_ap_fn, psrot, R=128, dt16=True, evac="act", NP=128):
    ident = C["ident16"] if dt16 else C["ident32"]
    for c0 in range(0, nchunks, 4):
        n = min(4, nchunks - c0)
        ps = psrot.next()
        for i in range(n):
            S.op("pe", lambda e, i=i: e.transpose(ps[0:NP, i, 0:R], src_ap_fn(c0 + i), ident[0:R, 0:R]), reads=[src, ident], writes=[ps], acc=(i > 0))
        if evac == "act":
            S.op("act", lambda e: e.activation(out=dst_ap_fn(c0, n), in_=ps[0:NP, 0:n, 0:R], func=AF.Copy), reads=[ps], writes=[dst], acc=True)
        else:
            S.op("dve", lambda e: e.tensor_copy(out=dst_ap_fn(c0, n), in_=ps[0:NP, 0:n, 0:R]), reads=[ps], writes=[dst], acc=True)


def build_moe(ntok=NTOK, nloc=NLOC, nblk=NBLK, ctx=None):
    NT = ntok // 128
    KC = D // 128
    FC = FF // 128
    BR = 512
    POOL = nblk * BR
    ZROW = POOL
    nc = ctx.nc if ctx else new_nc()
    with ExitStack() as es:
        S = ctx.S if ctx else Sched(nc, es)
        DR = (lambda name, shape, dt, kind="Internal": ctx.t[name]) if ctx else S.dram
        VR = (lambda k: ctx.vr[k]) if ctx else None
        h_all = DR("h_all", [ntok, D], BF16, kind="ExternalInput")
        idx_all = DR("idx_all", [ntok, 4], I32, kind="ExternalInput")
        p_all = DR("p_all", [ntok, 4], F32, kind="ExternalInput")
        eids = DR("eids", [1, nloc], F32, kind="ExternalInput")
        w_in = DR("w_in", [nloc * 16 * 128, 4096], F32, kind="ExternalInput")
        b_in = DR("b_in", [nloc * 128, 2 * FC], F32, kind="ExternalInput")
        w_out = DR("w_out", [nloc * 8 * 128, 4096], F32, kind="ExternalInput")
        b_out = DR("b_out", [nloc, D], F32, kind="ExternalInput")
        partial = DR("partial", [ntok, D], F32, kind="ExternalOutput")
        xs = DR("xs", [POOL, D], BF16)
        ysh = [DR(f"ys{i}", [POOL + 128, D // 2], F32) for i in range(2)]

        C = ctx.C if ctx else make_consts(S, es)
        breg_sc = nc.gpsimd.to_reg(POOL - 1)
        breg_ga = nc.gpsimd.to_reg(POOL + 127)
        dest_sc = S.sb(es, "dest_sc", [128, NT, 4], I32)
        dest_ga = S.sb(es, "dest_ga", [128, NT, 4], I32)
        G = S.sb(es, "G", [128, NT, 4], F32)
        widx_in = S.sb(es, "widx_in", [128, nblk, 16], I32)
        widx_out = S.sb(es, "widx_out", [128, nblk, 8], I32)
        bidx_in = S.sb(es, "bidx_in", [128, nblk], I32)
        bidx_out = S.sb(es, "bidx_out", [128, nblk], I32)
        breg_w = nc.gpsimd.to_reg(nloc * 16 * 128 - 1)

        with ExitStack() as e1:
            idx_i = S.sb(e1, "idx_i", [128, NT, 4], I32)
            idx_f = S.sb(e1, "idx_f", [128, NT, 4], F32)
            p_sb = S.sb(e1, "p_sb", [128, NT, 4], F32)
            eid_bc = S.sb(e1, "eid_bc", [128, nloc], F32)
            eq = S.sb(e1, "eq", [128, NT, 4], F32)
            M = S.sb(e1, "M", [128, NT, nloc], F32)
            M16 = S.sb(e1, "M16", [128, NT, nloc], BF16)
            Ls = S.sb(e1, "Ls", [128, 128], BF16)
            within = S.sb(e1, "within", [128, NT, nloc], F32)
            colsum = S.sb(e1, "colsum", [128, NT, nloc], F32)
            off = S.sb(e1, "off", [128, NT, nloc], F32)
            dest = S.sb(e1, "dest", [128, NT, nloc], F32)
            tmp = S.sb(e1, "tmp", [128, NT, nloc], F32)
            cnt = S.sb(e1, "cnt", [128, nloc], F32)
            nb = S.sb(e1, "nb", [128, nloc], F32)
            nbi = S.sb(e1, "nbi", [128, nloc], I32)
            bstart = S.sb(e1, "bstart", [128, nloc], F32)
            bend = S.sb(e1, "bend", [128, nloc], F32)
            base = S.sb(e1, "base", [128, nloc], F32)
            eblk_f = S.sb(e1, "eblk_f", [128, nblk], F32)
            etmp = S.sb(e1, "etmp", [128, nblk], F32)
            NPC = (NT * nloc + 511) // 512
            psA = S.ps(e1, "psA", [128, NPC, 512], F32)
            psB = S.ps(e1, "psB", [128, NPC, 512], F32)
            zero16 = S.sb(e1, "zero16", [128, D], BF16)
            zero32 = S.sb(e1, "zero32", [128, D // 2], F32)
            S.op("dve", lambda e: e.memset(zero16[:], 0.0), writes=[zero16])
            S.op("dve", lambda e: e.memset(zero32[:], 0.0), writes=[zero32])
            for r in range(POOL // 128):
                S.dma("sp", xs[r * 128:(r + 1) * 128, :], zero16[:], reads=[zero16], writes=[xs], acc=True)
            for i in range(2):
                S.dma("sp", ysh[i][ZROW:ZROW + 128, :], zero32[:], reads=[zero32], writes=[ysh[i]], acc=True)
            if ctx:
                rt_sb = S.sb(e1, "rt_sb", [128, NT, 8], F32)
                S.dma("sp", rt_sb[:], ctx.t["RT"][:, :].rearrange("(t p) k -> p t k", p=128), reads=[ctx.t["RT"]], writes=[rt_sb])
                S.op("dve", lambda e: e.tensor_copy(out=p_sb[:], in_=rt_sb[:, :, 4:8]), reads=[rt_sb], writes=[p_sb])
            else:
                S.dma("sp", idx_i[:], idx_all[:, :].rearrange("(t p) k -> p t k", p=128), writes=[idx_i])
                S.dma("sp", p_sb[:], p_all[:, :].rearrange("(t p) k -> p t k", p=128), writes=[p_sb])
            S.dma("sp", eid_bc[:], eids[0:1, :].partition_broadcast(128), writes=[eid_bc])
            if ctx:
                S.op("dve", lambda e: e.tensor_copy(out=idx_f[:], in_=rt_sb[:, :, 0:4]), reads=[rt_sb], writes=[idx_f])
            else:
                S.op("dve", lambda e: e.tensor_copy(out=idx_f[:], in_=idx_i[:]), reads=[idx_i], writes=[idx_f])
            S.op("dve", lambda e: e.tensor_scalar(out=Ls[:], in0=C["io_f"][:], scalar1=C["io_p"][:, 0:1], scalar2=None, op0=ALU.is_gt),
                 reads=[C["io_f"], C["io_p"]], writes=[Ls])
            for j in range(nloc):
                S.op("dve", lambda e, j=j: e.tensor_scalar(out=eq[:], in0=idx_f[:], scalar1=eid_bc[:, j:j + 1], scalar2=None, op0=ALU.is_equal),
                     reads=[idx_f, eid_bc], writes=[eq])
                S.op("dve", lambda e, j=j: e.tensor_reduce(out=M[:, :, j], in_=eq[:], axis=AX.X, op=ALU.add), reads=[eq], writes=[M], acc=True)
            S.op("dve", lambda e: e.tensor_copy(out=M16[:], in_=M[:]), reads=[M], writes=[M16])
            Mf = M16[:].rearrange("p t j -> p (t j)")
            W_ = NT * nloc
            for pc in range(NPC):
                a0, a1 = pc * 512, min(W_, (pc + 1) * 512)
                S.op("pe", lambda e: e.matmul(psA[:, pc, 0:a1 - a0], Ls[:], Mf[:, a0:a1], start=True, stop=True), reads=[Ls, M16], writes=[psA], acc=(pc > 0))
                S.op("pe", lambda e: e.matmul(psB[:, pc, 0:a1 - a0], C["ones16"][:], Mf[:, a0:a1], start=True, stop=True), reads=[C["ones16"], M16], writes=[psB], acc=(pc > 0))
                S.op("dve", lambda e: e.tensor_copy(out=within[:].rearrange("p t j -> p (t j)")[:, a0:a1], in_=psA[:, pc, 0:a1 - a0]), reads=[psA], writes=[within], acc=(pc > 0))
                S.op("dve", lambda e: e.tensor_copy(out=colsum[:].rearrange("p t j -> p (t j)")[:, a0:a1], in_=psB[:, pc, 0:a1 - a0]), reads=[psB], writes=[colsum], acc=(pc > 0))
            S.op("dve", lambda e: e.memset(off[:, 0, :], 0.0), writes=[off])
            for t in range(1, NT):
                S.op("dve", lambda e, t=t: e.tensor_tensor(out=off[:, t, :], in0=off[:, t - 1, :], in1=colsum[:, t - 1, :], op=ALU.add),
                     reads=[off, colsum], writes=[off])
            S.op("dve", lambda e: e.tensor_tensor(out=cnt[:], in0=off[:, NT - 1, :], in1=colsum[:, NT - 1, :], op=ALU.add), reads=[off, colsum], writes=[cnt])
            S.op("dve", lambda e: e.tensor_scalar(out=nb[:], in0=cnt[:], scalar1=1.0 / BR, scalar2=(BR - 1.0) / BR - 0.5 + 0.5 / BR, op0=ALU.mult, op1=ALU.add), reads=[cnt], writes=[nb])
            S.op("dve", lambda e: e.tensor_copy(out=nbi[:], in_=nb[:]), reads=[nb], writes=[nbi])
            S.op("dve", lambda e: e.tensor_copy(out=nb[:], in_=nbi[:]), reads=[nbi], writes=[nb])
            S.op("dve", lambda e: e.memset(bstart[:, 0:1], 0.0), writes=[bstart])
            for j in range(1, nloc):
                S.op("dve", lambda e, j=j: e.tensor_tensor(out=bstart[:, j:j + 1], in0=bstart[:, j - 1:j], in1=nb[:, j - 1:j], op=ALU.add), reads=[bstart, nb], writes=[bstart])
            S.op("dve", lambda e: e.tensor_tensor(out=bend[:], in0=bstart[:], in1=nb[:], op=ALU.add), reads=[bstart, nb], writes=[bend])
            S.op("dve", lambda e: e.tensor_scalar(out=base[:], in0=bstart[:], scalar1=float(BR), scalar2=None, op0=ALU.mult), reads=[bstart], writes=[base])
            S.op("dve", lambda e: e.memset(eblk_f[:], 0.0), writes=[eblk_f])
            for j in range(nloc - 1):
                S.op("dve", lambda e, j=j: e.tensor_scalar(out=etmp[:], in0=C["io_f"][:, 0:nblk], scalar1=bend[:, j:j + 1], scalar2=None, op0=ALU.is_ge), reads=[C["io_f"], bend], writes=[etmp])
                S.op("dve", lambda e: e.tensor_tensor(out=eblk_f[:], in0=eblk_f[:], in1=etmp[:], op=ALU.add), reads=[eblk_f, etmp], writes=[eblk_f])
            cpi = S.sb(e1, "cpi", [128, 16], F32)
            wtmp = S.sb(e1, "wtmp", [128, nblk, 16], F32)
            btmp = S.sb(e1, "btmp", [128, nblk], F32)
            for c in range(16):
                S.op("dve", lambda e, c=c: e.tensor_scalar(out=cpi[:, c:c + 1], in0=C["io_p"][:, 0:1], scalar1=float(c * 128), scalar2=None, op0=ALU.add), reads=[C["io_p"]], writes=[cpi], acc=(c > 0))
            for rb in range(nblk):
                S.op("dve", lambda e, rb=rb: e.scalar_tensor_tensor(out=wtmp[:, rb, :], in0=eblk_f[:, rb:rb + 1].to_broadcast([128, 16]), scalar=2048.0, in1=cpi[:], op0=ALU.mult, op1=ALU.add),
                     reads=[eblk_f, cpi], writes=[wtmp], acc=(rb > 0))
            S.op("dve", lambda e: e.tensor_copy(out=widx_in[:], in_=wtmp[:]), reads=[wtmp], writes=[widx_in])
            for rb in range(nblk):
                S.op("dve", lambda e, rb=rb: e.scalar_tensor_tensor(out=wtmp[:, rb, 0:8], in0=eblk_f[:, rb:rb + 1].to_broadcast([128, 8]), scalar=1024.0, in1=cpi[:, 0:8], op0=ALU.mult, op1=ALU.add),
                     reads=[eblk_f, cpi], writes=[wtmp], acc=(rb > 0))
            S.op("dve", lambda e: e.tensor_copy(out=widx_out[:], in_=wtmp[:, :, 0:8]), reads=[wtmp], writes=[widx_out])
            S.op("dve", lambda e: e.scalar_tensor_tensor(out=btmp[:], in0=eblk_f[:], scalar=128.0, in1=C["io_p"][:, 0:1].to_broadcast([128, nblk]), op0=ALU.mult, op1=ALU.add), reads=[eblk_f, C["io_p"]], writes=[btmp])
            S.op("dve", lambda e: e.tensor_copy(out=bidx_in[:], in_=btmp[:]), reads=[btmp], writes=[bidx_in])
            S.op("dve", lambda e: e.tensor_copy(out=bidx_out[:], in_=eblk_f[:]), reads=[eblk_f], writes=[bidx_out])
            S.op("dve", lambda e: e.tensor_tensor(out=dest[:], in0=within[:], in1=off[:], op=ALU.add), reads=[within, off], writes=[dest])
            for j in range(nloc):
                S.op("dve", lambda e, j=j: e.tensor_scalar(out=dest[:, :, j], in0=dest[:, :, j], scalar1=base[:, j:j + 1], scalar2=float(ZROW), op0=ALU.add, op1=ALU.min),
                     reads=[dest, base], writes=[dest])
            lk = S.sb(e1, "lk", [128, NT, 4], F32)
            dk = S.sb(e1, "dk", [128, NT, 4], F32)
            tk = S.sb(e1, "tk", [128, NT, 4], F32)
            S.op("dve", lambda e: e.memset(lk[:], 0.0), writes=[lk])
            S.op("dve", lambda e: e.memset(dk[:], 0.0), writes=[dk])
            for j in range(nloc):
                S.op("dve", lambda e, j=j: e.tensor_scalar(out=eq[:], in0=idx_f[:], scalar1=eid_bc[:, j:j + 1], scalar2=None, op0=ALU.is_equal),
                     reads=[idx_f, eid_bc], writes=[eq])
                S.op("dve", lambda e: e.tensor_tensor(out=lk[:], in0=lk[:], in1=eq[:], op=ALU.add), reads=[lk, eq], writes=[lk])
                S.op("dve", lambda e, j=j: e.tensor_tensor(out=tk[:], in0=eq[:], in1=dest[:, :, j:j + 1].to_broadcast([128, NT, 4]), op=ALU.mult), reads=[eq, dest], writes=[tk])
                S.op("dve", lambda e: e.tensor_tensor(out=dk[:], in0=dk[:], in1=tk[:], op=ALU.add), reads=[dk, tk], writes=[dk])
            S.op("dve", lambda e: e.tensor_tensor(out=G[:], in0=lk[:], in1=p_sb[:], op=ALU.mult), reads=[lk, p_sb], writes=[G])
            S.op("dve", lambda e: e.scalar_tensor_tensor(out=tk[:], in0=dk[:], scalar=-BIGIDX, in1=lk[:], op0=ALU.add, op1=ALU.mult), reads=[dk, lk], writes=[tk])
            S.op("dve", lambda e: e.tensor_scalar(out=tk[:], in0=tk[:], scalar1=BIGIDX, scalar2=None, op0=ALU.add), reads=[tk], writes=[tk])
            S.op("dve", lambda e: e.tensor_copy(out=dest_sc[:], in_=tk[:]), reads=[tk], writes=[dest_sc])
            S.op("dve", lambda e: e.scalar_tensor_tensor(out=tk[:], in0=dk[:], scalar=-float(ZROW), in1=lk[:], op0=ALU.add, op1=ALU.mult), reads=[dk, lk], writes=[tk])
            S.op("dve", lambda e: e.tensor_scalar(out=tk[:], in0=tk[:], scalar1=float(ZROW), scalar2=None, op0=ALU.add), reads=[tk], writes=[tk])
            S.op("dve", lambda e: e.tensor_copy(out=dest_ga[:], in_=tk[:]), reads=[tk], writes=[dest_ga])
            hbufs = Rot([S.sb(e1, f"hb{i}", [128, D], BF16) for i in range(3)])
            for t in range(NT):
                hb = hbufs.next()
                S.dma("sp", hb[:], h_all[t * 128:(t + 1) * 128, :], writes=[hb])
                for j in range(4):
                    S.dma("pool", None, None, reads=[hb, dest_sc], writes=[xs], acc=not (t == 0 and j == 0),
                          fn=lambda e, t=t, j=j, hb=hb: e.indirect_dma_start(
                              out=xs[:, :], out_offset=bass.IndirectOffsetOnAxis(ap=dest_sc[:, t, j:j + 1], axis=0),
                              in_=hb[:, :], in_offset=None, bounds_check=breg_sc, oob_is_err=False))
            S.barrier()

        with ExitStack() as e2:
            xsT = Rot([S.sb(e2, f"xsT{i}", [128, KC, BR], BF16) for i in range(2)])
            hidT = Rot([S.sb(e2, f"hidT{i}", [128, FC, BR], BF16) for i in range(2)])
            wst = Rot([S.sb(e2, f"wst{i}", [128, 16, 256], F32) for i in range(2)])
            wb = Rot([S.sb(e2, f"wb{i}", [128, 16, 256], BF16) for i in range(6)])
            xrow = Rot([S.sb(e2, f"xrow{i}", [128, D], BF16) for i in range(2)])
            bblk = Rot([S.sb(e2, f"bblk{i}", [128, 2 * FC], F32) for i in range(2)])
            bout = Rot([S.sb(e2, f"bout{i}", [128, D], F32) for i in range(1)])
            pst = Rot([S.ps(e2, f"pst{i}", [128, 4, 128], BF16) for i in range(2)])
            psg = Rot([S.ps(e2, f"psg{i}", [128, 512], F32) for i in range(2)])
            psu = Rot([S.ps(e2, f"psu{i}", [128, 512], F32) for i in range(2)])
            psy = Rot([S.ps(e2, f"psy{i}", [128, 512], F32) for i in range(2)])
            g_sb = Rot([S.sb(e2, f"g_sb{i}", [128, 512], F32) for i in range(2)])
            s_sb = Rot([S.sb(e2, f"s_sb{i}", [128, 512], F32) for i in range(2)])
            u_sb = Rot([S.sb(e2, f"u_sb{i}", [128, 512], F32) for i in range(2)])
            y_sb = Rot([S.sb(e2, f"y_sb{i}", [128, 256], F32) for i in range(3)])
            ncast = [0]

            def wchunk(src, idx_ap, idx_T):
                st = wst.next()
                S.dma("pool", None, None, reads=[src, idx_T], writes=[st],
                      fn=lambda e: e.indirect_dma_start(out=st[:].rearrange("p k n -> p (k n)"), out_offset=None, in_=src[:, :],
                                                        in_offset=bass.IndirectOffsetOnAxis(ap=idx_ap, axis=0), bounds_check=breg_w, oob_is_err=False))
                w = wb.next()
                ncast[0] += 1
                if ncast[0] % 3 == 0:
                    S.op("dve", lambda e: e.tensor_copy(out=w[:], in_=st[:]), reads=[st], writes=[w])
                else:
                    S.op("act", lambda e: e.activation(out=w[:], in_=st[:], func=AF.Copy), reads=[st], writes=[w])
                return w

            for rb in range(nblk):
                bb = bblk.next()
                S.dma("pool", None, None, reads=[b_in, bidx_in], writes=[bb],
                      fn=lambda e, bb=bb: e.indirect_dma_start(out=bb[:, :], out_offset=None, in_=b_in[:, :],
                                                               in_offset=bass.IndirectOffsetOnAxis(ap=bidx_in[:, rb:rb + 1], axis=0), bounds_check=breg_w, oob_is_err=False))
                bo = bout.next()
                S.dma("pool", None, None, reads=[b_out, bidx_out], writes=[bo],
                      fn=lambda e, bo=bo: e.indirect_dma_start(out=bo[:, :], out_offset=None, in_=b_out[:, :],
                                                               in_offset=bass.IndirectOffsetOnAxis(ap=bidx_out[:, rb:rb + 1], axis=0), bounds_check=breg_w, oob_is_err=False))
                xt = xsT.next()
                ht = hidT.next()
                for rt in range(BR // 128):
                    xr = xrow.next()
                    S.dma("sp", xr[:], xs[rb * BR + rt * 128:rb * BR + (rt + 1) * 128, :], reads=[xs], writes=[xr])
                    transpose_chunks(S, C, xr, lambda c, xr=xr: xr[:, c * 128:(c + 1) * 128], KC, xt,
                                     lambda c0, n, rt=rt: xt[:, c0:c0 + n, rt * 128:(rt + 1) * 128], pst)
                for cp in range(8):
                    wg = wchunk(w_in, widx_in[:, rb, cp:cp + 1], widx_in)
                    wu = wchunk(w_in, widx_in[:, rb, 8 + cp:8 + cp + 1], widx_in)
                    for sub in range(2):
                        fc = cp * 2 + sub
                        pg = psg.next()
                        pu = psu.next()
                        for kc in range(KC):
                            S.op("pe", lambda e, kc=kc: e.matmul(pg[:], wg[:, kc, sub * 128:(sub + 1) * 128], xt[:, kc, :], start=(kc == 0), stop=(kc == KC - 1)),
                                 reads=[wg, xt], writes=[pg], acc=(kc > 0))
                        for kc in range(KC):
                            S.op("pe", lambda e, kc=kc: e.matmul(pu[:], wu[:, kc, sub * 128:(sub + 1) * 128], xt[:, kc, :], start=(kc == 0), stop=(kc == KC - 1)),
                                 reads=[wu, xt], writes=[pu], acc=(kc > 0))
                        g = g_sb.next()
                        s_ = s_sb.next()
                        u = u_sb.next()
                        S.op("dve", lambda e: e.tensor_scalar(out=g[:], in0=pg[:], scalar1=bb[:, fc:fc + 1], scalar2=7.0, op0=ALU.add, op1=ALU.min), reads=[pg, bb], writes=[g])
                        S.op("act", lambda e: e.activation(out=s_[:], in_=g[:], func=AF.Silu, scale=1.702), reads=[g], writes=[s_])
                        S.op("dve", lambda e: e.tensor_scalar(out=u[:], in0=pu[:], scalar1=bb[:, FC + fc:FC + fc + 1], scalar2=7.0, op0=ALU.add, op1=ALU.min), reads=[pu, bb], writes=[u])
                        S.op("dve", lambda e: e.tensor_scalar(out=u[:], in0=u[:], scalar1=-7.0, scalar2=1.0, op0=ALU.max, op1=ALU.add), reads=[u], writes=[u])
                        S.op("dve", lambda e: e.scalar_tensor_tensor(out=ht[:, fc, :], in0=s_[:], scalar=1.0 / 1.702, in1=u[:], op0=ALU.mult, op1=ALU.mult),
                             reads=[s_, u], writes=[ht], acc=True)
                for nch in range(8):
                    wo = wchunk(w_out, widx_out[:, rb, nch:nch + 1], widx_out)
                    for rt in range(BR // 128):
                        py = psy.next()
                        for fc in range(FC):
                            S.op("pe", lambda e, fc=fc: e.matmul(py[:, 0:256], ht[:, fc, rt * 128:(rt + 1) * 128], wo[:, fc, :], start=(fc == 0), stop=(fc == FC - 1)),
                                 reads=[ht, wo], writes=[py], acc=(fc > 0))
                        yb = y_sb.next()
                        S.op("dve", lambda e: e.tensor_tensor(out=yb[:], in0=py[:, 0:256], in1=bo[:, nch * 256:(nch + 1) * 256], op=ALU.add), reads=[py, bo], writes=[yb])
                        r0 = rb * BR + rt * 128
                        S.dma("sp", ysh[nch // 4][r0:r0 + 128, (nch % 4) * 256:(nch % 4 + 1) * 256], yb[:], reads=[yb], writes=[ysh[nch // 4]], acc=True)
            S.barrier()

        with ExitStack() as e3:
            H = D // 2
            gts = Rot([[S.sb(e3, f"gt{i}_{j}", [128, H], F32) for j in range(4)] for i in range(2)])
            accs = Rot([S.sb(e3, f"acc{i}", [128, H], F32) for i in range(3)])
            for t in range(NT):
                for hf in range(2):
                    gt = gts.next()
                    for j in range(4):
                        S.dma("pool", None, None, reads=[ysh[hf], dest_ga], writes=[gt[j]],
                              fn=lambda e, t=t, j=j, gt=gt, hf=hf: e.indirect_dma_start(
                                  out=gt[j][:, :], out_offset=None, in_=ysh[hf][:, :],
                                  in_offset=bass.IndirectOffsetOnAxis(ap=dest_ga[:, t, j:j + 1], axis=0),
                                  bounds_check=breg_ga, oob_is_err=False))
                    acc = accs.next()
                    S.op("dve", lambda e: e.tensor_scalar(out=acc[:], in0=gt[0][:], scalar1=G[:, t, 0:1], scalar2=None, op0=ALU.mult), reads=[gt[0], G], writes=[acc])
                    for j in range(1, 4):
                        S.op("dve", lambda e, j=j: e.scalar_tensor_tensor(out=acc[:], in0=gt[j][:], scalar=G[:, t, j:j + 1], in1=acc[:], op0=ALU.mult, op1=ALU.add),
                             reads=[gt[j], G, acc], writes=[acc])
                    if ctx:
                        S.dma("pool", None, None, reads=[acc, ctx.sb["prow"]], writes=[ctx.t["PART%d" % hf]], acc=True,
                              fn=lambda e, t=t, hf=hf, acc=acc: e.indirect_dma_start(
                                  out=ctx.t["PART%d" % hf][:, :], out_offset=bass.IndirectOffsetOnAxis(ap=ctx.sb["prow"][:, t:t + 1], axis=0),
                                  in_=acc[:, :], in_offset=None, bounds_check=ctx.breg_part, oob_is_err=False))
                    else:
                        S.dma("sp", partial[t * 128:(t + 1) * 128, hf * H:(hf + 1) * H], acc[:], reads=[acc], writes=[partial], acc=True)
        if not ctx:
            S.finish([partial])
    return nc


def load_bc(S, q, dst, src, row_ap):
    S.dma(q, dst[:], row_ap.partition_broadcast(128), reads=[src], writes=[dst])


def plus_one(S, t):
    S.op("dve", lambda e: e.tensor_scalar(out=t[:], in0=t[:], scalar1=1.0, scalar2=None, op0=ALU.add), reads=[t], writes=[t])


def ln_tile(S, v, g_bc, b_bc, out, st, mv, rstd):
    for c in range(4):
        S.op("dve", lambda e, c=c: e.bn_stats(out=st[:, c, :], in_=v[:, c * 512:(c + 1) * 512]), reads=[v], writes=[st], acc=(c > 0))
    S.op("dve", lambda e: e.bn_aggr(out=mv[:], in_=st[:].rearrange("p c s -> p (c s)")), reads=[st], writes=[mv])
    S.op("act", lambda e: e.activation(out=rstd[:], in_=mv[:, 1:2], func=AF.Sqrt, bias=LN_EPS, scale=1.0), reads=[mv], writes=[rstd])
    S.op("dve", lambda e: e.reciprocal(out=rstd[:], in_=rstd[:]), reads=[rstd], writes=[rstd])
    S.op("dve", lambda e: e.tensor_scalar(out=v[:], in0=v[:], scalar1=mv[:, 0:1], scalar2=rstd[:, 0:1], op0=ALU.subtract, op1=ALU.mult), reads=[v, mv, rstd], writes=[v])
    S.op("dve", lambda e: e.tensor_tensor(out=v[:], in0=v[:], in1=g_bc[:], op=ALU.mult), reads=[v, g_bc], writes=[v])
    S.op("dve", lambda e: e.tensor_tensor(out=out[:], in0=v[:], in1=b_bc[:], op=ALU.add), reads=[v, b_bc], writes=[out])


def residual_ln(S, x, y, g1_bc, lng_bc, lnb_bc, out, st, mv, rstd):
    S.op("dve", lambda e: e.tensor_tensor(out=y[:], in0=y[:], in1=g1_bc[:], op=ALU.mult), reads=[y, g1_bc], writes=[y])
    S.op("dve", lambda e: e.scalar_tensor_tensor(out=y[:], in0=x[:], scalar=ALPHA, in1=y[:], op0=ALU.mult, op1=ALU.add), reads=[x, y], writes=[y])
    ln_tile(S, y, lng_bc, lnb_bc, out, st, mv, rstd)


class Tail:
    def __init__(self, S, C, es, sc_row, sh_row, vecs, wr_d, br_d, x_out, h_out, idx_out, p_out, scat=None):
        self.scat = scat
        self.S, self.C = S, C
        self.sc1 = S.sb(es, "t_sc1", [128, D], F32)
        self.sh = S.sb(es, "t_sh", [128, D], F32)
        load_bc(S, "sp", self.sc1, vecs, sc_row)
        plus_one(S, self.sc1)
        load_bc(S, "sp", self.sh, vecs, sh_row)
        self.wr = S.sb(es, "t_wr", [128, D // 128, NEXP], F32)
        S.dma("sp", self.wr[:], wr_d[:, :].rearrange("(kc p) e -> p kc e", p=128), writes=[self.wr])
        self.br = S.sb(es, "t_br", [1, NEXP], F32)
        S.dma("sp", self.br[:], br_d[0:1, :], writes=[self.br])
        self.h32 = Rot([S.sb(es, f"t_h32_{i}", [128, D], F32) for i in range(2)])
        self.h16 = Rot([S.sb(es, f"t_h16_{i}", [128, D], BF16) for i in range(2)])
        self.hT = S.sb(es, "t_hT", [128, D // 128, 128], F32)
        self.pst = Rot([S.ps(es, f"t_pst{i}", [128, 4, 128], F32) for i in range(2)])
        self.pl = S.ps(es, "t_pl", [128, NEXP], F32)
        self.lg = S.sb(es, "t_lg", [128, NEXP], F32)
        self.top = S.sb(es, "t_top", [128, 8], F32)
        self.topi = S.sb(es, "t_topi", [128, 8], U32)
        self.negm = S.sb(es, "t_negm", [128, 1], F32)
        self.ex = S.sb(es, "t_ex", [128, 4], F32)
        self.sm = S.sb(es, "t_sm", [128, 1], F32)
        self.x_out, self.h_out, self.idx_out, self.p_out = x_out, h_out, idx_out, p_out
        self.rt = Rot([S.sb(es, f"t_rt{i}", [128, 8], F32) for i in range(2)])

    def run(self, t, xn):
        S, C = self.S, self.C
        r0 = t * 128
        S.dma("sp", self.x_out[r0:r0 + 128, :], xn[:], reads=[xn], writes=[self.x_out], acc=True)
        h32 = self.h32.next()
        h16 = self.h16.next()
        S.op("dve", lambda e: e.tensor_tensor(out=h32[:], in0=xn[:], in1=self.sc1[:], op=ALU.mult), reads=[xn, self.sc1], writes=[h32])
        S.op("dve", lambda e: e.tensor_tensor(out=h32[:], in0=h32[:], in1=self.sh[:], op=ALU.add), reads=[h32, self.sh], writes=[h32])
        S.op("act", lambda e: e.activation(out=h16[:], in_=h32[:], func=AF.Copy), reads=[h32], writes=[h16])
        if self.scat is None:
            S.dma("sp", self.h_out[r0:r0 + 128, :], h16[:], reads=[h16], writes=[self.h_out], acc=True)
        else:
            sc = self.scat
            S.dma("pool", None, None, reads=[h16, sc["rowidx"]], writes=[sc["H"]], acc=True,
                  fn=lambda e: e.indirect_dma_start(out=sc["H"][:, :], out_offset=bass.IndirectOffsetOnAxis(ap=sc["rowidx"][:, sc["tile0"] + t:sc["tile0"] + t + 1], axis=0),
                                                    in_=h16[:, :], in_offset=None, bounds_check=sc["breg"], oob_is_err=False))
        transpose_chunks(S, C, h32, lambda c: h32[:, c * 128:(c + 1) * 128], D // 128, self.hT,
                         lambda c0, n: self.hT[:, c0:c0 + n, :], self.pst, dt16=False, evac="act")
        KC = D // 128
        for kc in range(KC):
            S.op("pe", lambda e, kc=kc: e.matmul(self.pl[:], self.hT[:, kc, :], self.wr[:, kc, :], start=(kc == 0), stop=False),
                 reads=[self.hT, self.wr], writes=[self.pl], acc=(kc > 0))
        S.op("pe", lambda e: e.matmul(self.pl[:], C["ones32"][0:1, :], self.br[0:1, :], start=False, stop=True), reads=[C["ones32"], self.br], writes=[self.pl], acc=True)
        S.op("dve", lambda e: e.tensor_copy(out=self.lg[:], in_=self.pl[:]), reads=[self.pl], writes=[self.lg])
        S.op("dve", lambda e: e.max(out=self.top[:], in_=self.lg[:]), reads=[self.lg], writes=[self.top])
        S.op("dve", lambda e: e.max_index(out=self.topi[:], in_max=self.top[:], in_values=self.lg[:]), reads=[self.lg, self.top], writes=[self.topi])
        S.op("dve", lambda e: e.tensor_scalar(out=self.negm[:], in0=self.top[:, 0:1], scalar1=-1.0, scalar2=None, op0=ALU.mult), reads=[self.top], writes=[self.negm])
        S.op("act", lambda e: e.activation(out=self.ex[:], in_=self.top[:, 0:4], func=AF.Exp, bias=self.negm[:, 0:1], scale=1.0, accum_out=self.sm[:]),
             reads=[self.top, self.negm], writes=[self.ex, self.sm])
        S.op("dve", lambda e: e.reciprocal(out=self.sm[:], in_=self.sm[:]), reads=[self.sm], writes=[self.sm])
        S.op("dve", lambda e: e.tensor_scalar(out=self.ex[:], in0=self.ex[:], scalar1=self.sm[:, 0:1], scalar2=None, op0=ALU.mult), reads=[self.ex, self.sm], writes=[self.ex])
        if self.scat is None:
            S.dma("sp", self.idx_out[r0:r0 + 128, :], self.topi[:, 0:4], reads=[self.topi], writes=[self.idx_out], acc=True)
            S.dma("sp", self.p_out[r0:r0 + 128, :], self.ex[:], reads=[self.ex], writes=[self.p_out], acc=True)
        else:
            sc = self.scat
            rt = self.rt.next()
            S.op("dve", lambda e: e.tensor_copy(out=rt[:, 0:4], in_=self.topi[:, 0:4]), reads=[self.topi], writes=[rt])
            S.op("dve", lambda e: e.tensor_copy(out=rt[:, 4:8], in_=self.ex[:]), reads=[self.ex], writes=[rt], acc=True)
            S.dma("pool", None, None, reads=[rt, sc["rowidx"]], writes=[sc["RT"]], acc=True,
                  fn=lambda e: e.indirect_dma_start(out=sc["RT"][:, :], out_offset=bass.IndirectOffsetOnAxis(ap=sc["rowidx"][:, sc["tile0"] + t:sc["tile0"] + t + 1], axis=0),
                                                    in_=rt[:, :], in_offset=None, bounds_check=sc["breg"], oob_is_err=False))


class PartSum:
    def __init__(self, S, es, parts, x_in, vecs, gate_row, lng_row, lnb_row, loader=None):
        self.S = S
        self.loader = loader
        self.parts, self.x_in = parts, x_in
        self.g1 = S.sb(es, "ps_g1", [128, D], F32)
        self.lng = S.sb(es, "ps_lng", [128, D], F32)
        self.lnb = S.sb(es, "ps_lnb", [128, D], F32)
        load_bc(S, "sp", self.g1, vecs, gate_row)
        plus_one(S, self.g1)
        load_bc(S, "sp", self.lng, vecs, lng_row)
        load_bc(S, "sp", self.lnb, vecs, lnb_row)
        self.pb = Rot([S.sb(es, f"ps_pb{i}", [128, D], F32) for i in range(3)])
        self.acc = Rot([S.sb(es, f"ps_acc{i}", [128, D], F32) for i in range(2)])
        self.xb = Rot([S.sb(es, f"ps_xb{i}", [128, D], F32) for i in range(2)])
        self.out = Rot([S.sb(es, f"ps_out{i}", [128, D], F32) for i in range(2)])
        self.st = S.sb(es, "ps_st", [128, 4, 6], F32)
        self.mv = S.sb(es, "ps_mv", [128, 2], F32)
        self.rstd = S.sb(es, "ps_rstd", [128, 1], F32)

    def run(self, t):
        S = self.S
        r0 = t * 128
        acc = self.acc.next()
        if self.loader is None:
            S.dma("sp", acc[:], self.parts[0, r0:r0 + 128, :], reads=[self.parts], writes=[acc])
            for c in range(1, NCORES):
                pb = self.pb.next()
                S.dma("act" if c % 2 else "sp", pb[:], self.parts[c, r0:r0 + 128, :], reads=[self.parts], writes=[pb])
                S.op("dve", lambda e: e.tensor_tensor(out=acc[:], in0=acc[:], in1=pb[:], op=ALU.add), reads=[acc, pb], writes=[acc])
        else:
            self.loader(S, 0, t, acc)
            for c in range(1, self.loader.nparts):
                pb = self.pb.next()
                self.loader(S, c, t, pb)
                S.op("dve", lambda e: e.tensor_tensor(out=acc[:], in0=acc[:], in1=pb[:], op=ALU.add), reads=[acc, pb], writes=[acc])
        xb = self.xb.next()
        S.dma("sp", xb[:], self.x_in[r0:r0 + 128, :], reads=[self.x_in], writes=[xb])
        out = self.out.next()
        residual_ln(S, xb, acc, self.g1, self.lng, self.lnb, out, self.st, self.mv, self.rstd)
        return out


def build_final(ctx=None):
    nc = ctx.nc if ctx else new_nc()
    with ExitStack() as es:
        S = ctx.S if ctx else Sched(nc, es)
        DR = (lambda name, shape, dt, kind="Internal": ctx.t[name]) if ctx else S.dram
        VR = (lambda k: ctx.vr[k]) if ctx else None
        parts = DR("parts", [NCORES, TOK, D], F32, kind="ExternalInput")
        x_in = DR("x_in", [TOK, D], F32, kind="ExternalInput")
        vecs = DR("vecs", [3, D], F32, kind="ExternalInput")
        out = DR("out", [TOK, D], F32, kind="ExternalOutput")
        P = PartSum(S, es, parts, x_in, vecs, (VR(0) if ctx else vecs[0:1, :]), (VR(1) if ctx else vecs[1:2, :]), (VR(2) if ctx else vecs[2:3, :]), loader=(ctx.part_loader if ctx else None))
        for t in range(TOK // 128):
            o = P.run(t)
            S.dma("sp", out[t * 128:(t + 1) * 128, :], o[:], reads=[o], writes=[out], acc=True)
        if not ctx:
            S.finish([out])
    return nc


MODW = 6 * D // NCORES


def build_cond():
    nc = new_nc()
    KC = D // 128
    with ExitStack() as es:
        S = Sched(nc, es)
        cT = S.dram("cT", [128, KC, 2], F32, kind="ExternalInput")
        cw = S.dram("cw", [DEPTH, D, MODW], F32, kind="ExternalInput")
        cb = S.dram("cb", [DEPTH, MODW], F32, kind="ExternalInput")
        out = S.dram("mod", [DEPTH, 2, MODW], F32, kind="ExternalOutput")
        C = make_consts(S, es)
        ct = S.sb(es, "ct", [128, KC, 2], F32)
        S.dma("sp", ct[:], cT[:, :, :], writes=[ct])
        S.op("act", lambda e: e.activation(out=ct[:], in_=ct[:], func=AF.Silu), reads=[ct], writes=[ct])
        wbs = Rot([S.sb(es, f"cwb{i}", [128, KC, 512], F32) for i in range(3)])
        brs = Rot([S.sb(es, f"cbr{i}", [1, 512], F32) for i in range(2)])
        pss = Rot([S.ps(es, f"cps{i}", [2, 512], F32) for i in range(2)])
        obs = Rot([S.sb(es, f"cob{i}", [2, 512], F32) for i in range(2)])
        for l in range(DEPTH):
            for nch in range(MODW // 512):
                wbuf = wbs.next()
                S.dma("sp" if nch % 2 == 0 else "act", wbuf[:], cw[l, :, nch * 512:(nch + 1) * 512].rearrange("(kc p) n -> p kc n", p=128), reads=[cw], writes=[wbuf])
                br = brs.next()
                S.dma("sp", br[:], cb[l:l + 1, nch * 512:(nch + 1) * 512], reads=[cb], writes=[br])
                ps = pss.next()
                for kc in range(KC):
                    S.op("pe", lambda e, kc=kc: e.matmul(ps[:], ct[:, kc, :], wbuf[:, kc, :], start=(kc == 0), stop=False), reads=[ct, wbuf], writes=[ps], acc=(kc > 0))
                S.op("pe", lambda e: e.matmul(ps[:], C["ones32"][0:1, 0:2], br[0:1, :], start=False, stop=True), reads=[C["ones32"], br], writes=[ps], acc=True)
                ob = obs.next()
                S.op("dve", lambda e: e.tensor_copy(out=ob[:], in_=ps[:]), reads=[ps], writes=[ob])
                S.dma("sp", out[l, :, nch * 512:(nch + 1) * 512], ob[:], reads=[ob], writes=[out], acc=True)
        S.finish([out])
    return nc


SGW = 4096
SGC = SGW // 128


def build_mid(ctx=None):
    nc = ctx.nc if ctx else new_nc()
    KC = D // 128
    NT = TOK // 128
    with ExitStack() as es:
        S = ctx.S if ctx else Sched(nc, es)
        DR = (lambda name, shape, dt, kind="Internal": ctx.t[name]) if ctx else S.dram
        VR = (lambda k: ctx.vr[k]) if ctx else None
        parts = DR("parts", [NCORES, TOK, D], F32, kind="ExternalInput")
        x_in = DR("x_in", [TOK, D], F32, kind="ExternalInput")
        vecs = DR("vecs", [10, D], F32, kind="ExternalInput")
        sg_w_in = DR("sg_w_in", [D, 2 * SGW], F32, kind="ExternalInput")
        bu_pk = DR("bu_pk", [128, SGC], F32, kind="ExternalInput")
        bv = DR("bv", [1, SGW], F32, kind="ExternalInput")
        lng_pk = DR("lng_pk", [128, SGC], F32, kind="ExternalInput")
        lnb_pk = DR("lnb_pk", [128, SGC], F32, kind="ExternalInput")
        w_sp = DR("w_sp", [16, 128, 128], F32, kind="ExternalInput")
        b_sp = DR("b_sp", [1, 16 * 128], F32, kind="ExternalInput")
        sg_w_out = DR("sg_w_out", [SGW, D], F32, kind="ExternalInput")
        wr_d = DR("wr", [D, NEXP], F32, kind="ExternalInput")
        br_d = DR("br", [1, NEXP], F32, kind="ExternalInput")
        x_out = DR("x_out", [TOK, D], F32, kind="ExternalOutput")
        h_out = DR("h_out", [TOK, D], BF16, kind="ExternalOutput")
        idx_out = DR("idx_out", [TOK, 4], U32, kind="ExternalOutput")
        p_out = DR("p_out", [TOK, 4], F32, kind="ExternalOutput")
        x2_scr = DR("x2_scr", [TOK, D], F32)
        y_scr = DR("y_scr", [TOK, D], F32)

        C = ctx.C if ctx else make_consts(S, es)
        hT = S.sb(es, "hT", [128, KC, TOK], BF16)
        with ExitStack() as e1:
            P = PartSum(S, e1, parts, x_in, vecs, (VR(0) if ctx else vecs[0:1, :]), (VR(1) if ctx else vecs[1:2, :]), (VR(2) if ctx else vecs[2:3, :]), loader=(ctx.part_loader if ctx else None))
            sc1 = S.sb(e1, "sc1m", [128, D], F32)
            sh = S.sb(e1, "shm", [128, D], F32)
            load_bc(S, "sp", sc1, vecs, (VR(4) if ctx else vecs[4:5, :]))
            plus_one(S, sc1)
            load_bc(S, "sp", sh, vecs, (VR(3) if ctx else vecs[3:4, :]))
            h16s = Rot([S.sb(e1, f"h16_{i}", [128, D], BF16) for i in range(2)])
            htmp = S.sb(e1, "htmp", [128, D], F32)
            pst = Rot([S.ps(e1, f"pst1_{i}", [128, 4, 128], BF16) for i in range(2)])
            for t in range(NT):
                x2 = P.run(t)
                S.dma("sp", x2_scr[t * 128:(t + 1) * 128, :], x2[:], reads=[x2], writes=[x2_scr], acc=True)
                h16 = h16s.next()
                S.op("dve", lambda e: e.tensor_tensor(out=htmp[:], in0=x2[:], in1=sc1[:], op=ALU.mult), reads=[x2, sc1], writes=[htmp])
                S.op("dve", lambda e: e.tensor_tensor(out=h16[:], in0=htmp[:], in1=sh[:], op=ALU.add), reads=[htmp, sh], writes=[h16])
                transpose_chunks(S, C, h16, lambda c, h16=h16: h16[:, c * 128:(c + 1) * 128], KC, hT,
                                 lambda c0, n, t=t: hT[:, c0:c0 + n, t * 128:(t + 1) * 128], pst)
            S.barrier()
        with ExitStack() as e2:
            WcT = S.sb(e2, "WcT", [128, 16, 128], BF16)
            addend = S.sb(e2, "addend", [128, SGC, 128], F32)
            lngp = S.sb(e2, "lngp", [128, SGC], F32)
            lnbp = S.sb(e2, "lnbp", [128, SGC], F32)
            bup = S.sb(e2, "bup", [128, SGC], F32)
            S.dma("sp", lngp[:], lng_pk[:, :], writes=[lngp])
            S.dma("sp", lnbp[:], lnb_pk[:, :], writes=[lnbp])
            S.dma("sp", bup[:], bu_pk[:, :], writes=[bup])
            pst = Rot([S.ps(e2, f"pst2_{i}", [128, 4, 128], BF16) for i in range(2)])
            psm = Rot([S.ps(e2, f"psm{i}", [128, 512], F32) for i in range(3)])
            pss = Rot([S.ps(e2, f"pss{i}", [128, 4, 128], F32) for i in range(2)])
            with ExitStack() as e2a:
                wsp32 = S.sb(e2a, "wsp32", [128, 16, 128], F32)
                wsp16 = S.sb(e2a, "wsp16", [128, 16, 128], BF16)
                tril = S.sb(e2a, "tril", [128, 128], F32)
                rsw = S.sb(e2a, "rsw", [128, 16, 128], F32)
                bsp = S.sb(e2a, "bsp", [128, 16, 128], F32)
                S.dma("sp", wsp32[:], w_sp[:, :, :].rearrange("g t s -> t g s"), writes=[wsp32])
                S.dma("sp", bsp[:].rearrange("p g t -> p (g t)"), b_sp[0:1, :].partition_broadcast(128), writes=[bsp])
                S.op("dve", lambda e: e.tensor_scalar(out=tril[:], in0=C["io_f"][:], scalar1=C["io_p"][:, 0:1], scalar2=None, op0=ALU.is_le),
                     reads=[C["io_f"], C["io_p"]], writes=[tril])
                for g in range(16):
                    S.op("dve", lambda e, g=g: e.tensor_tensor(out=wsp16[:, g, :], in0=wsp32[:, g, :], in1=tril[:], op=ALU.mult), reads=[wsp32, tril], writes=[wsp16], acc=(g > 0))
                transpose_chunks(S, C, wsp16, lambda c: wsp16[:, c, :], 16, WcT, lambda c0, n: WcT[:, c0:c0 + n, :], pst)
                for q in range(4):
                    ps = psm.next()
                    S.op("pe", lambda e, q=q: e.matmul(ps[:], C["ones16"][:], WcT[:, 4 * q:4 * q + 4, :].rearrange("p g t -> p (g t)"), start=True, stop=True),
                         reads=[C["ones16"], WcT], writes=[ps])
                    S.op("dve", lambda e, q=q: e.tensor_copy(out=rsw[:, 4 * q:4 * q + 4, :].rearrange("p g t -> p (g t)"), in_=ps[:]), reads=[ps], writes=[rsw], acc=(q > 0))
                for fc in range(SGC):
                    g = fc // 2
                    S.op("dve", lambda e, fc=fc, g=g: e.scalar_tensor_tensor(out=addend[:, fc, :], in0=rsw[:, g, :], scalar=lnbp[:, fc:fc + 1], in1=bsp[:, g, :], op0=ALU.mult, op1=ALU.add),
                         reads=[rsw, lnbp, bsp], writes=[addend], acc=(fc > 0))
                S.barrier()
            v16 = [S.sb(e2, f"v16_{i}", [128, SGW], BF16) for i in range(2)]
            mixedT = S.sb(e2, "mixedT", [128, SGC, 512], BF16)
            wb = Rot([S.sb(e2, f"wbm{i}", [128, 16, 512], BF16) for i in range(4)])
            brow = Rot([S.sb(e2, f"browm{i}", [1, 512], BF16) for i in range(2)])
            u_sb = Rot([S.sb(e2, f"u_sb{i}", [128, 512], F32) for i in range(2)])
            y_sb = Rot([S.sb(e2, f"y_sbm{i}", [128, 512], F32) for i in range(3)])
            st8 = S.sb(e2, "st8", [128, 8, 6], F32)
            mv = S.sb(e2, "mvm", [128, 2], F32)
            rstd = S.sb(e2, "rstdm", [128, 1], F32)
            for half in range(2):
                for pair in range(2):
                    for nch in range(SGW // 512):
                        w = wb.next()
                        S.dma("pool", w[:], sg_w_in[:, SGW + nch * 512:SGW + (nch + 1) * 512].rearrange("(kc p) n -> p kc n", p=128), reads=[sg_w_in], writes=[w])
                        br = brow.next()
                        S.dma("pool", br[:], bv[0:1, nch * 512:(nch + 1) * 512], reads=[bv], writes=[br])
                        for tl in range(2):
                            t = half * 4 + pair * 2 + tl
                            ps = psm.next()
                            for kc in range(KC):
                                S.op("pe", lambda e, kc=kc: e.matmul(ps[:], hT[:, kc, t * 128:(t + 1) * 128], w[:, kc, :], start=(kc == 0), stop=False),
                                     reads=[hT, w], writes=[ps], acc=(kc > 0))
                            S.op("pe", lambda e: e.matmul(ps[:], C["ones16"][0:1, :], br[0:1, :], start=False, stop=True), reads=[C["ones16"], br], writes=[ps], acc=True)
                            S.op("act", lambda e, tl=tl: e.activation(out=v16[tl][:, nch * 512:(nch + 1) * 512], in_=ps[:], func=AF.Gelu), reads=[ps], writes=[v16[tl]], acc=True)
                    for tl in range(2):
                        tloc = pair * 2 + tl
                        v = v16[tl]
                        for c in range(8):
                            S.op("dve", lambda e, c=c: e.bn_stats(out=st8[:, c, :], in_=v[:, c * 512:(c + 1) * 512]), reads=[v], writes=[st8], acc=(c > 0))
                        S.op("dve", lambda e: e.bn_aggr(out=mv[:], in_=st8[:].rearrange("p c s -> p (c s)")), reads=[st8], writes=[mv])
                        S.op("act", lambda e: e.activation(out=rstd[:], in_=mv[:, 1:2], func=AF.Sqrt, bias=LN_EPS, scale=1.0), reads=[mv], writes=[rstd])
                        S.op("dve", lambda e: e.reciprocal(out=rstd[:], in_=rstd[:]), reads=[rstd], writes=[rstd])
                        S.op("dve", lambda e: e.tensor_scalar(out=v[:], in0=v[:], scalar1=mv[:, 0:1], scalar2=rstd[:, 0:1], op0=ALU.subtract, op1=ALU.mult), reads=[v, mv, rstd], writes=[v])
                        for fc0 in range(0, SGC, 4):
                            ps = pss.next()
                            for i in range(4):
                                fc = fc0 + i
                                S.op("pe", lambda e, i=i, fc=fc: e.matmul(ps[:, i, :], v[:, fc * 128:(fc + 1) * 128], WcT[:, fc // 2, :], start=True, stop=True),
                                     reads=[v, WcT], writes=[ps], acc=(i > 0))
                            for i in range(4):
                                fc = fc0 + i
                                S.op("dve", lambda e, i=i, fc=fc: e.scalar_tensor_tensor(out=mixedT[:, fc, tloc * 128:(tloc + 1) * 128], in0=ps[:, i, :], scalar=lngp[:, fc:fc + 1], in1=addend[:, fc, :], op0=ALU.mult, op1=ALU.add),
                                     reads=[ps, lngp, addend], writes=[mixedT], acc=True)
                for nch in range(SGW // 512):
                    w = wb.next()
                    S.dma("pool", w[:], sg_w_in[:, nch * 512:(nch + 1) * 512].rearrange("(kc p) n -> p kc n", p=128), reads=[sg_w_in], writes=[w])
                    for sub in range(4):
                        fc = nch * 4 + sub
                        ps = psm.next()
                        for kc in range(KC):
                            S.op("pe", lambda e, kc=kc: e.matmul(ps[:], w[:, kc, sub * 128:(sub + 1) * 128], hT[:, kc, half * 512:(half + 1) * 512], start=(kc == 0), stop=(kc == KC - 1)),
                                 reads=[w, hT], writes=[ps], acc=(kc > 0))
                        u = u_sb.next()
                        S.op("act", lambda e: e.activation(out=u[:], in_=ps[:], func=AF.Gelu, bias=bup[:, fc:fc + 1], scale=1.0), reads=[ps, bup], writes=[u])
                        S.op("dve", lambda e: e.tensor_tensor(out=mixedT[:, fc, :], in0=u[:], in1=mixedT[:, fc, :], op=ALU.mult), reads=[u, mixedT], writes=[mixedT])
                for nch in range(D // 512):
                    wa = wb.next()
                    S.dma("pool", wa[:], sg_w_out[0:2048, nch * 512:(nch + 1) * 512].rearrange("(kc p) n -> p kc n", p=128), reads=[sg_w_out], writes=[wa])
                    wbb = wb.next()
                    S.dma("pool", wbb[:], sg_w_out[2048:4096, nch * 512:(nch + 1) * 512].rearrange("(kc p) n -> p kc n", p=128), reads=[sg_w_out], writes=[wbb])
                    for tloc in range(4):
                        ps = psm.next()
                        for fc in range(SGC):
                            ww = wa if fc < 16 else wbb
                            S.op("pe", lambda e, fc=fc, ww=ww: e.matmul(ps[:], mixedT[:, fc, tloc * 128:(tloc + 1) * 128], ww[:, fc % 16, :], start=(fc == 0), stop=(fc == SGC - 1)),
                                 reads=[mixedT, ww], writes=[ps], acc=(fc > 0))
                        yb = y_sb.next()
                        S.op("act", lambda e: e.activation(out=yb[:], in_=ps[:], func=AF.Copy), reads=[ps], writes=[yb])
                        r0 = (half * 4 + tloc) * 128
                        S.dma("sp", y_scr[r0:r0 + 128, nch * 512:(nch + 1) * 512], yb[:], reads=[yb], writes=[y_scr], acc=True)
            S.barrier()
        with ExitStack() as e3:
            g1 = S.sb(e3, "g1m", [128, D], F32)
            lng = S.sb(e3, "lng3", [128, D], F32)
            lnb = S.sb(e3, "lnb3", [128, D], F32)
            load_bc(S, "sp", g1, vecs, (VR(5) if ctx else vecs[5:6, :]))
            plus_one(S, g1)
            load_bc(S, "sp", lng, vecs, (VR(6) if ctx else vecs[6:7, :]))
            load_bc(S, "sp", lnb, vecs, (VR(7) if ctx else vecs[7:8, :]))
            tail = Tail(S, C, e3, (VR(9) if ctx else vecs[9:10, :]), (VR(8) if ctx else vecs[8:9, :]), vecs, wr_d, br_d, x_out, h_out, idx_out, p_out, scat=(ctx.scat if ctx else None))
            ys = Rot([S.sb(e3, f"y3_{i}", [128, D], F32) for i in range(2)])
            xs_ = Rot([S.sb(e3, f"x3_{i}", [128, D], F32) for i in range(2)])
            outs = Rot([S.sb(e3, f"o3_{i}", [128, D], F32) for i in range(2)])
            st = S.sb(e3, "st3", [128, 4, 6], F32)
            mv3 = S.sb(e3, "mv3", [128, 2], F32)
            rstd3 = S.sb(e3, "rstd3", [128, 1], F32)
            for t in range(NT):
                y = ys.next()
                xx = xs_.next()
                o = outs.next()
                S.dma("sp", y[:], y_scr[t * 128:(t + 1) * 128, :], reads=[y_scr], writes=[y])
                S.dma("act", xx[:], x2_scr[t * 128:(t + 1) * 128, :], reads=[x2_scr], writes=[xx])
                residual_ln(S, xx, y, g1, lng, lnb, o, st, mv3, rstd3)
                tail.run(t, o)
        if not ctx:
            S.finish([x_out, h_out, idx_out, p_out])
    return nc


EXT = 3072
OWN0 = 2048
DILS = (1, 4, 16)
NEG = -30000.0
ROPE_THETA = 500000.0


def group_tiles(g):
    tiles = []
    if g == 0:
        for j in range(9):
            tiles.append(dict(u0=1920 + 128 * j, R=128, q=(j >= 1), prev=j - 1, halo=(j == 1)))
    elif g == 1:
        for rho in range(4):
            for k in range(3):
                tiles.append(dict(u0=4 * (384 + 128 * k) + rho, R=128, q=(k >= 1), prev=rho * 3 + k - 1, halo=(k == 1)))
    else:
        for rho in range(16):
            tiles.append(dict(u0=rho, R=128, q=False, prev=None, halo=False))
        for rho in range(16):
            tiles.append(dict(u0=16 * 128 + rho, R=64, q=True, prev=rho, halo=True))
    return tiles


GT_OFF = (0, 9, 21)
NGT = 53


class _Stop(Exception):
    pass


def build_attn(stop=None, ctx=None):
    nc = ctx.nc if ctx else new_nc()
    KC = D // 128
    NT = TOK // 128
    try:
      with ExitStack() as es:
        S = ctx.S if ctx else Sched(nc, es)
        DR = (lambda name, shape, dt, kind="Internal": ctx.t[name]) if ctx else S.dram
        VR = (lambda k: ctx.vr[k]) if ctx else None
        xext = DR("xext", [EXT, D], F32, kind="ExternalInput")
        vecs = DR("vecs", [8, D], F32, kind="ExternalInput")
        postab = DR("postab", [128, NGT], I32, kind="ExternalInput")
        hb = DR("hb", [3, 128], F32, kind="ExternalInput")
        w_qkv = DR("w_qkv", [D, 9216], F32, kind="ExternalInput")
        w_o = DR("w_o", [3072, D], F32, kind="ExternalInput")
        wr_d = DR("wr", [D, NEXP], F32, kind="ExternalInput")
        br_d = DR("br", [1, NEXP], F32, kind="ExternalInput")
        x_out = DR("x_out", [TOK, D], F32, kind="ExternalOutput")
        h_out = DR("h_out", [TOK, D], BF16, kind="ExternalOutput")
        idx_out = DR("idx_out", [TOK, 4], U32, kind="ExternalOutput")
        p_out = DR("p_out", [TOK, 4], F32, kind="ExternalOutput")
        o_scr = [DR(f"o_scr{g}", [TOK, 1024], F32) for g in range(3)]
        lse_scr = [DR(f"lse_scr{g}", [TOK, 16], F32) for g in range(3)]
        y_scr = DR("y_scr_a", [TOK, D], F32)

        C = ctx.C if ctx else make_consts(S, es)
        with ExitStack() as eA:
            hT = S.sb(eA, "hTx", [128, KC, EXT], BF16)
            if stop == 'consts':
                S.barrier(); raise _Stop()
            maskP = S.sb(eA, "maskP", [128, 256], F32)
            maskH = [S.sb(eA, f"maskH{g}", [128, 256], F32) for g in range(3)]
            S.op("dve", lambda e: e.tensor_scalar(out=maskP[:, 0:128], in0=C["io_f"][:], scalar1=C["io_p"][:, 0:1], scalar2=None, op0=ALU.is_ge),
                 reads=[C["io_f"], C["io_p"]], writes=[maskP])
            S.op("dve", lambda e: e.tensor_scalar(out=maskP[:, 128:256], in0=C["io_f"][:], scalar1=C["io_p"][:, 0:1], scalar2=None, op0=ALU.is_le),
                 reads=[C["io_f"], C["io_p"]], writes=[maskP])
            S.op("dve", lambda e: e.tensor_scalar(out=maskP[:], in0=maskP[:], scalar1=-1.0, scalar2=-NEG, op0=ALU.add, op1=ALU.mult), reads=[maskP], writes=[maskP])
            for g in range(3):
                S.dma("sp", maskH[g][:, 0:128], hb[g:g + 1, :].partition_broadcast(128), writes=[maskH[g]])
                S.op("dve", lambda e, g=g: e.tensor_tensor(out=maskH[g][:, 0:128], in0=maskH[g][:, 0:128], in1=maskP[:, 0:128], op=ALU.add), reads=[maskH[g], maskP], writes=[maskH[g]])
                S.op("dve", lambda e, g=g: e.tensor_copy(out=maskH[g][:, 128:256], in_=maskP[:, 128:256]), reads=[maskP], writes=[maskH[g]], acc=True)
            if stop == 'masks':
                S.barrier(); raise _Stop()
            cos_t = S.sb(eA, "cos_t", [128, NGT, 8], F32)
            sin_t = S.sb(eA, "sin_t", [128, NGT, 8], F32)
            with ExitStack() as e0:
                pos_i = S.sb(e0, "pos_i", [128, NGT], I32)
                pos_f = S.sb(e0, "pos_f", [128, NGT], F32)
                ang = S.sb(e0, "ang", [128, NGT, 8], F32)
                kf = S.sb(e0, "kf", [128, NGT, 8], F32)
                ki = S.sb(e0, "ki", [128, NGT, 8], I32)
                rr = S.sb(e0, "rr", [128, NGT, 8], F32)
                S.dma("sp", pos_i[:], postab[:, :], writes=[pos_i])
                S.op("dve", lambda e: e.tensor_copy(out=pos_f[:], in_=pos_i[:]), reads=[pos_i], writes=[pos_f])
                for f in range(8):
                    inv = float(np.float32(ROPE_THETA) ** np.float32(-f / 8.0))
                    S.op("dve", lambda e, f=f, inv=inv: e.tensor_scalar(out=ang[:, :, f], in0=pos_f[:], scalar1=inv, scalar2=None, op0=ALU.mult), reads=[pos_f], writes=[ang], acc=(f > 0))
                if stop == 'ang':
                    S.barrier(); raise _Stop()
                TWO_PI = 2.0 * float(np.pi)
                for which, dst in ((0, sin_t), (1, cos_t)):
                    if which == 1:
                        S.op("dve", lambda e: e.tensor_scalar(out=ang[:], in0=ang[:], scalar1=float(np.pi) / 2, scalar2=None, op0=ALU.add), reads=[ang], writes=[ang])
                    S.op("dve", lambda e: e.tensor_scalar(out=kf[:], in0=ang[:], scalar1=1.0 / TWO_PI, scalar2=None, op0=ALU.mult), reads=[ang], writes=[kf])
                    S.op("dve", lambda e: e.tensor_copy(out=ki[:], in_=kf[:]), reads=[kf], writes=[ki])
                    S.op("dve", lambda e: e.tensor_copy(out=kf[:], in_=ki[:]), reads=[ki], writes=[kf])
                    S.op("dve", lambda e: e.scalar_tensor_tensor(out=rr[:], in0=kf[:], scalar=-TWO_PI, in1=ang[:], op0=ALU.mult, op1=ALU.add), reads=[kf, ang], writes=[rr])
                    S.op("dve", lambda e: e.tensor_scalar(out=rr[:], in0=rr[:], scalar1=-3.14159, scalar2=3.14159, op0=ALU.max, op1=ALU.min), reads=[rr], writes=[rr])
                    S.op("act", lambda e, dst=dst: e.activation(out=dst[:], in_=rr[:], func=AF.Sin), reads=[rr], writes=[dst])
                if stop == 'tables':
                    S.barrier(); raise _Stop()
                sc1 = S.sb(e0, "sc1a", [128, D], F32)
                sh = S.sb(e0, "sha", [128, D], F32)
                load_bc(S, "sp", sc1, vecs, (VR(1) if ctx else vecs[1:2, :]))
                plus_one(S, sc1)
                load_bc(S, "sp", sh, vecs, (VR(0) if ctx else vecs[0:1, :]))
                xts = Rot([S.sb(e0, f"xta{i}", [128, D], F32) for i in range(3)])
                h16s = Rot([S.sb(e0, f"h16a{i}", [128, D], BF16) for i in range(2)])
                pst0 = Rot([S.ps(e0, f"pst0_{i}", [128, 4, 128], BF16) for i in range(2)])
                for t in range(EXT // 128):
                    xt = xts.next()
                    S.dma("sp", xt[:], xext[t * 128:(t + 1) * 128, :], reads=[xext], writes=[xt])
                    h16 = h16s.next()
                    S.op("dve", lambda e: e.tensor_tensor(out=xt[:], in0=xt[:], in1=sc1[:], op=ALU.mult), reads=[xt, sc1], writes=[xt])
                    S.op("dve", lambda e: e.tensor_tensor(out=h16[:], in0=xt[:], in1=sh[:], op=ALU.add), reads=[xt, sh], writes=[h16])
                    transpose_chunks(S, C, h16, lambda c, h16=h16: h16[:, c * 128:(c + 1) * 128], KC, hT,
                                     lambda c0, n, t=t: hT[:, c0:c0 + n, t * 128:(t + 1) * 128], pst0)
                S.barrier()
            if stop == 'hT':
                raise _Stop()
            KQ = S.sb(eA, "KQ", [128, 32, 2, 128], BF16)
            V = S.sb(eA, "V", [128, 32, 128], BF16)
            KT = S.sb(eA, "KT", [64, 2, 32, 128], BF16)
            QT = S.sb(eA, "QT", [64, 2, 16, 128], BF16)
            lse_sb = S.sb(eA, "lse_sb", [128, 16, 16], F32)
            wq = Rot([S.sb(eA, f"wq{i}", [128, KC, 128], BF16) for i in range(2)])
            wk = Rot([S.sb(eA, f"wk{i}", [128, KC, 128], BF16) for i in range(2)])
            wv = Rot([S.sb(eA, f"wv{i}", [128, KC, 128], BF16) for i in range(2)])
            pq = Rot([S.ps(eA, f"pq{i}", [128, 4, 128], F32) for i in range(2)])
            pst = Rot([S.ps(eA, "pstA", [128, 8, 128], BF16)])
            ps_s = Rot([S.ps(eA, f"ps_s{i}", [128, 2, 256], F32) for i in range(2)])
            ptp = Rot([S.ps(eA, "ptp", [128, 8, 128], BF16)])
            ps_o = Rot([S.ps(eA, "ps_o", [128, 8, 64], F32)])
            tmp = [Rot([S.sb(eA, f"rt{k}_{i}", [128, 4, 8], F32) for i in range(2)]) for k in range(4)]
            sm = Rot([S.sb(eA, f"sm{i}", [128, 2, 256], F32) for i in range(2)])
            Pb = Rot([S.sb(eA, f"Pb{i}", [128, 2, 256], BF16) for i in range(2)])
            PT = Rot([S.sb(eA, f"PT{i}", [128, 4, 128], BF16) for i in range(2)])
            negmx = Rot([S.sb(eA, f"negmx{i}", [128, 2], F32) for i in range(2)])
            rs = Rot([S.sb(eA, f"rs{i}", [128, 2], F32) for i in range(2)])
            lnrs = Rot([S.sb(eA, f"lnrs{i}", [128, 2], F32) for i in range(2)])
            o_sb = Rot([S.sb(eA, f"o_sb{i}", [128, 2, 64], F32) for i in range(3)])
            for g in range(3):
                d = DILS[g]
                tiles = group_tiles(g)
                ntile = len(tiles)
                qtiles = [i for i, tl in enumerate(tiles) if tl["q"]]
                nq = len(qtiles)
                Rq = tiles[qtiles[0]]["R"]
                for hp in range(8):
                    c0 = g * 1024 + hp * 128
                    w_q, w_k, w_v = wq.next(), wk.next(), wv.next()
                    S.dma("pool", w_q[:], w_qkv[:, c0:c0 + 128].rearrange("(kc p) n -> p kc n", p=128), reads=[w_qkv], writes=[w_q])
                    S.dma("pool", w_k[:], w_qkv[:, 3072 + c0:3072 + c0 + 128].rearrange("(kc p) n -> p kc n", p=128), reads=[w_qkv], writes=[w_k])
                    S.dma("pool", w_v[:], w_qkv[:, 6144 + c0:6144 + c0 + 128].rearrange("(kc p) n -> p kc n", p=128), reads=[w_qkv], writes=[w_v])
                    if stop == 'wload':
                        S.barrier(); raise _Stop()
                    for ti, tl in enumerate(tiles):
                        R, u0 = tl["R"], tl["u0"]
                        ps = pq.next()
                        stop_ = u0 + d * (R - 1) + 1
                        mats = [(0, w_k), (2, w_v)] + ([(1, w_q)] if tl["q"] else [])
                        first = True
                        for slot, ww in mats:
                            for kc in range(KC):
                                S.op("pe", lambda e, kc=kc, slot=slot, ww=ww: e.matmul(ps[0:R, slot, :], hT[:, kc, u0:stop_:d], ww[:, kc, :], start=(kc == 0), stop=(kc == KC - 1)),
                                     reads=[hT, ww], writes=[ps], acc=not first)
                                first = False
                        if stop == 'mm0' and ti == 0:
                            S.barrier(); raise _Stop()
                        if stop == 'proj_mm' and ti == 1:
                            S.barrier(); raise _Stop()
                        nx = 2 if tl["q"] else 1
                        src = ps[0:R, 0:nx, :].rearrange("p x (h d) -> p (x h) d", d=64)
                        dst = KQ[0:R, ti, 0:nx, :].rearrange("p x (h d) -> p (x h) d", d=64)
                        gti = GT_OFF[g] + ti
                        cb = cos_t[0:R, gti, :].unsqueeze(1).to_broadcast([R, 2 * nx, 8])
                        sb_ = sin_t[0:R, gti, :].unsqueeze(1).to_broadcast([R, 2 * nx, 8])
                        t1, t2, t3, t4 = (tmp[k].next() for k in range(4))
                        n2 = 2 * nx
                        S.op("dve", lambda e: e.tensor_tensor(out=t1[0:R, 0:n2, :], in0=src[:, :, 0:8], in1=cb, op=ALU.mult), reads=[ps, cos_t], writes=[t1])
                        if stop == 'rot0a' and ti == 0:
                            S.barrier(); raise _Stop()
                        S.op("dve", lambda e: e.tensor_tensor(out=t2[0:R, 0:n2, :], in0=src[:, :, 8:16], in1=sb_, op=ALU.mult), reads=[ps, sin_t], writes=[t2])
                        S.op("dve", lambda e: e.tensor_tensor(out=t3[0:R, 0:n2, :], in0=src[:, :, 8:16], in1=cb, op=ALU.mult), reads=[ps, cos_t], writes=[t3])
                        S.op("dve", lambda e: e.tensor_tensor(out=t4[0:R, 0:n2, :], in0=src[:, :, 0:8], in1=sb_, op=ALU.mult), reads=[ps, sin_t], writes=[t4])
                        S.op("dve", lambda e: e.tensor_tensor(out=dst[:, :, 0:8], in0=t1[0:R, 0:n2, :], in1=t2[0:R, 0:n2, :], op=ALU.subtract), reads=[t1, t2], writes=[KQ], acc=True)
                        S.op("dve", lambda e: e.tensor_tensor(out=dst[:, :, 8:16], in0=t3[0:R, 0:n2, :], in1=t4[0:R, 0:n2, :], op=ALU.add), reads=[t3, t4], writes=[KQ], acc=True)
                        if stop == 'rot0' and ti == 0:
                            S.barrier(); raise _Stop()
                        if stop == 'proj_rot' and ti == 1:
                            S.barrier(); raise _Stop()
                        S.op("dve", lambda e: e.tensor_copy(out=dst[:, :, 16:64], in_=src[:, :, 16:64]), reads=[ps], writes=[KQ], acc=True)
                        if stop == 'cp0' and ti == 0:
                            S.barrier(); raise _Stop()
                        S.op("dve", lambda e: e.tensor_copy(out=V[0:R, ti, :], in_=ps[0:R, 2, :]), reads=[ps], writes=[V], acc=True)
                        if stop == 'cp1' and ti == 0:
                            S.barrier(); raise _Stop()
                    if stop == 'proj':
                        S.barrier(); raise _Stop()
                    for hh in range(2):
                        i0 = 0
                        while i0 < ntile:
                            R = tiles[i0]["R"]
                            i1 = i0
                            while i1 < ntile and tiles[i1]["R"] == R:
                                i1 += 1
                            transpose_chunks(S, C, KQ, lambda c, i0=i0, R=R, hh=hh: KQ[0:R, i0 + c, 0, hh * 64:(hh + 1) * 64], i1 - i0, KT,
                                             lambda c0_, n, i0=i0, R=R, hh=hh: KT[:, hh, i0 + c0_:i0 + c0_ + n, 0:R], pst, R=R, NP=64)
                            i0 = i1
                        transpose_chunks(S, C, KQ, lambda s_, hh=hh: KQ[0:Rq, qtiles[s_], 1, hh * 64:(hh + 1) * 64], nq, QT,
                                         lambda c0_, n, hh=hh: QT[:, hh, c0_:c0_ + n, 0:Rq], pst, R=Rq, NP=64)
                    if stop == 'tr':
                        S.barrier(); raise _Stop()
                    for s, ti in enumerate(qtiles):
                        tl = tiles[ti]
                        pi = tl["prev"]
                        NK = 128 + Rq
                        mask = maskH[g] if tl["halo"] else maskP
                        pss = ps_s.next()
                        for hh in range(2):
                            pb = 64 * hh
                            if stop == 's_mm1' and s == 0 and hp == 0 and g == 0 and hh == 1:
                                S.barrier(); raise _Stop()
                            S.op("pe", lambda e, hh=hh, pb=pb: e.matmul(pss[0:Rq, hh, 0:128], QT[:, hh, s, 0:Rq], KT[:, hh, pi, 0:128], start=True, stop=True),
                                 reads=[QT, KT], writes=[pss], acc=(hh > 0))
                            if stop == 's_mm0' and s == 0 and hp == 0 and g == 0:
                                S.barrier(); raise _Stop()
                            S.op("pe", lambda e, hh=hh, pb=pb: e.matmul(pss[0:Rq, hh, 128:NK], QT[:, hh, s, 0:Rq], KT[:, hh, ti, 0:Rq], start=True, stop=True),
                                 reads=[QT, KT], writes=[pss], acc=True)
                        if stop == 's_mm' and s == 0 and hp == 0 and g == 0:
                            S.barrier(); raise _Stop()
                        smt = sm.next()
                        S.op("dve", lambda e: e.scalar_tensor_tensor(out=smt[0:Rq, :, 0:NK], in0=pss[0:Rq, :, 0:NK], scalar=0.125,
                                                                      in1=mask[0:Rq, 0:NK].unsqueeze(1).to_broadcast([Rq, 2, NK]), op0=ALU.mult, op1=ALU.add),
                             reads=[pss, mask], writes=[smt])
                        if stop == 's_sm' and s == 0 and hp == 0 and g == 0:
                            S.barrier(); raise _Stop()
                        nm = negmx.next()
                        S.op("dve", lambda e: e.tensor_reduce(out=nm[0:Rq, :], in_=smt[0:Rq, :, 0:NK], axis=AX.X, op=ALU.max, negate=True), reads=[smt], writes=[nm])
                        if stop == 's_max' and s == 0 and hp == 0 and g == 0:
                            S.barrier(); raise _Stop()
                        pbt = Pb.next()
                        rst = rs.next()
                        for hh in range(2):
                            S.op("act", lambda e, hh=hh: e.activation(out=pbt[0:Rq, hh, 0:NK], in_=smt[0:Rq, hh, 0:NK], func=AF.Exp, bias=nm[0:Rq, hh:hh + 1], scale=1.0, accum_out=rst[0:Rq, hh:hh + 1]),
                                 reads=[smt, nm], writes=[pbt, rst], acc=(hh > 0))
                        if stop == 's_exp' and s == 0 and hp == 0 and g == 0:
                            S.barrier(); raise _Stop()
                        ptt = ptp.next()
                        first = True
                        for hh in range(2):
                            S.op("pe", lambda e, hh=hh: e.transpose(ptt[0:128, 2 * hh, 0:Rq], pbt[0:Rq, hh, 0:128], C["ident16"][0:Rq, 0:Rq]), reads=[pbt, C["ident16"]], writes=[ptt], acc=not first)
                            first = False
                            S.op("pe", lambda e, hh=hh: e.transpose(ptt[0:Rq, 2 * hh + 1, 0:Rq], pbt[0:Rq, hh, 128:NK], C["ident16"][0:Rq, 0:Rq]), reads=[pbt, C["ident16"]], writes=[ptt], acc=True)
                        if stop == 's_pt' and s == 0 and hp == 0 and g == 0:
                            S.barrier(); raise _Stop()
                        PTt = PT.next()
                        S.op("act", lambda e: e.activation(out=PTt[:, :, 0:Rq], in_=ptt[:, 0:4, 0:Rq], func=AF.Copy), reads=[ptt], writes=[PTt])
                        if stop == 's_ptc' and s == 0 and hp == 0 and g == 0:
                            S.barrier(); raise _Stop()
                        pso = ps_o.next()
                        for hh in range(2):
                            S.op("pe", lambda e, hh=hh: e.matmul(pso[0:Rq, hh, :], PTt[0:128, 2 * hh, 0:Rq], V[0:128, pi, hh * 64:(hh + 1) * 64], start=True, stop=False),
                                 reads=[PTt, V], writes=[pso], acc=(hh > 0))
                            S.op("pe", lambda e, hh=hh: e.matmul(pso[0:Rq, hh, :], PTt[0:Rq, 2 * hh + 1, 0:Rq], V[0:Rq, ti, hh * 64:(hh + 1) * 64], start=False, stop=True),
                                 reads=[PTt, V], writes=[pso], acc=True)
                        if stop == 's_o' and s == 0 and hp == 0 and g == 0:
                            S.barrier(); raise _Stop()
                        lr = lnrs.next()
                        S.op("act", lambda e: e.activation(out=lr[0:Rq, :], in_=rst[0:Rq, :], func=AF.Ln), reads=[rst], writes=[lr])
                        S.op("dve", lambda e: e.tensor_tensor(out=lse_sb[0:Rq, s, 2 * hp:2 * hp + 2], in0=lr[0:Rq, :], in1=nm[0:Rq, :], op=ALU.subtract), reads=[lr, nm], writes=[lse_sb], acc=True)
                        S.op("dve", lambda e: e.reciprocal(out=rst[0:Rq, :], in_=rst[0:Rq, :]), reads=[rst, lr], writes=[rst])
                        if stop == 's_ln' and s == 0 and hp == 0 and g == 0:
                            S.barrier(); raise _Stop()
                        ob = o_sb.next()
                        S.op("dve", lambda e: e.tensor_tensor(out=ob[0:Rq, :, :], in0=pso[0:Rq, 0:2, :], in1=rst[0:Rq, :].unsqueeze(2).to_broadcast([Rq, 2, 64]), op=ALU.mult), reads=[pso, rst], writes=[ob])
                        if stop == 's_ob' and s == 0 and hp == 0 and g == 0:
                            S.barrier(); raise _Stop()
                        n0 = tl["u0"] - OWN0
                        ov = o_scr[g][:, :].rearrange("(m d) c -> d m c", d=d)
                        S.dma("sp", ov[n0 % d, n0 // d:n0 // d + Rq, hp * 128:(hp + 1) * 128], ob[0:Rq, :, :].rearrange("p h d -> p (h d)"), reads=[ob], writes=[o_scr[g]], acc=True)
                if stop == 'attn0':
                    S.barrier(); raise _Stop()
                for s, ti in enumerate(qtiles):
                    n0 = tiles[ti]["u0"] - OWN0
                    lv = lse_scr[g][:, :].rearrange("(m d) c -> d m c", d=d)
                    S.dma("sp", lv[n0 % d, n0 // d:n0 // d + Rq, :], lse_sb[0:Rq, s, :], reads=[lse_sb], writes=[lse_scr[g]], acc=True)
            S.barrier()
        if stop == 'attn':
            raise _Stop()
        with ExitStack() as eB:
            mixedT = S.sb(eB, "mixedTa", [128, 24, TOK], BF16)
            with ExitStack() as eB1:
                og = [Rot([S.sb(eB1, f"og{g}_{i}", [128, 1024], F32) for i in range(2)]) for g in range(3)]
                lg = [Rot([S.sb(eB1, f"lg{g}_{i}", [128, 16], F32) for i in range(2)]) for g in range(3)]
                mx = S.sb(eB1, "mx_m", [128, 16], F32)
                ssum = S.sb(eB1, "ssum_m", [128, 16], F32)
                mixed = Rot([S.sb(eB1, f"mixed{i}", [128, 3072], BF16) for i in range(2)])
                pstB = Rot([S.ps(eB1, f"pstB{i}", [128, 4, 128], BF16) for i in range(2)])
                for t in range(NT):
                    ogt = [og[g].next() for g in range(3)]
                    lgt = [lg[g].next() for g in range(3)]
                    for g in range(3):
                        S.dma("sp", ogt[g][:], o_scr[g][t * 128:(t + 1) * 128, :], reads=[o_scr[g]], writes=[ogt[g]])
                        S.dma("sp", lgt[g][:], lse_scr[g][t * 128:(t + 1) * 128, :], reads=[lse_scr[g]], writes=[lgt[g]])
                    S.op("dve", lambda e: e.tensor_tensor(out=mx[:], in0=lgt[0][:], in1=lgt[1][:], op=ALU.max), reads=[lgt[0], lgt[1]], writes=[mx])
                    S.op("dve", lambda e: e.tensor_tensor(out=mx[:], in0=mx[:], in1=lgt[2][:], op=ALU.max), reads=[mx, lgt[2]], writes=[mx])
                    for g in range(3):
                        S.op("dve", lambda e, g=g: e.tensor_tensor(out=lgt[g][:], in0=lgt[g][:], in1=mx[:], op=ALU.subtract), reads=[lgt[g], mx], writes=[lgt[g]])
                        S.op("act", lambda e, g=g: e.activation(out=lgt[g][:], in_=lgt[g][:], func=AF.Exp), reads=[lgt[g]], writes=[lgt[g]])
                    S.op("dve", lambda e: e.tensor_tensor(out=ssum[:], in0=lgt[0][:], in1=lgt[1][:], op=ALU.add), reads=[lgt[0], lgt[1]], writes=[ssum])
                    S.op("dve", lambda e: e.tensor_tensor(out=ssum[:], in0=ssum[:], in1=lgt[2][:], op=ALU.add), reads=[ssum, lgt[2]], writes=[ssum])
                    S.op("dve", lambda e: e.reciprocal(out=ssum[:], in_=ssum[:]), reads=[ssum], writes=[ssum])
                    mt = mixed.next()
                    for g in range(3):
                        S.op("dve", lambda e, g=g: e.tensor_tensor(out=lgt[g][:], in0=lgt[g][:], in1=ssum[:], op=ALU.mult), reads=[lgt[g], ssum], writes=[lgt[g]])
                        S.op("dve", lambda e, g=g: e.tensor_tensor(out=mt[:, g * 1024:(g + 1) * 1024].rearrange("p (s d) -> p s d", d=64),
                                                                    in0=ogt[g][:].rearrange("p (s d) -> p s d", d=64),
                                                                    in1=lgt[g][:, :].unsqueeze(2).to_broadcast([128, 16, 64]), op=ALU.mult),
                             reads=[ogt[g], lgt[g]], writes=[mt], acc=(g > 0))
                    transpose_chunks(S, C, mt, lambda c, mt=mt: mt[:, c * 128:(c + 1) * 128], 24, mixedT,
                                     lambda c0_, n, t=t: mixedT[:, c0_:c0_ + n, t * 128:(t + 1) * 128], pstB)
                S.barrier()
            wo = Rot([S.sb(eB, f"wo{i}", [128, 24, 512], BF16) for i in range(2)])
            psy = Rot([S.ps(eB, f"psyA{i}", [128, 512], F32) for i in range(2)])
            ysb = Rot([S.sb(eB, f"ysbA{i}", [128, 512], F32) for i in range(3)])
            for nch in range(D // 512):
                w = wo.next()
                S.dma("pool", w[:], w_o[:, nch * 512:(nch + 1) * 512].rearrange("(kc p) n -> p kc n", p=128), reads=[w_o], writes=[w])
                for t in range(NT):
                    ps = psy.next()
                    for kc in range(24):
                        S.op("pe", lambda e, kc=kc: e.matmul(ps[:], mixedT[:, kc, t * 128:(t + 1) * 128], w[:, kc, :], start=(kc == 0), stop=(kc == 23)),
                             reads=[mixedT, w], writes=[ps], acc=(kc > 0))
                    yb = ysb.next()
                    S.op("act", lambda e: e.activation(out=yb[:], in_=ps[:], func=AF.Copy), reads=[ps], writes=[yb])
                    S.dma("sp", y_scr[t * 128:(t + 1) * 128, nch * 512:(nch + 1) * 512], yb[:], reads=[yb], writes=[y_scr], acc=True)
            S.barrier()
        if stop == 'wo':
            raise _Stop()
        with ExitStack() as e3:
            g1 = S.sb(e3, "g1a", [128, D], F32)
            lng = S.sb(e3, "lnga", [128, D], F32)
            lnb = S.sb(e3, "lnba", [128, D], F32)
            load_bc(S, "sp", g1, vecs, (VR(2) if ctx else vecs[2:3, :]))
            plus_one(S, g1)
            load_bc(S, "sp", lng, vecs, (VR(3) if ctx else vecs[3:4, :]))
            load_bc(S, "sp", lnb, vecs, (VR(4) if ctx else vecs[4:5, :]))
            tail = Tail(S, C, e3, (VR(6) if ctx else vecs[6:7, :]), (VR(5) if ctx else vecs[5:6, :]), vecs, wr_d, br_d, x_out, h_out, idx_out, p_out, scat=(ctx.scat if ctx else None))
            ys = Rot([S.sb(e3, f"ya_{i}", [128, D], F32) for i in range(2)])
            xs_ = Rot([S.sb(e3, f"xa_{i}", [128, D], F32) for i in range(2)])
            outs = Rot([S.sb(e3, f"oa_{i}", [128, D], F32) for i in range(2)])
            st = S.sb(e3, "sta", [128, 4, 6], F32)
            mv3 = S.sb(e3, "mva", [128, 2], F32)
            rstd3 = S.sb(e3, "rstda", [128, 1], F32)
            for t in range(NT):
                y = ys.next()
                xx = xs_.next()
                o = outs.next()
                S.dma("sp", y[:], y_scr[t * 128:(t + 1) * 128, :], reads=[y_scr], writes=[y])
                S.dma("sp", xx[:], xext[OWN0 + t * 128:OWN0 + (t + 1) * 128, :], reads=[xext], writes=[xx])
                residual_ln(S, xx, y, g1, lng, lnb, o, st, mv3, rstd3)
                tail.run(t, o)
        if not ctx:
            S.finish([x_out, h_out, idx_out, p_out])
    except _Stop:
        pass
    return nc


def attn_core_inputs(x_b, pos_b, T0):
    xext = np.zeros((EXT, D), np.float32)
    pext = np.zeros((EXT,), np.int32)
    lo = T0 - OWN0
    s = max(lo, 0)
    xext[s - lo:] = x_b[s:T0 + TOK]
    pext[s - lo:] = pos_b[s:T0 + TOK]
    postab = np.zeros((128, NGT), np.int32)
    hbv = np.zeros((3, 128), np.float32)
    for g in range(3):
        d = DILS[g]
        for ti, tl in enumerate(group_tiles(g)):
            u = tl["u0"] + d * np.arange(tl["R"])
            postab[:tl["R"], GT_OFF[g] + ti] = pext[u]
        first_q = next(tl for tl in group_tiles(g) if tl["halo"])
        prev = group_tiles(g)[first_q["prev"]]
        u = prev["u0"] + d * np.arange(128)
        hbv[g] = np.where(u + lo >= 0, 0.0, NEG)
    return {"xext": xext, "postab": postab, "hb": hbv}


_NC_CACHE = {}
_DBG = None


def _get(name, fn):
    if name not in _NC_CACHE:
        _NC_CACHE[name] = fn()
    return _NC_CACHE[name]


def _run(nc, in_maps):
    res = run_bass_kernel_spmd(nc, in_maps, core_ids=list(range(NCORES)))
    return res.results


def _pk(v):
    return np.ascontiguousarray(np.asarray(v).reshape(-1, 128).T)


def moe_w_layout(w, nchunk):
    nl = w.shape[0]
    r = w.reshape(nl, 16, 128, nchunk, 256).transpose(0, 3, 2, 1, 4)
    return np.ascontiguousarray(r).reshape(nl * nchunk * 128, 16 * 256)


def _moe_launch(h_list, idx_list, p_list, w_in, b_in, w_out, b_out):
    nc = _get("moe", build_moe)
    h_all = np.concatenate(h_list, 0)
    idx_all = np.concatenate(idx_list, 0).view(np.int32)
    p_all = np.concatenate(p_list, 0)
    in_maps = []
    for i in range(NCORES):
        e0 = i * NLOC
        in_maps.append({
            "h_all": h_all, "idx_all": idx_all, "p_all": p_all,
            "eids": np.arange(e0, e0 + NLOC, dtype=np.float32).reshape(1, NLOC),
            "w_in": moe_w_layout(w_in[e0:e0 + NLOC], 16),
            "b_in": np.ascontiguousarray(b_in[e0:e0 + NLOC].reshape(NLOC, 2 * FF // 128, 128).transpose(0, 2, 1).reshape(NLOC * 128, 2 * FF // 128)),
            "w_out": moe_w_layout(w_out[e0:e0 + NLOC], 8),
            "b_out": np.ascontiguousarray(b_out[e0:e0 + NLOC]),
        })
    res = _run(nc, in_maps)
    partials = [r["partial"] for r in res]
    return [np.stack([partials[c][i * TOK:(i + 1) * TOK] for c in range(NCORES)], 0) for i in range(NCORES)]


def kernel_unfused(x, c, positions, cond_w, cond_b, ln_g, ln_b, attn_w_qkv, attn_w_o,
           sg_w_in, sg_b_in, sg_ln_g, sg_ln_b, sg_w_spatial, sg_b_spatial, sg_w_out,
           router_w, router_b, expert_w_in, expert_b_in, expert_w_out, expert_b_out):
    f32 = lambda a: np.asarray(a, dtype=np.float32)
    x, c, cond_w, cond_b, ln_g, ln_b = f32(x), f32(c), f32(cond_w), f32(cond_b), f32(ln_g), f32(ln_b)
    positions = np.asarray(positions).astype(np.int32)
    attn_w_qkv, attn_w_o = f32(attn_w_qkv), f32(attn_w_o)
    sg_w_in, sg_b_in, sg_ln_g, sg_ln_b = f32(sg_w_in), f32(sg_b_in), f32(sg_ln_g), f32(sg_ln_b)
    sg_w_spatial, sg_b_spatial, sg_w_out = f32(sg_w_spatial), f32(sg_b_spatial), f32(sg_w_out)
    router_w, router_b = f32(router_w), f32(router_b)
    expert_w_in, expert_b_in, expert_w_out, expert_b_out = f32(expert_w_in), f32(expert_b_in), f32(expert_w_out), f32(expert_b_out)
    B = x.shape[0]

    cT = np.ascontiguousarray(c.reshape(B, D // 128, 128).transpose(2, 1, 0))
    in_maps = [{"cT": cT, "cw": np.ascontiguousarray(cond_w[:, :, i * MODW:(i + 1) * MODW]),
                "cb": np.ascontiguousarray(cond_b[:, i * MODW:(i + 1) * MODW])} for i in range(NCORES)]
    res = _run(_get("cond", build_cond), in_maps)
    mod = np.concatenate([r["mod"] for r in res], axis=2).reshape(DEPTH, B, 6, D)
    zero = np.zeros((D,), np.float32)

    in_maps = []
    for i in range(NCORES):
        b, T0 = i // 4, (i % 4) * TOK
        m = attn_core_inputs(x[b], positions[b], T0)
        md = mod[0, b]
        m["vecs"] = np.stack([md[0], md[1], md[2], ln_g[0, 0], ln_b[0, 0], md[3], md[4], zero], 0)
        m.update({"w_qkv": attn_w_qkv[0], "w_o": attn_w_o[0], "wr": router_w[0], "br": router_b[0][None, :]})
        in_maps.append(m)
    res = _run(_get("attn", build_attn), in_maps)
    x1 = [r["x_out"] for r in res]
    if _DBG is not None:
        _DBG.update(mod=mod, x1=x1, h2=[r["h_out"] for r in res], idx2=[r["idx_out"] for r in res], p2=[r["p_out"] for r in res])
        if _DBG.get("stop") == 1:
            return None
    parts = _moe_launch([r["h_out"] for r in res], [r["idx_out"] for r in res], [r["p_out"] for r in res],
                        expert_w_in[0], expert_b_in[0], expert_w_out[0], expert_b_out[0])

    if _DBG is not None:
        _DBG.update(parts0=parts)
        if _DBG.get("stop") == 2:
            return None
    in_maps = []
    for i in range(NCORES):
        b = i // 4
        m0, m1 = mod[0, b], mod[1, b]
        vecs = np.stack([m0[5], ln_g[0, 1], ln_b[0, 1], m1[0], m1[1], m1[2], ln_g[1, 0], ln_b[1, 0], m1[3], m1[4]], 0)
        in_maps.append({
            "parts": parts[i], "x_in": x1[i], "vecs": vecs, "sg_w_in": sg_w_in[0],
            "bu_pk": _pk(sg_b_in[0, :SGW]), "bv": np.ascontiguousarray(sg_b_in[0, SGW:][None, :]),
            "lng_pk": _pk(sg_ln_g[0]), "lnb_pk": _pk(sg_ln_b[0]),
            "w_sp": sg_w_spatial[0], "b_sp": np.ascontiguousarray(sg_b_spatial[0].reshape(1, -1)),
            "sg_w_out": sg_w_out[0], "wr": router_w[1], "br": router_b[1][None, :],
        })
    res = _run(_get("mid", build_mid), in_maps)
    x3 = [r["x_out"] for r in res]
    if _DBG is not None:
        _DBG.update(x3=x3, h4=[r["h_out"] for r in res], idx4=[r["idx_out"] for r in res], p4=[r["p_out"] for r in res])
        if _DBG.get("stop") == 3:
            return None
    parts = _moe_launch([r["h_out"] for r in res], [r["idx_out"] for r in res], [r["p_out"] for r in res],
                        expert_w_in[1], expert_b_in[1], expert_w_out[1], expert_b_out[1])

    in_maps = []
    for i in range(NCORES):
        b = i // 4
        in_maps.append({"parts": parts[i], "x_in": x3[i], "vecs": np.stack([mod[1, b][5], ln_g[1, 1], ln_b[1, 1]], 0)})
    res = _run(_get("final", build_final), in_maps)
    out = np.concatenate([r["out"] for r in res], 0).reshape(B, SEQ, D)
    return out.astype(np.float32)


NF = 2
NCHK = SEQ // TOK
NLOCF = NEXP // NF
NBLKF = 64
MODF = 6 * D


class Ctx:
    pass


def xbarrier(S, nc, tok):
    S.barrier()
    nc.all_core_barrier()
    tp, ts, dsrc = tok
    S.op("pool", lambda e: e.memset(tp[:], 1.0), writes=[tp])
    S.dma("sp", ts[:], dsrc, writes=[ts])
    for e in ("pe", "dve", "act", "pool", "sp"):
        S._deps(e, [tp, ts], [], False)


def emit_cond_f(S, C, cT, cw, cb, modv):
    KC = D // 128
    with ExitStack() as es:
        ct = S.sb(es, "ct", [128, KC, 1], F32)
        S.dma("sp", ct[:], cT[:, :, :], writes=[ct])
        S.op("act", lambda e: e.activation(out=ct[:], in_=ct[:], func=AF.Silu), reads=[ct], writes=[ct])
        wbs = Rot([S.sb(es, f"cwb{i}", [128, KC, 512], F32) for i in range(3)])
        brs = Rot([S.sb(es, f"cbr{i}", [1, 512], F32) for i in range(2)])
        pss = Rot([S.ps(es, f"cps{i}", [1, 512], F32) for i in range(2)])
        obs = Rot([S.sb(es, f"cob{i}", [1, 512], F32) for i in range(2)])
        for l in range(DEPTH):
            for nch in range(MODF // 512):
                wbuf = wbs.next()
                S.dma("sp", wbuf[:], cw[l, :, nch * 512:(nch + 1) * 512].rearrange("(kc p) n -> p kc n", p=128), reads=[cw], writes=[wbuf])
                br = brs.next()
                S.dma("sp", br[:], cb[l:l + 1, nch * 512:(nch + 1) * 512], reads=[cb], writes=[br])
                ps = pss.next()
                for kc in range(KC):
                    S.op("pe", lambda e, kc=kc: e.matmul(ps[:], ct[:, kc, :], wbuf[:, kc, :], start=(kc == 0), stop=False), reads=[ct, wbuf], writes=[ps], acc=(kc > 0))
                S.op("pe", lambda e: e.matmul(ps[:], C["ones32"][0:1, 0:1], br[0:1, :], start=False, stop=True), reads=[C["ones32"], br], writes=[ps], acc=True)
                ob = obs.next()
                S.op("dve", lambda e: e.tensor_copy(out=ob[:], in_=ps[:]), reads=[ps], writes=[ob])
                S.dma("sp", modv[l:l + 1, nch * 512:(nch + 1) * 512], ob[:], reads=[ob], writes=[modv], acc=True)
        S.barrier()


class PartLoader:
    nparts = NF

    def __init__(self, PART, pidx, tile0, breg):
        self.PART, self.pidx, self.tile0, self.breg = PART, pidx, tile0, breg

    def __call__(self, S, c, t, dst):
        for hf in range(2):
            S.dma("pool", None, None, reads=[self.PART[hf], self.pidx], writes=[dst], acc=(hf > 0),
                  fn=lambda e, hf=hf: e.indirect_dma_start(out=dst[:, hf * (D // 2):(hf + 1) * (D // 2)], out_offset=None, in_=self.PART[hf][:, :],
                                                           in_offset=bass.IndirectOffsetOnAxis(ap=self.pidx[:, c, self.tile0 + t:self.tile0 + t + 1], axis=0),
                                                           bounds_check=self.breg, oob_is_err=False))


def build_fused():
    nc = bass.Bass("TRN2", target_bir_lowering=False, num_devices=NF)
    with ExitStack() as es:
        S = Sched(nc, es)
        I = lambda name, shape, dt: S.dram(name, shape, dt, kind="ExternalInput")
        x_pad = I("x_pad", [OWN0 + SEQ, D], F32)
        postab = I("postab", [NCHK, 128, NGT], I32)
        hb = I("hb", [NCHK, 3, 128], F32)
        cT = I("cT", [128, D // 128, 1], F32)
        cond_w = I("cond_w", [DEPTH, D, MODF], F32)
        cond_b = I("cond_b", [DEPTH, MODF], F32)
        lnp = I("lnp", [8, D], F32)
        w_qkv = I("w_qkv", [D, 9216], F32)
        w_o = I("w_o", [3072, D], F32)
        wr = I("wr", [DEPTH, D, NEXP], F32)
        br = I("br", [DEPTH, NEXP], F32)
        sg_w_in = I("sg_w_in", [D, 2 * SGW], F32)
        bu_pk = I("bu_pk", [128, SGC], F32)
        bv = I("bv", [1, SGW], F32)
        lng_pk = I("lng_pk", [128, SGC], F32)
        lnb_pk = I("lnb_pk", [128, SGC], F32)
        w_sp = I("w_sp", [16, 128, 128], F32)
        b_sp = I("b_sp", [1, 16 * 128], F32)
        sg_w_out = I("sg_w_out", [SGW, D], F32)
        e_w_in = [I(f"e_w_in{l}", [NLOCF * 16 * 128, 4096], F32) for l in range(DEPTH)]
        e_b_in = [I(f"e_b_in{l}", [NLOCF * 128, 2 * FF // 128], F32) for l in range(DEPTH)]
        e_w_out = [I(f"e_w_out{l}", [NLOCF * 8 * 128, 4096], F32) for l in range(DEPTH)]
        e_b_out = [I(f"e_b_out{l}", [NLOCF, D], F32) for l in range(DEPTH)]
        eids = I("eids", [1, NLOCF], F32)
        rowidx_d = I("rowidx", [128, NCHK * 8], I32)
        prow_d = I("prow", [128, NTOK // 128], I32)
        pidx_d = I("pidx", [128, NF, NCHK * 8], I32)
        out = S.dram("out", [SEQ, D], F32, kind="ExternalOutput")
        modv = S.dram("modv", [DEPTH, MODF], F32)
        x1_all = S.dram("x1_all", [SEQ, D], F32)
        x3_all = S.dram("x3_all", [SEQ, D], F32)
        o_scr = [S.dram(f"o_scr{g}", [TOK, 1024], F32) for g in range(3)]
        lse_scr = [S.dram(f"lse_scr{g}", [TOK, 16], F32) for g in range(3)]
        y_scr_a = S.dram("y_scr_a", [TOK, D], F32)
        x2_scr = S.dram("x2_scr", [TOK, D], F32)
        y_scr = S.dram("y_scr", [TOK, D], F32)
        xs = S.dram("xs", [NBLKF * 512, D], BF16)
        ysd = [S.dram(f"ys{i}", [NBLKF * 512 + 128, D // 2], F32) for i in range(2)]
        SH = lambda name, shape, dt: T(nc.dram_tensor(name, shape, dt, addr_space="Shared").ap(), name)
        H = SH("H_sh", [NTOK, D], BF16)
        RT = SH("RT_sh", [NTOK, 8], F32)
        PART = [SH(f"PART{i}_sh", [NF * NTOK, D // 2], F32) for i in range(2)]

        C = make_consts(S, es)
        rowidx = S.sb(es, "rowidx", [128, NCHK * 8], I32)
        prow = S.sb(es, "prow", [128, NTOK // 128], I32)
        pidx = S.sb(es, "pidx", [128, NF, NCHK * 8], I32)
        S.dma("sp", rowidx[:], rowidx_d[:, :], writes=[rowidx])
        S.dma("sp", prow[:], prow_d[:, :], writes=[prow])
        S.dma("sp", pidx[:], pidx_d[:, :, :], writes=[pidx])
        tok = (S.sb(es, "tokp", [1, 8], F32), S.sb(es, "toks", [1, 8], F32), lnp[0:1, 0:8])
        breg_h = nc.gpsimd.to_reg(NTOK - 1)
        breg_part = nc.gpsimd.to_reg(NF * NTOK - 1)
        dummy = T(None, "dummy")

        def mk(**kw):
            c = Ctx()
            c.nc, c.S, c.C = nc, S, C
            c.t = {}
            c.sb = {"prow": prow}
            c.breg_part = breg_part
            c.scat = None
            c.part_loader = None
            c.vr = {}
            for k, v in kw.items():
                setattr(c, k, v)
            return c

        mrow = lambda l, j: modv[l:l + 1, j * D:(j + 1) * D]
        lrow = lambda k: lnp[k:k + 1, :]
        Tv = lambda ap, name: T(ap, name)

        emit_cond_f(S, C, cT, cond_w, cond_b, modv)

        for ch in range(NCHK):
            ctx = mk()
            ctx.t = {"xext": Tv(x_pad[ch * TOK:ch * TOK + EXT, :], "xext"), "vecs": dummy, "postab": Tv(postab[ch], "postab"), "hb": Tv(hb[ch], "hb"),
                     "w_qkv": w_qkv, "w_o": w_o, "wr": Tv(wr[0], "wr0"), "br": Tv(br[0:1, :], "br0"),
                     "x_out": Tv(x1_all[ch * TOK:(ch + 1) * TOK, :], "x1c"), "h_out": dummy, "idx_out": dummy, "p_out": dummy,
                     "o_scr0": o_scr[0], "o_scr1": o_scr[1], "o_scr2": o_scr[2], "lse_scr0": lse_scr[0], "lse_scr1": lse_scr[1], "lse_scr2": lse_scr[2],
                     "y_scr_a": y_scr_a}
            ctx.vr = {0: mrow(0, 0), 1: mrow(0, 1), 2: mrow(0, 2), 3: lrow(0), 4: lrow(1), 5: mrow(0, 3), 6: mrow(0, 4)}
            ctx.scat = {"H": H, "RT": RT, "rowidx": rowidx, "tile0": ch * 8, "breg": breg_h}
            build_attn(None, ctx)
            S.barrier()
        xbarrier(S, nc, tok)

        def moe(l):
            ctx = mk()
            ctx.t = {"h_all": H, "RT": RT, "idx_all": dummy, "p_all": dummy, "eids": eids, "w_in": e_w_in[l], "b_in": e_b_in[l],
                     "w_out": e_w_out[l], "b_out": e_b_out[l], "partial": dummy, "xs": xs, "ys0": ysd[0], "ys1": ysd[1],
                     "PART0": PART[0], "PART1": PART[1]}
            build_moe(NTOK, NLOCF, NBLKF, ctx)
            xbarrier(S, nc, tok)

        moe(0)

        for ch in range(NCHK):
            ctx = mk()
            ctx.t = {"parts": dummy, "x_in": Tv(x1_all[ch * TOK:(ch + 1) * TOK, :], "x1c"), "vecs": dummy, "sg_w_in": sg_w_in, "bu_pk": bu_pk, "bv": bv,
                     "lng_pk": lng_pk, "lnb_pk": lnb_pk, "w_sp": w_sp, "b_sp": b_sp, "sg_w_out": sg_w_out, "wr": Tv(wr[1], "wr1"), "br": Tv(br[1:2, :], "br1"),
                     "x_out": Tv(x3_all[ch * TOK:(ch + 1) * TOK, :], "x3c"), "h_out": dummy, "idx_out": dummy, "p_out": dummy, "x2_scr": x2_scr, "y_scr": y_scr}
            ctx.vr = {0: mrow(0, 5), 1: lrow(2), 2: lrow(3), 3: mrow(1, 0), 4: mrow(1, 1), 5: mrow(1, 2), 6: lrow(4), 7: lrow(5), 8: mrow(1, 3), 9: mrow(1, 4)}
            ctx.scat = {"H": H, "RT": RT, "rowidx": rowidx, "tile0": ch * 8, "breg": breg_h}
            ctx.part_loader = PartLoader(PART, pidx, ch * 8, breg_part)
            build_mid(ctx)
            S.barrier()
        xbarrier(S, nc, tok)

        moe(1)

        for ch in range(NCHK):
            ctx = mk()
            ctx.t = {"parts": dummy, "x_in": Tv(x3_all[ch * TOK:(ch + 1) * TOK, :], "x3c"), "vecs": dummy, "out": Tv(out[ch * TOK:(ch + 1) * TOK, :], "outc")}
            ctx.vr = {0: mrow(1, 5), 1: lrow(6), 2: lrow(7)}
            ctx.part_loader = PartLoader(PART, pidx, ch * 8, breg_part)
            build_final(ctx)
            S.barrier()
        S.barrier()
    return nc


def kernel(x, c, positions, cond_w, cond_b, ln_g, ln_b, attn_w_qkv, attn_w_o,
           sg_w_in, sg_b_in, sg_ln_g, sg_ln_b, sg_w_spatial, sg_b_spatial, sg_w_out,
           router_w, router_b, expert_w_in, expert_b_in, expert_w_out, expert_b_out):
    f32 = lambda a: np.asarray(a, dtype=np.float32)
    x, c, cond_w, cond_b, ln_g, ln_b = f32(x), f32(c), f32(cond_w), f32(cond_b), f32(ln_g), f32(ln_b)
    positions = np.asarray(positions).astype(np.int32)
    attn_w_qkv, attn_w_o = f32(attn_w_qkv), f32(attn_w_o)
    sg_w_in, sg_b_in, sg_ln_g, sg_ln_b = f32(sg_w_in), f32(sg_b_in), f32(sg_ln_g), f32(sg_ln_b)
    sg_w_spatial, sg_b_spatial, sg_w_out = f32(sg_w_spatial), f32(sg_b_spatial), f32(sg_w_out)
    router_w, router_b = f32(router_w), f32(router_b)
    expert_w_in, expert_b_in, expert_w_out, expert_b_out = f32(expert_w_in), f32(expert_b_in), f32(expert_w_out), f32(expert_b_out)
    lnp = np.ascontiguousarray(np.stack([ln_g[0, 0], ln_b[0, 0], ln_g[0, 1], ln_b[0, 1], ln_g[1, 0], ln_b[1, 0], ln_g[1, 1], ln_b[1, 1]], 0))
    p128 = np.arange(128, dtype=np.int32)[:, None]
    in_maps = []
    for ci in range(NF):
        b = ci
        x_pad = np.zeros((OWN0 + SEQ, D), np.float32)
        x_pad[OWN0:] = x[b]
        pt, hbv = [], []
        for ch in range(NCHK):
            m = attn_core_inputs(x[b], positions[b], ch * TOK)
            pt.append(m["postab"])
            hbv.append(m["hb"])
        e0 = ci * NLOCF
        in_maps.append({
            "x_pad": x_pad, "postab": np.stack(pt, 0), "hb": np.stack(hbv, 0),
            "cT": np.ascontiguousarray(c[b].reshape(D // 128, 128).T[:, :, None]),
            "cond_w": cond_w, "cond_b": cond_b, "lnp": lnp, "w_qkv": attn_w_qkv[0], "w_o": attn_w_o[0],
            "wr": router_w, "br": router_b,
            "sg_w_in": sg_w_in[0], "bu_pk": _pk(sg_b_in[0, :SGW]), "bv": np.ascontiguousarray(sg_b_in[0, SGW:][None, :]),
            "lng_pk": _pk(sg_ln_g[0]), "lnb_pk": _pk(sg_ln_b[0]), "w_sp": sg_w_spatial[0],
            "b_sp": np.ascontiguousarray(sg_b_spatial[0].reshape(1, -1)), "sg_w_out": sg_w_out[0],
            **{f"e_w_in{l}": moe_w_layout(expert_w_in[l, e0:e0 + NLOCF], 16) for l in range(DEPTH)},
            **{f"e_b_in{l}": np.ascontiguousarray(expert_b_in[l, e0:e0 + NLOCF].reshape(NLOCF, 2 * FF // 128, 128).transpose(0, 2, 1).reshape(NLOCF * 128, 2 * FF // 128)) for l in range(DEPTH)},
            **{f"e_w_out{l}": moe_w_layout(expert_w_out[l, e0:e0 + NLOCF], 8) for l in range(DEPTH)},
            **{f"e_b_out{l}": np.ascontiguousarray(expert_b_out[l, e0:e0 + NLOCF]) for l in range(DEPTH)},
            "eids": np.arange(e0, e0 + NLOCF, dtype=np.float32).reshape(1, NLOCF),
            "rowidx": (ci * SEQ + np.arange(NCHK * 8, dtype=np.int32)[None, :] * 128 + p128).astype(np.int32),
            "prow": (ci * NTOK + np.arange(NTOK // 128, dtype=np.int32)[None, :] * 128 + p128).astype(np.int32),
            "pidx": np.stack([(cc * NTOK + ci * SEQ + np.arange(NCHK * 8, dtype=np.int32)[None, :] * 128 + p128) for cc in range(NF)], 1).astype(np.int32),
        })
    nc = _get("fused", build_fused)
    res = run_bass_kernel_spmd(nc, in_maps, core_ids=list(range(NF)))
    out = np.stack([r["out"] for r in res.results], 0)
    return out.astype(np.float32)
```

```python
import numpy as np
from contextlib import ExitStack
import concourse.bass as bass
import concourse.mybir as mybir
from concourse.bass_utils import run_bass_kernel_spmd

F32 = mybir.dt.float32
BF16 = mybir.dt.bfloat16
I32 = mybir.dt.int32
U32 = mybir.dt.uint32
AF = mybir.ActivationFunctionType
ALU = mybir.AluOpType
AX = mybir.AxisListType

D = 2048
NCORES = 8
TOK = 1024
NTOK = 8192
SEQ = 4096
DEPTH = 2
ALPHA = float((2 * DEPTH) ** 0.25)
LN_EPS = 1e-5
NEXP = 32
NLOC = 4
NBLK = 20
FF = 2048
BIGIDX = 1.0e6


class T:
    def __init__(self, t, name=""):
        self.t = t
        self.name = name
        self.w = {}
        self.r = {}

    def __getitem__(self, idx):
        return self.t[idx]


class Sched:
    NDMA = 20

    def __init__(self, nc, es):
        self.nc = nc
        self.es = es
        self.eng = {"pe": nc.tensor, "dve": nc.vector, "act": nc.scalar, "pool": nc.gpsimd, "sp": nc.sync}
        self.sem = {}
        self.cnt = {}
        self.known = {}
        for k in self.eng:
            self.sem[k] = es.enter_context(nc.semaphore("sem_" + k))
            self.cnt[k] = 0
            self.known[k] = {}
        self.dsem = {}
        self.dcnt = {}
        self.dptr = {}
        for q in ("sp", "pool", "act"):
            self.dsem[q] = [es.enter_context(nc.semaphore(f"dma_{q}_{i}")) for i in range(self.NDMA)]
            self.dcnt[q] = [0] * self.NDMA
            self.dptr[q] = 0
        self.ntensors = 0

    def sb(self, es, name, shape, dt):
        self.ntensors += 1
        return T(es.enter_context(self.nc.sbuf_tensor(f"{name}_{self.ntensors}", list(shape), dt)), name)

    def ps(self, es, name, shape, dt=F32):
        self.ntensors += 1
        return T(es.enter_context(self.nc.psum_tensor(f"{name}_{self.ntensors}", list(shape), dt)), name)

    def dram(self, name, shape, dt, kind="Internal"):
        h = self.nc.dram_tensor(name, list(shape), dt, kind=kind)
        return T(h.ap(), name)

    def _wait(self, e, sem, val):
        k = self.known[e]
        key = id(sem)
        if k.get(key, 0) >= val:
            return
        self.eng[e].wait_ge(sem, val)
        k[key] = val

    def _deps(self, e, reads, writes, acc):
        own = id(self.sem[e]) if e == "pe" else None
        for b in list(reads) + list(writes):
            if acc and any(b is x for x in writes):
                continue
            for key, (sem, val) in b.w.items():
                if key == own:
                    continue
                self._wait(e, sem, val)
        for b in writes:
            for key, (sem, val) in b.r.items():
                if key == own:
                    continue
                self._wait(e, sem, val)

    def _update(self, sem, val, reads, writes, acc):
        key = id(sem)
        for b in writes:
            if not acc:
                b.w = {}
                b.r = {}
            b.w[key] = (sem, val)
        for b in reads:
            if any(b is x for x in writes):
                continue
            b.r[key] = (sem, val)

    def op(self, e, fn, reads=(), writes=(), acc=False):
        self._deps(e, reads, writes, acc)
        ins = fn(self.eng[e])
        self.cnt[e] += 1
        ins.then_inc(self.sem[e], 1)
        self._update(self.sem[e], self.cnt[e], reads, writes, acc)
        return ins

    def dma(self, q, out, in_, reads=(), writes=(), acc=False, fn=None):
        i = self.dptr[q] % self.NDMA
        self.dptr[q] += 1
        sem = self.dsem[q][i]
        if self.dcnt[q][i] > 0:
            self._wait(q, sem, 16 * self.dcnt[q][i])
        self._deps(q, reads, writes, acc)
        if fn is None:
            ins = self.eng[q].dma_start(out=out, in_=in_)
        else:
            ins = fn(self.eng[q])
        ins.then_inc(sem, 16)
        self.dcnt[q][i] += 1
        self._update(sem, 16 * self.dcnt[q][i], reads, writes, acc)
        return ins

    def barrier(self):
        evs = []
        for k in self.eng:
            if self.cnt[k] > 0:
                evs.append((self.sem[k], self.cnt[k]))
        for q in self.dsem:
            for i in range(self.NDMA):
                if self.dcnt[q][i] > 0:
                    evs.append((self.dsem[q][i], 16 * self.dcnt[q][i]))
        for e in self.eng:
            for sem, val in evs:
                if e == "pe" and sem is self.sem["pe"]:
                    continue
                self._wait(e, sem, val)

    def finish(self, outs):
        for b in outs:
            for key, (sem, val) in b.w.items():
                self._wait("sp", sem, val)


class Rot:
    def __init__(self, items):
        self.items = items
        self.i = 0

    def next(self):
        x = self.items[self.i % len(self.items)]
        self.i += 1
        return x


def new_nc():
    return bass.Bass("TRN2", target_bir_lowering=False)


def make_consts(S, es):
    C = {}
    io_f_i = S.sb(es, "io_f_i", [128, 128], I32)
    io_p_i = S.sb(es, "io_p_i", [128, 1], I32)
    C["io_f"] = S.sb(es, "io_f", [128, 128], F32)
    C["io_p"] = S.sb(es, "io_p", [128, 1], F32)
    S.op("pool", lambda e: e.iota(io_f_i[:], pattern=[[1, 128]], base=0, channel_multiplier=0), writes=[io_f_i])
    S.op("pool", lambda e: e.iota(io_p_i[:], pattern=[[0, 1]], base=0, channel_multiplier=1), writes=[io_p_i])
    S.op("dve", lambda e: e.tensor_copy(out=C["io_f"][:], in_=io_f_i[:]), reads=[io_f_i], writes=[C["io_f"]])
    S.op("dve", lambda e: e.tensor_copy(out=C["io_p"][:], in_=io_p_i[:]), reads=[io_p_i], writes=[C["io_p"]])
    C["ident32"] = S.sb(es, "ident32", [128, 128], F32)
    C["ident16"] = S.sb(es, "ident16", [128, 128], BF16)
    S.op("dve", lambda e: e.tensor_scalar(out=C["ident32"][:], in0=C["io_f"][:], scalar1=C["io_p"][:, 0:1], scalar2=None, op0=ALU.is_equal),
         reads=[C["io_f"], C["io_p"]], writes=[C["ident32"]])
    S.op("dve", lambda e: e.tensor_copy(out=C["ident16"][:], in_=C["ident32"][:]), reads=[C["ident32"]], writes=[C["ident16"]])
    C["ones16"] = S.sb(es, "ones16", [128, 128], BF16)
    S.op("dve", lambda e: e.memset(C["ones16"][:], 1.0), writes=[C["ones16"]])
    C["ones32"] = S.sb(es, "ones32", [128, 128], F32)
    S.op("dve", lambda e: e.memset(C["ones32"][:], 1.0), writes=[C["ones32"]])
    return C


def transpose_chunks(S, C, src, src_ap_fn, nchunks, dst, dst_ap_fn, psrot, R=128, dt16=True, evac="act", NP=128):
    ident = C["ident16"] if dt16 else C["ident32"]
    for c0 in range(0, nchunks, 4):
        n = min(4, nchunks - c0)
        ps = psrot.next()
        for i in range(n):
            S.op("pe", lambda e, i=i: e.transpose(ps[0:NP, i, 0:R], src_ap_fn(c0 + i), ident[0:R, 0:R]), reads=[src, ident], writes=[ps], acc=(i > 0))
        if evac == "act":
            S.op("act", lambda e: e.activation(out=dst_ap_fn(c0, n), in_=ps[0:NP, 0:n, 0:R], func=AF.Copy), reads=[ps], writes=[dst], acc=True)
        else:
            S.op("dve", lambda e: e.tensor_copy(out=dst_ap_fn(c0, n), in_=ps[0:NP, 0:n, 0:R]), reads=[ps], writes=[dst], acc=True)


def build_moe(ntok=NTOK, nloc=NLOC, nblk=NBLK, ctx=None):
    NT = ntok // 128
    KC = D // 128
    FC = FF // 128
    BR = 512
    POOL = nblk * BR
    ZROW = POOL
    nc = ctx.nc if ctx else new_nc()
    with ExitStack() as es:
        S = ctx.S if ctx else Sched(nc, es)
        DR = (lambda name, shape, dt, kind="Internal": ctx.t[name]) if ctx else S.dram
        VR = (lambda k: ctx.vr[k]) if ctx else None
        h_all = DR("h_all", [ntok, D], BF16, kind="ExternalInput")
        idx_all = DR("idx_all", [ntok, 4], I32, kind="ExternalInput")
        p_all = DR("p_all", [ntok, 4], F32, kind="ExternalInput")
        eids = DR("eids", [1, nloc], F32, kind="ExternalInput")
        w_in = DR("w_in", [nloc * 16 * 128, 4096], F32, kind="ExternalInput")
        b_in = DR("b_in", [nloc * 128, 2 * FC], F32, kind="ExternalInput")
        w_out = DR("w_out", [nloc * 8 * 128, 4096], F32, kind="ExternalInput")
        b_out = DR("b_out", [nloc, D], F32, kind="ExternalInput")
        partial = DR("partial", [ntok, D], F32, kind="ExternalOutput")
        xs = DR("xs", [POOL, D], BF16)
        ysh = [DR(f"ys{i}", [POOL + 128, D // 2], F32) for i in range(2)]

        C = ctx.C if ctx else make_consts(S, es)
        breg_sc = nc.gpsimd.to_reg(POOL - 1)
        breg_ga = nc.gpsimd.to_reg(POOL + 127)
        dest_sc = S.sb(es, "dest_sc", [128, NT, 4], I32)
        dest_ga = S.sb(es, "dest_ga", [128, NT, 4], I32)
        G = S.sb(es, "G", [128, NT, 4], F32)
        widx_in = S.sb(es, "widx_in", [128, nblk, 16], I32)
        widx_out = S.sb(es, "widx_out", [128, nblk, 8], I32)
        bidx_in = S.sb(es, "bidx_in", [128, nblk], I32)
        bidx_out = S.sb(es, "bidx_out", [128, nblk], I32)
        breg_w = nc.gpsimd.to_reg(nloc * 16 * 128 - 1)

        with ExitStack() as e1:
            idx_i = S.sb(e1, "idx_i", [128, NT, 4], I32)
            idx_f = S.sb(e1, "idx_f", [128, NT, 4], F32)
            p_sb = S.sb(e1, "p_sb", [128, NT, 4], F32)
            eid_bc = S.sb(e1, "eid_bc", [128, nloc], F32)
            eq = S.sb(e1, "eq", [128, NT, 4], F32)
            M = S.sb(e1, "M", [128, NT, nloc], F32)
            M16 = S.sb(e1, "M16", [128, NT, nloc], BF16)
            Ls = S.sb(e1, "Ls", [128, 128], BF16)
            within = S.sb(e1, "within", [128, NT, nloc], F32)
            colsum = S.sb(e1, "colsum", [128, NT, nloc], F32)
            off = S.sb(e1, "off", [128, NT, nloc], F32)
            dest = S.sb(e1, "dest", [128, NT, nloc], F32)
            tmp = S.sb(e1, "tmp", [128, NT, nloc], F32)
            cnt = S.sb(e1, "cnt", [128, nloc], F32)
            nb = S.sb(e1, "nb", [128, nloc], F32)
            nbi = S.sb(e1, "nbi", [128, nloc], I32)
            bstart = S.sb(e1, "bstart", [128, nloc], F32)
            bend = S.sb(e1, "bend", [128, nloc], F32)
            base = S.sb(e1, "base", [128, nloc], F32)
            eblk_f = S.sb(e1, "eblk_f", [128, nblk], F32)
            etmp = S.sb(e1, "etmp", [128, nblk], F32)
            NPC = (NT * nloc + 511) // 512
            psA = S.ps(e1, "psA", [128, NPC, 512], F32)
            psB = S.ps(e1, "psB", [128, NPC, 512], F32)
            zero16 = S.sb(e1, "zero16", [128, D], BF16)
            zero32 = S.sb(e1, "zero32", [128, D // 2], F32)
            S.op("dve", lambda e: e.memset(zero16[:], 0.0), writes=[zero16])
            S.op("dve", lambda e: e.memset(zero32[:], 0.0), writes=[zero32])
            for r in range(POOL // 128):
                S.dma("sp", xs[r * 128:(r + 1) * 128, :], zero16[:], reads=[zero16], writes=[xs], acc=True)
            for i in range(2):
                S.dma("sp", ysh[i][ZROW:ZROW + 128, :], zero32[:], reads=[zero32], writes=[ysh[i]], acc=True)
            if ctx:
                rt_sb = S.sb(e1, "rt_sb", [128, NT, 8], F32)
                S.dma("sp", rt_sb[:], ctx.t["RT"][:, :].rearrange("(t p) k -> p t k", p=128), reads=[ctx.t["RT"]], writes=[rt_sb])
                S.op("dve", lambda e: e.tensor_copy(out=p_sb[:], in_=rt_sb[:, :, 4:8]), reads=[rt_sb], writes=[p_sb])
            else:
                S.dma("sp", idx_i[:], idx_all[:, :].rearrange("(t p) k -> p t k", p=128), writes=[idx_i])
                S.dma("sp", p_sb[:], p_all[:, :].rearrange("(t p) k -> p t k", p=128), writes=[p_sb])
            S.dma("sp", eid_bc[:], eids[0:1, :].partition_broadcast(128), writes=[eid_bc])
            if ctx:
                S.op("dve", lambda e: e.tensor_copy(out=idx_f[:], in_=rt_sb[:, :, 0:4]), reads=[rt_sb], writes=[idx_f])
            else:
                S.op("dve", lambda e: e.tensor_copy(out=idx_f[:], in_=idx_i[:]), reads=[idx_i], writes=[idx_f])
            S.op("dve", lambda e: e.tensor_scalar(out=Ls[:], in0=C["io_f"][:], scalar1=C["io_p"][:, 0:1], scalar2=None, op0=ALU.is_gt),
                 reads=[C["io_f"], C["io_p"]], writes=[Ls])
            for j in range(nloc):
                S.op("dve", lambda e, j=j: e.tensor_scalar(out=eq[:], in0=idx_f[:], scalar1=eid_bc[:, j:j + 1], scalar2=None, op0=ALU.is_equal),
                     reads=[idx_f, eid_bc], writes=[eq])
                S.op("dve", lambda e, j=j: e.tensor_reduce(out=M[:, :, j], in_=eq[:], axis=AX.X, op=ALU.add), reads=[eq], writes=[M], acc=True)
            S.op("dve", lambda e: e.tensor_copy(out=M16[:], in_=M[:]), reads=[M], writes=[M16])
            Mf = M16[:].rearrange("p t j -> p (t j)")
            W_ = NT * nloc
            for pc in range(NPC):
                a0, a1 = pc * 512, min(W_, (pc + 1) * 512)
                S.op("pe", lambda e: e.matmul(psA[:, pc, 0:a1 - a0], Ls[:], Mf[:, a0:a1], start=True, stop=True), reads=[Ls, M16], writes=[psA], acc=(pc > 0))
                S.op("pe", lambda e: e.matmul(psB[:, pc, 0:a1 - a0], C["ones16"][:], Mf[:, a0:a1], start=True, stop=True), reads=[C["ones16"], M16], writes=[psB], acc=(pc > 0))
                S.op("dve", lambda e: e.tensor_copy(out=within[:].rearrange("p t j -> p (t j)")[:, a0:a1], in_=psA[:, pc, 0:a1 - a0]), reads=[psA], writes=[within], acc=(pc > 0))
                S.op("dve", lambda e: e.tensor_copy(out=colsum[:].rearrange("p t j -> p (t j)")[:, a0:a1], in_=psB[:, pc, 0:a1 - a0]), reads=[psB], writes=[colsum], acc=(pc > 0))
            S.op("dve", lambda e: e.memset(off[:, 0, :], 0.0), writes=[off])
            for t in range(1, NT):
                S.op("dve", lambda e, t=t: e.tensor_tensor(out=off[:, t, :], in0=off[:, t - 1, :], in1=colsum[:, t - 1, :], op=ALU.add),
                     reads=[off, colsum], writes=[off])
            S.op("dve", lambda e: e.tensor_tensor(out=cnt[:], in0=off[:, NT - 1, :], in1=colsum[:, NT - 1, :], op=ALU.add), reads=[off, colsum], writes=[cnt])
            S.op("dve", lambda e: e.tensor_scalar(out=nb[:], in0=cnt[:], scalar1=1.0 / BR, scalar2=(BR - 1.0) / BR - 0.5 + 0.5 / BR, op0=ALU.mult, op1=ALU.add), reads=[cnt], writes=[nb])
            S.op("dve", lambda e: e.tensor_copy(out=nbi[:], in_=nb[:]), reads=[nb], writes=[nbi])
            S.op("dve", lambda e: e.tensor_copy(out=nb[:], in_=nbi[:]), reads=[nbi], writes=[nb])
            S.op("dve", lambda e: e.memset(bstart[:, 0:1], 0.0), writes=[bstart])
            for j in range(1, nloc):
                S.op("dve", lambda e, j=j: e.tensor_tensor(out=bstart[:, j:j + 1], in0=bstart[:, j - 1:j], in1=nb[:, j - 1:j], op=ALU.add), reads=[bstart, nb], writes=[bstart])
            S.op("dve", lambda e: e.tensor_tensor(out=bend[:], in0=bstart[:], in1=nb[:], op=ALU.add), reads=[bstart, nb], writes=[bend])
            S.op("dve", lambda e: e.tensor_scalar(out=base[:], in0=bstart[:], scalar1=float(BR), scalar2=None, op0=ALU.mult), reads=[bstart], writes=[base])
            S.op("dve", lambda e: e.memset(eblk_f[:], 0.0), writes=[eblk_f])
            for j in range(nloc - 1):
                S.op("dve", lambda e, j=j: e.tensor_scalar(out=etmp[:], in0=C["io_f"][:, 0:nblk], scalar1=bend[:, j:j + 1], scalar2=None, op0=ALU.is_ge), reads=[C["io_f"], bend], writes=[etmp])
                S.op("dve", lambda e: e.tensor_tensor(out=eblk_f[:], in0=eblk_f[:], in1=etmp[:], op=ALU.add), reads=[eblk_f, etmp], writes=[eblk_f])
            cpi = S.sb(e1, "cpi", [128, 16], F32)
            wtmp = S.sb(e1, "wtmp", [128, nblk, 16], F32)
            btmp = S.sb(e1, "btmp", [128, nblk], F32)
            for c in range(16):
                S.op("dve", lambda e, c=c: e.tensor_scalar(out=cpi[:, c:c + 1], in0=C["io_p"][:, 0:1], scalar1=float(c * 128), scalar2=None, op0=ALU.add), reads=[C["io_p"]], writes=[cpi], acc=(c > 0))
            for rb in range(nblk):
                S.op("dve", lambda e, rb=rb: e.scalar_tensor_tensor(out=wtmp[:, rb, :], in0=eblk_f[:, rb:rb + 1].to_broadcast([128, 16]), scalar=2048.0, in1=cpi[:], op0=ALU.mult, op1=ALU.add),
                     reads=[eblk_f, cpi], writes=[wtmp], acc=(rb > 0))
            S.op("dve", lambda e: e.tensor_copy(out=widx_in[:], in_=wtmp[:]), reads=[wtmp], writes=[widx_in])
            for rb in range(nblk):
                S.op("dve", lambda e, rb=rb: e.scalar_tensor_tensor(out=wtmp[:, rb, 0:8], in0=eblk_f[:, rb:rb + 1].to_broadcast([128, 8]), scalar=1024.0, in1=cpi[:, 0:8], op0=ALU.mult, op1=ALU.add),
                     reads=[eblk_f, cpi], writes=[wtmp], acc=(rb > 0))
            S.op("dve", lambda e: e.tensor_copy(out=widx_out[:], in_=wtmp[:, :, 0:8]), reads=[wtmp], writes=[widx_out])
            S.op("dve", lambda e: e.scalar_tensor_tensor(out=btmp[:], in0=eblk_f[:], scalar=128.0, in1=C["io_p"][:, 0:1].to_broadcast([128, nblk]), op0=ALU.mult, op1=ALU.add), reads=[eblk_f, C["io_p"]], writes=[btmp])
            S.op("dve", lambda e: e.tensor_copy(out=bidx_in[:], in_=btmp[:]), reads=[btmp], writes=[bidx_in])
            S.op("dve", lambda e: e.tensor_copy(out=bidx_out[:], in_=eblk_f[:]), reads=[eblk_f], writes=[bidx_out])
            S.op("dve", lambda e: e.tensor_tensor(out=dest[:], in0=within[:], in1=off[:], op=ALU.add), reads=[within, off], writes=[dest])
            for j in range(nloc):
                S.op("dve", lambda e, j=j: e.tensor_scalar(out=dest[:, :, j], in0=dest[:, :, j], scalar1=base[:, j:j + 1], scalar2=float(ZROW), op0=ALU.add, op1=ALU.min),
                     reads=[dest, base], writes=[dest])
            lk = S.sb(e1, "lk", [128, NT, 4], F32)
            dk = S.sb(e1, "dk", [128, NT, 4], F32)
            tk = S.sb(e1, "tk", [128, NT, 4], F32)
            S.op("dve", lambda e: e.memset(lk[:], 0.0), writes=[lk])
            S.op("dve", lambda e: e.memset(dk[:], 0.0), writes=[dk])
            for j in range(nloc):
                S.op("dve", lambda e, j=j: e.tensor_scalar(out=eq[:], in0=idx_f[:], scalar1=eid_bc[:, j:j + 1], scalar2=None, op0=ALU.is_equal),
                     reads=[idx_f, eid_bc], writes=[eq])
                S.op("dve", lambda e: e.tensor_tensor(out=lk[:], in0=lk[:], in1=eq[:], op=ALU.add), reads=[lk, eq], writes=[lk])
                S.op("dve", lambda e, j=j: e.tensor_tensor(out=tk[:], in0=eq[:], in1=dest[:, :, j:j + 1].to_broadcast([128, NT, 4]), op=ALU.mult), reads=[eq, dest], writes=[tk])
                S.op("dve", lambda e: e.tensor_tensor(out=dk[:], in0=dk[:], in1=tk[:], op=ALU.add), reads=[dk, tk], writes=[dk])
            S.op("dve", lambda e: e.tensor_tensor(out=G[:], in0=lk[:], in1=p_sb[:], op=ALU.mult), reads=[lk, p_sb], writes=[G])
            S.op("dve", lambda e: e.scalar_tensor_tensor(out=tk[:], in0=dk[:], scalar=-BIGIDX, in1=lk[:], op0=ALU.add, op1=ALU.mult), reads=[dk, lk], writes=[tk])
            S.op("dve", lambda e: e.tensor_scalar(out=tk[:], in0=tk[:], scalar1=BIGIDX, scalar2=None, op0=ALU.add), reads=[tk], writes=[tk])
            S.op("dve", lambda e: e.tensor_copy(out=dest_sc[:], in_=tk[:]), reads=[tk], writes=[dest_sc])
            S.op("dve", lambda e: e.scalar_tensor_tensor(out=tk[:], in0=dk[:], scalar=-float(ZROW), in1=lk[:], op0=ALU.add, op1=ALU.mult), reads=[dk, lk], writes=[tk])
            S.op("dve", lambda e: e.tensor_scalar(out=tk[:], in0=tk[:], scalar1=float(ZROW), scalar2=None, op0=ALU.add), reads=[tk], writes=[tk])
            S.op("dve", lambda e: e.tensor_copy(out=dest_ga[:], in_=tk[:]), reads=[tk], writes=[dest_ga])
            hbufs = Rot([S.sb(e1, f"hb{i}", [128, D], BF16) for i in range(3)])
            for t in range(NT):
                hb = hbufs.next()
                S.dma("sp", hb[:], h_all[t * 128:(t + 1) * 128, :], writes=[hb])
                for j in range(4):
                    S.dma("pool", None, None, reads=[hb, dest_sc], writes=[xs], acc=not (t == 0 and j == 0),
                          fn=lambda e, t=t, j=j, hb=hb: e.indirect_dma_start(
                              out=xs[:, :], out_offset=bass.IndirectOffsetOnAxis(ap=dest_sc[:, t, j:j + 1], axis=0),
                              in_=hb[:, :], in_offset=None, bounds_check=breg_sc, oob_is_err=False))
            S.barrier()

        with ExitStack() as e2:
            xsT = Rot([S.sb(e2, f"xsT{i}", [128, KC, BR], BF16) for i in range(2)])
            hidT = Rot([S.sb(e2, f"hidT{i}", [128, FC, BR], BF16) for i in range(2)])
            wst = Rot([S.sb(e2, f"wst{i}", [128, 16, 256], F32) for i in range(2)])
            wb = Rot([S.sb(e2, f"wb{i}", [128, 16, 256], BF16) for i in range(6)])
            xrow = Rot([S.sb(e2, f"xrow{i}", [128, D], BF16) for i in range(2)])
            bblk = Rot([S.sb(e2, f"bblk{i}", [128, 2 * FC], F32) for i in range(2)])
            bout = Rot([S.sb(e2, f"bout{i}", [128, D], F32) for i in range(1)])
            pst = Rot([S.ps(e2, f"pst{i}", [128, 4, 128], BF16) for i in range(2)])
            psg = Rot([S.ps(e2, f"psg{i}", [128, 512], F32) for i in range(2)])
            psu = Rot([S.ps(e2, f"psu{i}", [128, 512], F32) for i in range(2)])
            psy = Rot([S.ps(e2, f"psy{i}", [128, 512], F32) for i in range(2)])
            g_sb = Rot([S.sb(e2, f"g_sb{i}", [128, 512], F32) for i in range(2)])
            s_sb = Rot([S.sb(e2, f"s_sb{i}", [128, 512], F32) for i in range(2)])
            u_sb = Rot([S.sb(e2, f"u_sb{i}", [128, 512], F32) for i in range(2)])
            y_sb = Rot([S.sb(e2, f"y_sb{i}", [128, 256], F32) for i in range(3)])
            ncast = [0]

            def wchunk(src, idx_ap, idx_T):
                st = wst.next()
                S.dma("pool", None, None, reads=[src, idx_T], writes=[st],
                      fn=lambda e: e.indirect_dma_start(out=st[:].rearrange("p k n -> p (k n)"), out_offset=None, in_=src[:, :],
                                                        in_offset=bass.IndirectOffsetOnAxis(ap=idx_ap, axis=0), bounds_check=breg_w, oob_is_err=False))
                w = wb.next()
                ncast[0] += 1
                if ncast[0] % 3 == 0:
                    S.op("dve", lambda e: e.tensor_copy(out=w[:], in_=st[:]), reads=[st], writes=[w])
                else:
                    S.op("act", lambda e: e.activation(out=w[:], in_=st[:], func=AF.Copy), reads=[st], writes=[w])
                return w

            for rb in range(nblk):
                bb = bblk.next()
                S.dma("pool", None, None, reads=[b_in, bidx_in], writes=[bb],
                      fn=lambda e, bb=bb: e.indirect_dma_start(out=bb[:, :], out_offset=None, in_=b_in[:, :],
                                                               in_offset=bass.IndirectOffsetOnAxis(ap=bidx_in[:, rb:rb + 1], axis=0), bounds_check=breg_w, oob_is_err=False))
                bo = bout.next()
                S.dma("pool", None, None, reads=[b_out, bidx_out], writes=[bo],
                      fn=lambda e, bo=bo: e.indirect_dma_start(out=bo[:, :], out_offset=None, in_=b_out[:, :],
                                                               in_offset=bass.IndirectOffsetOnAxis(ap=bidx_out[:, rb:rb + 1], axis=0), bounds_check=breg_w, oob_is_err=False))
                xt = xsT.next()
                ht = hidT.next()
                for rt in range(BR // 128):
                    xr = xrow.next()
                    S.dma("sp", xr[:], xs[rb * BR + rt * 128:rb * BR + (rt + 1) * 128, :], reads=[xs], writes=[xr])
                    transpose_chunks(S, C, xr, lambda c, xr=xr: xr[:, c * 128:(c + 1) * 128], KC, xt,
                                     lambda c0, n, rt=rt: xt[:, c0:c0 + n, rt * 128:(rt + 1) * 128], pst)
                for cp in range(8):
                    wg = wchunk(w_in, widx_in[:, rb, cp:cp + 1], widx_in)
                    wu = wchunk(w_in, widx_in[:, rb, 8 + cp:8 + cp + 1], widx_in)
                    for sub in range(2):
                        fc = cp * 2 + sub
                        pg = psg.next()
                        pu = psu.next()
                        for kc in range(KC):
                            S.op("pe", lambda e, kc=kc: e.matmul(pg[:], wg[:, kc, sub * 128:(sub + 1) * 128], xt[:, kc, :], start=(kc == 0), stop=(kc == KC - 1)),
                                 reads=[wg, xt], writes=[pg], acc=(kc > 0))
                        for kc in range(KC):
                            S.op("pe", lambda e, kc=kc: e.matmul(pu[:], wu[:, kc, sub * 128:(sub + 1) * 128], xt[:, kc, :], start=(kc == 0), stop=(kc == KC - 1)),
                                 reads=[wu, xt], writes=[pu], acc=(kc > 0))
                        g = g_sb.next()
                        s_ = s_sb.next()
                        u = u_sb.next()
                        S.op("dve", lambda e: e.tensor_scalar(out=g[:], in0=pg[:], scalar1=bb[:, fc:fc + 1], scalar2=7.0, op0=ALU.add, op1=ALU.min), reads=[pg, bb], writes=[g])
                        S.op("act", lambda e: e.activation(out=s_[:], in_=g[:], func=AF.Silu, scale=1.702), reads=[g], writes=[s_])
                        S.op("dve", lambda e: e.tensor_scalar(out=u[:], in0=pu[:], scalar1=bb[:, FC + fc:FC + fc + 1], scalar2=7.0, op0=ALU.add, op1=ALU.min), reads=[pu, bb], writes=[u])
                        S.op("dve", lambda e: e.tensor_scalar(out=u[:], in0=u[:], scalar1=-7.0, scalar2=1.0, op0=ALU.max, op1=ALU.add), reads=[u], writes=[u])
                        S.op("dve", lambda e: e.scalar_tensor_tensor(out=ht[:, fc, :], in0=s_[:], scalar=1.0 / 1.702, in1=u[:], op0=ALU.mult, op1=ALU.mult),
                             reads=[s_, u], writes=[ht], acc=True)
                for nch in range(8):
                    wo = wchunk(w_out, widx_out[:, rb, nch:nch + 1], widx_out)
                    for rt in range(BR // 128):
                        py = psy.next()
                        for fc in range(FC):
                            S.op("pe", lambda e, fc=fc: e.matmul(py[:, 0:256], ht[:, fc, rt * 128:(rt + 1) * 128], wo[:, fc, :], start=(fc == 0), stop=(fc == FC - 1)),
                                 reads=[ht, wo], writes=[py], acc=(fc > 0))
                        yb = y_sb.next()
                        S.op("dve", lambda e: e.tensor_tensor(out=yb[:], in0=py[:, 0:256], in1=bo[:, nch * 256:(nch + 1) * 256], op=ALU.add), reads=[py, bo], writes=[yb])
                        r0 = rb * BR + rt * 128
                        S.dma("sp", ysh[nch // 4][r0:r0 + 128, (nch % 4) * 256:(nch % 4 + 1) * 256], yb[:], reads=[yb], writes=[ysh[nch // 4]], acc=True)
            S.barrier()

        with ExitStack() as e3:
            H = D // 2
            gts = Rot([[S.sb(e3, f"gt{i}_{j}", [128, H], F32) for j in range(4)] for i in range(2)])
            accs = Rot([S.sb(e3, f"acc{i}", [128, H], F32) for i in range(3)])
            for t in range(NT):
                for hf in range(2):
                    gt = gts.next()
                    for j in range(4):
                        S.dma("pool", None, None, reads=[ysh[hf], dest_ga], writes=[gt[j]],
                              fn=lambda e, t=t, j=j, gt=gt, hf=hf: e.indirect_dma_start(
                                  out=gt[j][:, :], out_offset=None, in_=ysh[hf][:, :],
                                  in_offset=bass.IndirectOffsetOnAxis(ap=dest_ga[:, t, j:j + 1], axis=0),
                                  bounds_check=breg_ga, oob_is_err=False))
                    acc = accs.next()
                    S.op("dve", lambda e: e.tensor_scalar(out=acc[:], in0=gt[0][:], scalar1=G[:, t, 0:1], scalar2=None, op0=ALU.mult), reads=[gt[0], G], writes=[acc])
                    for j in range(1, 4):
                        S.op("dve", lambda e, j=j: e.scalar_tensor_tensor(out=acc[:], in0=gt[j][:], scalar=G[:, t, j:j + 1], in1=acc[:], op0=ALU.mult, op1=ALU.add),
                             reads=[gt[j], G, acc], writes=[acc])
                    if ctx:
                        S.dma("pool", None, None, reads=[acc, ctx.sb["prow"]], writes=[ctx.t["PART%d" % hf]], acc=True,
                              fn=lambda e, t=t, hf=hf, acc=acc: e.indirect_dma_start(
                                  out=ctx.t["PART%d" % hf][:, :], out_offset=bass.IndirectOffsetOnAxis(ap=ctx.sb["prow"][:, t:t + 1], axis=0),
                                  in_=acc[:, :], in_offset=None, bounds_check=ctx.breg_part, oob_is_err=False))
                    else:
                        S.dma("sp", partial[t * 128:(t + 1) * 128, hf * H:(hf + 1) * H], acc[:], reads=[acc], writes=[partial], acc=True)
        if not ctx:
            S.finish([partial])
    return nc


def load_bc(S, q, dst, src, row_ap):
    S.dma(q, dst[:], row_ap.partition_broadcast(128), reads=[src], writes=[dst])


def plus_one(S, t):
    S.op("dve", lambda e: e.tensor_scalar(out=t[:], in0=t[:], scalar1=1.0, scalar2=None, op0=ALU.add), reads=[t], writes=[t])


def ln_tile(S, v, g_bc, b_bc, out, st, mv, rstd):
    for c in range(4):
        S.op("dve", lambda e, c=c: e.bn_stats(out=st[:, c, :], in_=v[:, c * 512:(c + 1) * 512]), reads=[v], writes=[st], acc=(c > 0))
    S.op("dve", lambda e: e.bn_aggr(out=mv[:], in_=st[:].rearrange("p c s -> p (c s)")), reads=[st], writes=[mv])
    S.op("act", lambda e: e.activation(out=rstd[:], in_=mv[:, 1:2], func=AF.Sqrt, bias=LN_EPS, scale=1.0), reads=[mv], writes=[rstd])
    S.op("dve", lambda e: e.reciprocal(out=rstd[:], in_=rstd[:]), reads=[rstd], writes=[rstd])
    S.op("dve", lambda e: e.tensor_scalar(out=v[:], in0=v[:], scalar1=mv[:, 0:1], scalar2=rstd[:, 0:1], op0=ALU.subtract, op1=ALU.mult), reads=[v, mv, rstd], writes=[v])
    S.op("dve", lambda e: e.tensor_tensor(out=v[:], in0=v[:], in1=g_bc[:], op=ALU.mult), reads=[v, g_bc], writes=[v])
    S.op("dve", lambda e: e.tensor_tensor(out=out[:], in0=v[:], in1=b_bc[:], op=ALU.add), reads=[v, b_bc], writes=[out])


def residual_ln(S, x, y, g1_bc, lng_bc, lnb_bc, out, st, mv, rstd):
    S.op("dve", lambda e: e.tensor_tensor(out=y[:], in0=y[:], in1=g1_bc[:], op=ALU.mult), reads=[y, g1_bc], writes=[y])
    S.op("dve", lambda e: e.scalar_tensor_tensor(out=y[:], in0=x[:], scalar=ALPHA, in1=y[:], op0=ALU.mult, op1=ALU.add), reads=[x, y], writes=[y])
    ln_tile(S, y, lng_bc, lnb_bc, out, st, mv, rstd)


class Tail:
    def __init__(self, S, C, es, sc_row, sh_row, vecs, wr_d, br_d, x_out, h_out, idx_out, p_out, scat=None):
        self.scat = scat
        self.S, self.C = S, C
        self.sc1 = S.sb(es, "t_sc1", [128, D], F32)
        self.sh = S.sb(es, "t_sh", [128, D], F32)
        load_bc(S, "sp", self.sc1, vecs, sc_row)
        plus_one(S, self.sc1)
        load_bc(S, "sp", self.sh, vecs, sh_row)
        self.wr = S.sb(es, "t_wr", [128, D // 128, NEXP], F32)
        S.dma("sp", self.wr[:], wr_d[:, :].rearrange("(kc p) e -> p kc e", p=128), writes=[self.wr])
        self.br = S.sb(es, "t_br", [1, NEXP], F32)
        S.dma("sp", self.br[:], br_d[0:1, :], writes=[self.br])
        self.h32 = Rot([S.sb(es, f"t_h32_{i}", [128, D], F32) for i in range(2)])
        self.h16 = Rot([S.sb(es, f"t_h16_{i}", [128, D], BF16) for i in range(2)])
        self.hT = S.sb(es, "t_hT", [128, D // 128, 128], F32)
        self.pst = Rot([S.ps(es, f"t_pst{i}", [128, 4, 128], F32) for i in range(2)])
        self.pl = S.ps(es, "t_pl", [128, NEXP], F32)
        self.lg = S.sb(es, "t_lg", [128, NEXP], F32)
        self.top = S.sb(es, "t_top", [128, 8], F32)
        self.topi = S.sb(es, "t_topi", [128, 8], U32)
        self.negm = S.sb(es, "t_negm", [128, 1], F32)
        self.ex = S.sb(es, "t_ex", [128, 4], F32)
        self.sm = S.sb(es, "t_sm", [128, 1], F32)
        self.x_out, self.h_out, self.idx_out, self.p_out = x_out, h_out, idx_out, p_out
        self.rt = Rot([S.sb(es, f"t_rt{i}", [128, 8], F32) for i in range(2)])

    def run(self, t, xn):
        S, C = self.S, self.C
        r0 = t * 128
        S.dma("sp", self.x_out[r0:r0 + 128, :], xn[:], reads=[xn], writes=[self.x_out], acc=True)
        h32 = self.h32.next()
        h16 = self.h16.next()
        S.op("dve", lambda e: e.tensor_tensor(out=h32[:], in0=xn[:], in1=self.sc1[:], op=ALU.mult), reads=[xn, self.sc1], writes=[h32])
        S.op("dve", lambda e: e.tensor_tensor(out=h32[:], in0=h32[:], in1=self.sh[:], op=ALU.add), reads=[h32, self.sh], writes=[h32])
        S.op("act", lambda e: e.activation(out=h16[:], in_=h32[:], func=AF.Copy), reads=[h32], writes=[h16])
        if self.scat is None:
            S.dma("sp", self.h_out[r0:r0 + 128, :], h16[:], reads=[h16], writes=[self.h_out], acc=True)
        else:
            sc = self.scat
            S.dma("pool", None, None, reads=[h16, sc["rowidx"]], writes=[sc["H"]], acc=True,
                  fn=lambda e: e.indirect_dma_start(out=sc["H"][:, :], out_offset=bass.IndirectOffsetOnAxis(ap=sc["rowidx"][:, sc["tile0"] + t:sc["tile0"] + t + 1], axis=0),
                                                    in_=h16[:, :], in_offset=None, bounds_check=sc["breg"], oob_is_err=False))
        transpose_chunks(S, C, h32, lambda c: h32[:, c * 128:(c + 1) * 128], D // 128, self.hT,
                         lambda c0, n: self.hT[:, c0:c0 + n, :], self.pst, dt16=False, evac="act")
        KC = D // 128
        for kc in range(KC):
            S.op("pe", lambda e, kc=kc: e.matmul(self.pl[:], self.hT[:, kc, :], self.wr[:, kc, :], start=(kc == 0), stop=False),
                 reads=[self.hT, self.wr], writes=[self.pl], acc=(kc > 0))
        S.op("pe", lambda e: e.matmul(self.pl[:], C["ones32"][0:1, :], self.br[0:1, :], start=False, stop=True), reads=[C["ones32"], self.br], writes=[self.pl], acc=True)
        S.op("dve", lambda e: e.tensor_copy(out=self.lg[:], in_=self.pl[:]), reads=[self.pl], writes=[self.lg])
        S.op("dve", lambda e: e.max(out=self.top[:], in_=self.lg[:]), reads=[self.lg], writes=[self.top])
        S.op("dve", lambda e: e.max_index(out=self.topi[:], in_max=self.top[:], in_values=self.lg[:]), reads=[self.lg, self.top], writes=[self.topi])
        S.op("dve", lambda e: e.tensor_scalar(out=self.negm[:], in0=self.top[:, 0:1], scalar1=-1.0, scalar2=None, op0=ALU.mult), reads=[self.top], writes=[self.negm])
        S.op("act", lambda e: e.activation(out=self.ex[:], in_=self.top[:, 0:4], func=AF.Exp, bias=self.negm[:, 0:1], scale=1.0, accum_out=self.sm[:]),
             reads=[self.top, self.negm], writes=[self.ex, self.sm])
        S.op("dve", lambda e: e.reciprocal(out=self.sm[:], in_=self.sm[:]), reads=[self.sm], writes=[self.sm])
        S.op("dve", lambda e: e.tensor_scalar(out=self.ex[:], in0=self.ex[:], scalar1=self.sm[:, 0:1], scalar2=None, op0=ALU.mult), reads=[self.ex, self.sm], writes=[self.ex])
        if self.scat is None:
            S.dma("sp", self.idx_out[r0:r0 + 128, :], self.topi[:, 0:4], reads=[self.topi], writes=[self.idx_out], acc=True)
            S.dma("sp", self.p_out[r0:r0 + 128, :], self.ex[:], reads=[self.ex], writes=[self.p_out], acc=True)
        else:
            sc = self.scat
            rt = self.rt.next()
            S.op("dve", lambda e: e.tensor_copy(out=rt[:, 0:4], in_=self.topi[:, 0:4]), reads=[self.topi], writes=[rt])
            S.op("dve", lambda e: e.tensor_copy(out=rt[:, 4:8], in_=self.ex[:]), reads=[self.ex], writes=[rt], acc=True)
            S.dma("pool", None, None, reads=[rt, sc["rowidx"]], writes=[sc["RT"]], acc=True,
                  fn=lambda e: e.indirect_dma_start(out=sc["RT"][:, :], out_offset=bass.IndirectOffsetOnAxis(ap=sc["rowidx"][:, sc["tile0"] + t:sc["tile0"] + t + 1], axis=0),
                                                    in_=rt[:, :], in_offset=None, bounds_check=sc["breg"], oob_is_err=False))


class PartSum:
    def __init__(self, S, es, parts, x_in, vecs, gate_row, lng_row, lnb_row, loader=None):
        self.S = S
        self.loader = loader
        self.parts, self.x_in = parts, x_in
        self.g1 = S.sb(es, "ps_g1", [128, D], F32)
        self.lng = S.sb(es, "ps_lng", [128, D], F32)
        self.lnb = S.sb(es, "ps_lnb", [128, D], F32)
        load_bc(S, "sp", self.g1, vecs, gate_row)
        plus_one(S, self.g1)
        load_bc(S, "sp", self.lng, vecs, lng_row)
        load_bc(S, "sp", self.lnb, vecs, lnb_row)
        self.pb = Rot([S.sb(es, f"ps_pb{i}", [128, D], F32) for i in range(3)])
        self.acc = Rot([S.sb(es, f"ps_acc{i}", [128, D], F32) for i in range(2)])
        self.xb = Rot([S.sb(es, f"ps_xb{i}", [128, D], F32) for i in range(2)])
        self.out = Rot([S.sb(es, f"ps_out{i}", [128, D], F32) for i in range(2)])
        self.st = S.sb(es, "ps_st", [128, 4, 6], F32)
        self.mv = S.sb(es, "ps_mv", [128, 2], F32)
        self.rstd = S.sb(es, "ps_rstd", [128, 1], F32)

    def run(self, t):
        S = self.S
        r0 = t * 128
        acc = self.acc.next()
        if self.loader is None:
            S.dma("sp", acc[:], self.parts[0, r0:r0 + 128, :], reads=[self.parts], writes=[acc])
            for c in range(1, NCORES):
                pb = self.pb.next()
                S.dma("act" if c % 2 else "sp", pb[:], self.parts[c, r0:r0 + 128, :], reads=[self.parts], writes=[pb])
                S.op("dve", lambda e: e.tensor_tensor(out=acc[:], in0=acc[:], in1=pb[:], op=ALU.add), reads=[acc, pb], writes=[acc])
        else:
            self.loader(S, 0, t, acc)
            for c in range(1, self.loader.nparts):
                pb = self.pb.next()
                self.loader(S, c, t, pb)
                S.op("dve", lambda e: e.tensor_tensor(out=acc[:], in0=acc[:], in1=pb[:], op=ALU.add), reads=[acc, pb], writes=[acc])
        xb = self.xb.next()
        S.dma("sp", xb[:], self.x_in[r0:r0 + 128, :], reads=[self.x_in], writes=[xb])
        out = self.out.next()
        residual_ln(S, xb, acc, self.g1, self.lng, self.lnb, out, self.st, self.mv, self.rstd)
        return out


def build_final(ctx=None):
    nc = ctx.nc if ctx else new_nc()
    with ExitStack() as es:
        S = ctx.S if ctx else Sched(nc, es)
        DR = (lambda name, shape, dt, kind="Internal": ctx.t[name]) if ctx else S.dram
        VR = (lambda k: ctx.vr[k]) if ctx else None
        parts = DR("parts", [NCORES, TOK, D], F32, kind="ExternalInput")
        x_in = DR("x_in", [TOK, D], F32, kind="ExternalInput")
        vecs = DR("vecs", [3, D], F32, kind="ExternalInput")
        out = DR("out", [TOK, D], F32, kind="ExternalOutput")
        P = PartSum(S, es, parts, x_in, vecs, (VR(0) if ctx else vecs[0:1, :]), (VR(1) if ctx else vecs[1:2, :]), (VR(2) if ctx else vecs[2:3, :]), loader=(ctx.part_loader if ctx else None))
        for t in range(TOK // 128):
            o = P.run(t)
            S.dma("sp", out[t * 128:(t + 1) * 128, :], o[:], reads=[o], writes=[out], acc=True)
        if not ctx:
            S.finish([out])
    return nc


MODW = 6 * D // NCORES


def build_cond():
    nc = new_nc()
    KC = D // 128
    with ExitStack() as es:
        S = Sched(nc, es)
        cT = S.dram("cT", [128, KC, 2], F32, kind="ExternalInput")
        cw = S.dram("cw", [DEPTH, D, MODW], F32, kind="ExternalInput")
        cb = S.dram("cb", [DEPTH, MODW], F32, kind="ExternalInput")
        out = S.dram("mod", [DEPTH, 2, MODW], F32, kind="ExternalOutput")
        C = make_consts(S, es)
        ct = S.sb(es, "ct", [128, KC, 2], F32)
        S.dma("sp", ct[:], cT[:, :, :], writes=[ct])
        S.op("act", lambda e: e.activation(out=ct[:], in_=ct[:], func=AF.Silu), reads=[ct], writes=[ct])
        wbs = Rot([S.sb(es, f"cwb{i}", [128, KC, 512], F32) for i in range(3)])
        brs = Rot([S.sb(es, f"cbr{i}", [1, 512], F32) for i in range(2)])
        pss = Rot([S.ps(es, f"cps{i}", [2, 512], F32) for i in range(2)])
        obs = Rot([S.sb(es, f"cob{i}", [2, 512], F32) for i in range(2)])
        for l in range(DEPTH):
            for nch in range(MODW // 512):
                wbuf = wbs.next()
                S.dma("sp" if nch % 2 == 0 else "act", wbuf[:], cw[l, :, nch * 512:(nch + 1) * 512].rearrange("(kc p) n -> p kc n", p=128), reads=[cw], writes=[wbuf])
                br = brs.next()
                S.dma("sp", br[:], cb[l:l + 1, nch * 512:(nch + 1) * 512], reads=[cb], writes=[br])
                ps = pss.next()
                for kc in range(KC):
                    S.op("pe", lambda e, kc=kc: e.matmul(ps[:], ct[:, kc, :], wbuf[:, kc, :], start=(kc == 0), stop=False), reads=[ct, wbuf], writes=[ps], acc=(kc > 0))
                S.op("pe", lambda e: e.matmul(ps[:], C["ones32"][0:1, 0:2], br[0:1, :], start=False, stop=True), reads=[C["ones32"], br], writes=[ps], acc=True)
                ob = obs.next()
                S.op("dve", lambda e: e.tensor_copy(out=ob[:], in_=ps[:]), reads=[ps], writes=[ob])
                S.dma("sp", out[l, :, nch * 512:(nch + 1) * 512], ob[:], reads=[ob], writes=[out], acc=True)
        S.finish([out])
    return nc


SGW = 4096
SGC = SGW // 128


def build_mid(ctx=None):
    nc = ctx.nc if ctx else new_nc()
    KC = D // 128
    NT = TOK // 128
    with ExitStack() as es:
        S = ctx.S if ctx else Sched(nc, es)
        DR = (lambda name, shape, dt, kind="Internal": ctx.t[name]) if ctx else S.dram
        VR = (lambda k: ctx.vr[k]) if ctx else None
        parts = DR("parts", [NCORES, TOK, D], F32, kind="ExternalInput")
        x_in = DR("x_in", [TOK, D], F32, kind="ExternalInput")
        vecs = DR("vecs", [10, D], F32, kind="ExternalInput")
        sg_w_in = DR("sg_w_in", [D, 2 * SGW], F32, kind="ExternalInput")
        bu_pk = DR("bu_pk", [128, SGC], F32, kind="ExternalInput")
        bv = DR("bv", [1, SGW], F32, kind="ExternalInput")
        lng_pk = DR("lng_pk", [128, SGC], F32, kind="ExternalInput")
        lnb_pk = DR("lnb_pk", [128, SGC], F32, kind="ExternalInput")
        w_sp = DR("w_sp", [16, 128, 128], F32, kind="ExternalInput")
        b_sp = DR("b_sp", [1, 16 * 128], F32, kind="ExternalInput")
        sg_w_out = DR("sg_w_out", [SGW, D], F32, kind="ExternalInput")
        wr_d = DR("wr", [D, NEXP], F32, kind="ExternalInput")
        br_d = DR("br", [1, NEXP], F32, kind="ExternalInput")
        x_out = DR("x_out", [TOK, D], F32, kind="ExternalOutput")
        h_out = DR("h_out", [TOK, D], BF16, kind="ExternalOutput")
        idx_out = DR("idx_out", [TOK, 4], U32, kind="ExternalOutput")
        p_out = DR("p_out", [TOK, 4], F32, kind="ExternalOutput")
        x2_scr = DR("x2_scr", [TOK, D], F32)
        y_scr = DR("y_scr", [TOK, D], F32)

        C = ctx.C if ctx else make_consts(S, es)
        hT = S.sb(es, "hT", [128, KC, TOK], BF16)
        with ExitStack() as e1:
            P = PartSum(S, e1, parts, x_in, vecs, (VR(0) if ctx else vecs[0:1, :]), (VR(1) if ctx else vecs[1:2, :]), (VR(2) if ctx else vecs[2:3, :]), loader=(ctx.part_loader if ctx else None))
            sc1 = S.sb(e1, "sc1m", [128, D], F32)
            sh = S.sb(e1, "shm", [128, D], F32)
            load_bc(S, "sp", sc1, vecs, (VR(4) if ctx else vecs[4:5, :]))
            plus_one(S, sc1)
            load_bc(S, "sp", sh, vecs, (VR(3) if ctx else vecs[3:4, :]))
            h16s = Rot([S.sb(e1, f"h16_{i}", [128, D], BF16) for i in range(2)])
            htmp = S.sb(e1, "htmp", [128, D], F32)
            pst = Rot([S.ps(e1, f"pst1_{i}", [128, 4, 128], BF16) for i in range(2)])
            for t in range(NT):
                x2 = P.run(t)
                S.dma("sp", x2_scr[t * 128:(t + 1) * 128, :], x2[:], reads=[x2], writes=[x2_scr], acc=True)
                h16 = h16s.next()
                S.op("dve", lambda e: e.tensor_tensor(out=htmp[:], in0=x2[:], in1=sc1[:], op=ALU.mult), reads=[x2, sc1], writes=[htmp])
                S.op("dve", lambda e: e.tensor_tensor(out=h16[:], in0=htmp[:], in1=sh[:], op=ALU.add), reads=[htmp, sh], writes=[h16])
                transpose_chunks(S, C, h16, lambda c, h16=h16: h16[:, c * 128:(c + 1) * 128], KC, hT,
                                 lambda c0, n, t=t: hT[:, c0:c0 + n, t * 128:(t + 1) * 128], pst)
            S.barrier()
        with ExitStack() as e2:
            WcT = S.sb(e2, "WcT", [128, 16, 128], BF16)
            addend = S.sb(e2, "addend", [128, SGC, 128], F32)
            lngp = S.sb(e2, "lngp", [128, SGC], F32)
            lnbp = S.sb(e2, "lnbp", [128, SGC], F32)
            bup = S.sb(e2, "bup", [128, SGC], F32)
            S.dma("sp", lngp[:], lng_pk[:, :], writes=[lngp])
            S.dma("sp", lnbp[:], lnb_pk[:, :], writes=[lnbp])
            S.dma("sp", bup[:], bu_pk[:, :], writes=[bup])
            pst = Rot([S.ps(e2, f"pst2_{i}", [128, 4, 128], BF16) for i in range(2)])
            psm = Rot([S.ps(e2, f"psm{i}", [128, 512], F32) for i in range(3)])
            pss = Rot([S.ps(e2, f"pss{i}", [128, 4, 128], F32) for i in range(2)])
            with ExitStack() as e2a:
                wsp32 = S.sb(e2a, "wsp32", [128, 16, 128], F32)
                wsp16 = S.sb(e2a, "wsp16", [128, 16, 128], BF16)
                tril = S.sb(e2a, "tril", [128, 128], F32)
                rsw = S.sb(e2a, "rsw", [128, 16, 128], F32)
                bsp = S.sb(e2a, "bsp", [128, 16, 128], F32)
                S.dma("sp", wsp32[:], w_sp[:, :, :].rearrange("g t s -> t g s"), writes=[wsp32])
                S.dma("sp", bsp[:].rearrange("p g t -> p (g t)"), b_sp[0:1, :].partition_broadcast(128), writes=[bsp])
                S.op("dve", lambda e: e.tensor_scalar(out=tril[:], in0=C["io_f"][:], scalar1=C["io_p"][:, 0:1], scalar2=None, op0=ALU.is_le),
                     reads=[C["io_f"], C["io_p"]], writes=[tril])
                for g in range(16):
                    S.op("dve", lambda e, g=g: e.tensor_tensor(out=wsp16[:, g, :], in0=wsp32[:, g, :], in1=tril[:], op=ALU.mult), reads=[wsp32, tril], writes=[wsp16], acc=(g > 0))
                transpose_chunks(S, C, wsp16, lambda c: wsp16[:, c, :], 16, WcT, lambda c0, n: WcT[:, c0:c0 + n, :], pst)
                for q in range(4):
                    ps = psm.next()
                    S.op("pe", lambda e, q=q: e.matmul(ps[:], C["ones16"][:], WcT[:, 4 * q:4 * q + 4, :].rearrange("p g t -> p (g t)"), start=True, stop=True),
                         reads=[C["ones16"], WcT], writes=[ps])
                    S.op("dve", lambda e, q=q: e.tensor_copy(out=rsw[:, 4 * q:4 * q + 4, :].rearrange("p g t -> p (g t)"), in_=ps[:]), reads=[ps], writes=[rsw], acc=(q > 0))
                for fc in range(SGC):
                    g = fc // 2
                    S.op("dve", lambda e, fc=fc, g=g: e.scalar_tensor_tensor(out=addend[:, fc, :], in0=rsw[:, g, :], scalar=lnbp[:, fc:fc + 1], in1=bsp[:, g, :], op0=ALU.mult, op1=ALU.add),
                         reads=[rsw, lnbp, bsp], writes=[addend], acc=(fc > 0))
                S.barrier()
            v16 = [S.sb(e2, f"v16_{i}", [128, SGW], BF16) for i in range(2)]
            mixedT = S.sb(e2, "mixedT", [128, SGC, 512], BF16)
            wb = Rot([S.sb(e2, f"wbm{i}", [128, 16, 512], BF16) for i in range(4)])
            brow = Rot([S.sb(e2, f"browm{i}", [1, 512], BF16) for i in range(2)])
            u_sb = Rot([S.sb(e2, f"u_sb{i}", [128, 512], F32) for i in range(2)])
            y_sb = Rot([S.sb(e2, f"y_sbm{i}", [128, 512], F32) for i in range(3)])
            st8 = S.sb(e2, "st8", [128, 8, 6], F32)
            mv = S.sb(e2, "mvm", [128, 2], F32)
            rstd = S.sb(e2, "rstdm", [128, 1], F32)
            for half in range(2):
                for pair in range(2):
                    for nch in range(SGW // 512):
                        w = wb.next()
                        S.dma("pool", w[:], sg_w_in[:, SGW + nch * 512:SGW + (nch + 1) * 512].rearrange("(kc p) n -> p kc n", p=128), reads=[sg_w_in], writes=[w])
                        br = brow.next()
                        S.dma("pool", br[:], bv[0:1, nch * 512:(nch + 1) * 512], reads=[bv], writes=[br])
                        for tl in range(2):
                            t = half * 4 + pair * 2 + tl
                            ps = psm.next()
                            for kc in range(KC):
                                S.op("pe", lambda e, kc=kc: e.matmul(ps[:], hT[:, kc, t * 128:(t + 1) * 128], w[:, kc, :], start=(kc == 0), stop=False),
                                     reads=[hT, w], writes=[ps], acc=(kc > 0))
                            S.op("pe", lambda e: e.matmul(ps[:], C["ones16"][0:1, :], br[0:1, :], start=False, stop=True), reads=[C["ones16"], br], writes=[ps], acc=True)
                            S.op("act", lambda e, tl=tl: e.activation(out=v16[tl][:, nch * 512:(nch + 1) * 512], in_=ps[:], func=AF.Gelu), reads=[ps], writes=[v16[tl]], acc=True)
                    for tl in range(2):
                        tloc = pair * 2 + tl
                        v = v16[tl]
                        for c in range(8):
                            S.op("dve", lambda e, c=c: e.bn_stats(out=st8[:, c, :], in_=v[:, c * 512:(c + 1) * 512]), reads=[v], writes=[st8], acc=(c > 0))
                        S.op("dve", lambda e: e.bn_aggr(out=mv[:], in_=st8[:].rearrange("p c s -> p (c s)")), reads=[st8], writes=[mv])
                        S.op("act", lambda e: e.activation(out=rstd[:], in_=mv[:, 1:2], func=AF.Sqrt, bias=LN_EPS, scale=1.0), reads=[mv], writes=[rstd])
                        S.op("dve", lambda e: e.reciprocal(out=rstd[:], in_=rstd[:]), reads=[rstd], writes=[rstd])
                        S.op("dve", lambda e: e.tensor_scalar(out=v[:], in0=v[:], scalar1=mv[:, 0:1], scalar2=rstd[:, 0:1], op0=ALU.subtract, op1=ALU.mult), reads=[v, mv, rstd], writes=[v])
                        for fc0 in range(0, SGC, 4):
                            ps = pss.next()
                            for i in range(4):
                                fc = fc0 + i
                                S.op("pe", lambda e, i=i, fc=fc: e.matmul(ps[:, i, :], v[:, fc * 128:(fc + 1) * 128], WcT[:, fc // 2, :], start=True, stop=True),
                                     reads=[v, WcT], writes=[ps], acc=(i > 0))
                            for i in range(4):
                                fc = fc0 + i
                                S.op("dve", lambda e, i=i, fc=fc: e.scalar_tensor_tensor(out=mixedT[:, fc, tloc * 128:(tloc + 1) * 128], in0=ps[:, i, :], scalar=lngp[:, fc:fc + 1], in1=addend[:, fc, :], op0=ALU.mult, op1=ALU.add),
                                     reads=[ps, lngp, addend], writes=[mixedT], acc=True)
                for nch in range(SGW // 512):
                    w = wb.next()
                    S.dma("pool", w[:], sg_w_in[:, nch * 512:(nch + 1) * 512].rearrange("(kc p) n -> p kc n", p=128), reads=[sg_w_in], writes=[w])
                    for sub in range(4):
                        fc = nch * 4 + sub
                        ps = psm.next()
                        for kc in range(KC):
                            S.op("pe", lambda e, kc=kc: e.matmul(ps[:], w[:, kc, sub * 128:(sub + 1) * 128], hT[:, kc, half * 512:(half + 1) * 512], start=(kc == 0), stop=(kc == KC - 1)),
                                 reads=[w, hT], writes=[ps], acc=(kc > 0))
                        u = u_sb.next()
                        S.op("act", lambda e: e.activation(out=u[:], in_=ps[:], func=AF.Gelu, bias=bup[:, fc:fc + 1], scale=1.0), reads=[ps, bup], writes=[u])
                        S.op("dve", lambda e: e.tensor_tensor(out=mixedT[:, fc, :], in0=u[:], in1=mixedT[:, fc, :], op=ALU.mult), reads=[u, mixedT], writes=[mixedT])
                for nch in range(D // 512):
                    wa = wb.next()
                    S.dma("pool", wa[:], sg_w_out[0:2048, nch * 512:(nch + 1) * 512].rearrange("(kc p) n -> p kc n", p=128), reads=[sg_w_out], writes=[wa])
                    wbb = wb.next()
                    S.dma("pool", wbb[:], sg_w_out[2048:4096, nch * 512:(nch + 1) * 512].rearrange("(kc p) n -> p kc n", p=128), reads=[sg_w_out], writes=[wbb])
                    for tloc in range(4):
                        ps = psm.next()
                        for fc in range(SGC):
                            ww = wa if fc < 16 else wbb
                            S.op("pe", lambda e, fc=fc, ww=ww: e.matmul(ps[:], mixedT[:, fc, tloc * 128:(tloc + 1) * 128], ww[:, fc % 16, :], start=(fc == 0), stop=(fc == SGC - 1)),
                                 reads=[mixedT, ww], writes=[ps], acc=(fc > 0))
                        yb = y_sb.next()
                        S.op("act", lambda e: e.activation(out=yb[:], in_=ps[:], func=AF.Copy), reads=[ps], writes=[yb])
                        r0 = (half * 4 + tloc) * 128
                        S.dma("sp", y_scr[r0:r0 + 128, nch * 512:(nch + 1) * 512], yb[:], reads=[yb], writes=[y_scr], acc=True)
            S.barrier()
        with ExitStack() as e3:
            g1 = S.sb(e3, "g1m", [128, D], F32)
            lng = S.sb(e3, "lng3", [128, D], F32)
            lnb = S.sb(e3, "lnb3", [128, D], F32)
            load_bc(S, "sp", g1, vecs, (VR(5) if ctx else vecs[5:6, :]))
            plus_one(S, g1)
            load_bc(S, "sp", lng, vecs, (VR(6) if ctx else vecs[6:7, :]))
            load_bc(S, "sp", lnb, vecs, (VR(7) if ctx else vecs[7:8, :]))
            tail = Tail(S, C, e3, (VR(9) if ctx else vecs[9:10, :]), (VR(8) if ctx else vecs[8:9, :]), vecs, wr_d, br_d, x_out, h_out, idx_out, p_out, scat=(ctx.scat if ctx else None))
            ys = Rot([S.sb(e3, f"y3_{i}", [128, D], F32) for i in range(2)])
            xs_ = Rot([S.sb(e3, f"x3_{i}", [128, D], F32) for i in range(2)])
            outs = Rot([S.sb(e3, f"o3_{i}", [128, D], F32) for i in range(2)])
            st = S.sb(e3, "st3", [128, 4, 6], F32)
            mv3 = S.sb(e3, "mv3", [128, 2], F32)
            rstd3 = S.sb(e3, "rstd3", [128, 1], F32)
            for t in range(NT):
                y = ys.next()
                xx = xs_.next()
                o = outs.next()
                S.dma("sp", y[:], y_scr[t * 128:(t + 1) * 128, :], reads=[y_scr], writes=[y])
                S.dma("act", xx[:], x2_scr[t * 128:(t + 1) * 128, :], reads=[x2_scr], writes=[xx])
                residual_ln(S, xx, y, g1, lng, lnb, o, st, mv3, rstd3)
                tail.run(t, o)
        if not ctx:
            S.finish([x_out, h_out, idx_out, p_out])
    return nc


EXT = 3072
OWN0 = 2048
DILS = (1, 4, 16)
NEG = -30000.0
ROPE_THETA = 500000.0


def group_tiles(g):
    tiles = []
    if g == 0:
        for j in range(9):
            tiles.append(dict(u0=1920 + 128 * j, R=128, q=(j >= 1), prev=j - 1, halo=(j == 1)))
    elif g == 1:
        for rho in range(4):
            for k in range(3):
                tiles.append(dict(u0=4 * (384 + 128 * k) + rho, R=128, q=(k >= 1), prev=rho * 3 + k - 1, halo=(k == 1)))
    else:
        for rho in range(16):
            tiles.append(dict(u0=rho, R=128, q=False, prev=None, halo=False))
        for rho in range(16):
            tiles.append(dict(u0=16 * 128 + rho, R=64, q=True, prev=rho, halo=True))
    return tiles


GT_OFF = (0, 9, 21)
NGT = 53


class _Stop(Exception):
    pass


def build_attn(stop=None, ctx=None):
    nc = ctx.nc if ctx else new_nc()
    KC = D // 128
    NT = TOK // 128
    try:
      with ExitStack() as es:
        S = ctx.S if ctx else Sched(nc, es)
        DR = (lambda name, shape, dt, kind="Internal": ctx.t[name]) if ctx else S.dram
        VR = (lambda k: ctx.vr[k]) if ctx else None
        xext = DR("xext", [EXT, D], F32, kind="ExternalInput")
        vecs = DR("vecs", [8, D], F32, kind="ExternalInput")
        postab = DR("postab", [128, NGT], I32, kind="ExternalInput")
        hb = DR("hb", [3, 128], F32, kind="ExternalInput")
        w_qkv = DR("w_qkv", [D, 9216], F32, kind="ExternalInput")
        w_o = DR("w_o", [3072, D], F32, kind="ExternalInput")
        wr_d = DR("wr", [D, NEXP], F32, kind="ExternalInput")
        br_d = DR("br", [1, NEXP], F32, kind="ExternalInput")
        x_out = DR("x_out", [TOK, D], F32, kind="ExternalOutput")
        h_out = DR("h_out", [TOK, D], BF16, kind="ExternalOutput")
        idx_out = DR("idx_out", [TOK, 4], U32, kind="ExternalOutput")
        p_out = DR("p_out", [TOK, 4], F32, kind="ExternalOutput")
        o_scr = [DR(f"o_scr{g}", [TOK, 1024], F32) for g in range(3)]
        lse_scr = [DR(f"lse_scr{g}", [TOK, 16], F32) for g in range(3)]
        y_scr = DR("y_scr_a", [TOK, D], F32)

        C = ctx.C if ctx else make_consts(S, es)
        with ExitStack() as eA:
            hT = S.sb(eA, "hTx", [128, KC, EXT], BF16)
            if stop == 'consts':
                S.barrier(); raise _Stop()
            maskP = S.sb(eA, "maskP", [128, 256], F32)
            maskH = [S.sb(eA, f"maskH{g}", [128, 256], F32) for g in range(3)]
            S.op("dve", lambda e: e.tensor_scalar(out=maskP[:, 0:128], in0=C["io_f"][:], scalar1=C["io_p"][:, 0:1], scalar2=None, op0=ALU.is_ge),
                 reads=[C["io_f"], C["io_p"]], writes=[maskP])
            S.op("dve", lambda e: e.tensor_scalar(out=maskP[:, 128:256], in0=C["io_f"][:], scalar1=C["io_p"][:, 0:1], scalar2=None, op0=ALU.is_le),
                 reads=[C["io_f"], C["io_p"]], writes=[maskP])
            S.op("dve", lambda e: e.tensor_scalar(out=maskP[:], in0=maskP[:], scalar1=-1.0, scalar2=-NEG, op0=ALU.add, op1=ALU.mult), reads=[maskP], writes=[maskP])
            for g in range(3):
                S.dma("sp", maskH[g][:, 0:128], hb[g:g + 1, :].partition_broadcast(128), writes=[maskH[g]])
                S.op("dve", lambda e, g=g: e.tensor_tensor(out=maskH[g][:, 0:128], in0=maskH[g][:, 0:128], in1=maskP[:, 0:128], op=ALU.add), reads=[maskH[g], maskP], writes=[maskH[g]])
                S.op("dve", lambda e, g=g: e.tensor_copy(out=maskH[g][:, 128:256], in_=maskP[:, 128:256]), reads=[maskP], writes=[maskH[g]], acc=True)
            if stop == 'masks':
                S.barrier(); raise _Stop()
            cos_t = S.sb(eA, "cos_t", [128, NGT, 8], F32)
            sin_t = S.sb(eA, "sin_t", [128, NGT, 8], F32)
            with ExitStack() as e0:
                pos_i = S.sb(e0, "pos_i", [128, NGT], I32)
                pos_f = S.sb(e0, "pos_f", [128, NGT], F32)
                ang = S.sb(e0, "ang", [128, NGT, 8], F32)
                kf = S.sb(e0, "kf", [128, NGT, 8], F32)
                ki = S.sb(e0, "ki", [128, NGT, 8], I32)
                rr = S.sb(e0, "rr", [128, NGT, 8], F32)
                S.dma("sp", pos_i[:], postab[:, :], writes=[pos_i])
                S.op("dve", lambda e: e.tensor_copy(out=pos_f[:], in_=pos_i[:]), reads=[pos_i], writes=[pos_f])
                for f in range(8):
                    inv = float(np.float32(ROPE_THETA) ** np.float32(-f / 8.0))
                    S.op("dve", lambda e, f=f, inv=inv: e.tensor_scalar(out=ang[:, :, f], in0=pos_f[:], scalar1=inv, scalar2=None, op0=ALU.mult), reads=[pos_f], writes=[ang], acc=(f > 0))
                if stop == 'ang':
                    S.barrier(); raise _Stop()
                TWO_PI = 2.0 * float(np.pi)
                for which, dst in ((0, sin_t), (1, cos_t)):
                    if which == 1:
                        S.op("dve", lambda e: e.tensor_scalar(out=ang[:], in0=ang[:], scalar1=float(np.pi) / 2, scalar2=None, op0=ALU.add), reads=[ang], writes=[ang])
                    S.op("dve", lambda e: e.tensor_scalar(out=kf[:], in0=ang[:], scalar1=1.0 / TWO_PI, scalar2=None, op0=ALU.mult), reads=[ang], writes=[kf])
                    S.op("dve", lambda e: e.tensor_copy(out=ki[:], in_=kf[:]), reads=[kf], writes=[ki])
                    S.op("dve", lambda e: e.tensor_copy(out=kf[:], in_=ki[:]), reads=[ki], writes=[kf])
                    S.op("dve", lambda e: e.scalar_tensor_tensor(out=rr[:], in0=kf[:], scalar=-TWO_PI, in1=ang[:], op0=ALU.mult, op1=ALU.add), reads=[kf, ang], writes=[rr])
                    S.op("dve", lambda e: e.tensor_scalar(out=rr[:], in0=rr[:], scalar1=-3.14159, scalar2=3.14159, op0=ALU.max, op1=ALU.min), reads=[rr], writes=[rr])
                    S.op("act", lambda e, dst=dst: e.activation(out=dst[:], in_=rr[:], func=AF.Sin), reads=[rr], writes=[dst])
                if stop == 'tables':
                    S.barrier(); raise _Stop()
                sc1 = S.sb(e0, "sc1a", [128, D], F32)
                sh = S.sb(e0, "sha", [128, D], F32)
                load_bc(S, "sp", sc1, vecs, (VR(1) if ctx else vecs[1:2, :]))
                plus_one(S, sc1)
                load_bc(S, "sp", sh, vecs, (VR(0) if ctx else vecs[0:1, :]))
                xts = Rot([S.sb(e0, f"xta{i}", [128, D], F32) for i in range(3)])
                h16s = Rot([S.sb(e0, f"h16a{i}", [128, D], BF16) for i in range(2)])
                pst0 = Rot([S.ps(e0, f"pst0_{i}", [128, 4, 128], BF16) for i in range(2)])
                for t in range(EXT // 128):
                    xt = xts.next()
                    S.dma("sp", xt[:], xext[t * 128:(t + 1) * 128, :], reads=[xext], writes=[xt])
                    h16 = h16s.next()
                    S.op("dve", lambda e: e.tensor_tensor(out=xt[:], in0=xt[:], in1=sc1[:], op=ALU.mult), reads=[xt, sc1], writes=[xt])
                    S.op("dve", lambda e: e.tensor_tensor(out=h16[:], in0=xt[:], in1=sh[:], op=ALU.add), reads=[xt, sh], writes=[h16])
                    transpose_chunks(S, C, h16, lambda c, h16=h16: h16[:, c * 128:(c + 1) * 128], KC, hT,
                                     lambda c0, n, t=t: hT[:, c0:c0 + n, t * 128:(t + 1) * 128], pst0)
                S.barrier()
            if stop == 'hT':
                raise _Stop()
            KQ = S.sb(eA, "KQ", [128, 32, 2, 128], BF16)
            V = S.sb(eA, "V", [128, 32, 128], BF16)
            KT = S.sb(eA, "KT", [64, 2, 32, 128], BF16)
            QT = S.sb(eA, "QT", [64, 2, 16, 128], BF16)
            lse_sb = S.sb(eA, "lse_sb", [128, 16, 16], F32)
            wq = Rot([S.sb(eA, f"wq{i}", [128, KC, 128], BF16) for i in range(2)])
            wk = Rot([S.sb(eA, f"wk{i}", [128, KC, 128], BF16) for i in range(2)])
            wv = Rot([S.sb(eA, f"wv{i}", [128, KC, 128], BF16) for i in range(2)])
            pq = Rot([S.ps(eA, f"pq{i}", [128, 4, 128], F32) for i in range(2)])
            pst = Rot([S.ps(eA, "pstA", [128, 8, 128], BF16)])
            ps_s = Rot([S.ps(eA, f"ps_s{i}", [128, 2, 256], F32) for i in range(2)])
            ptp = Rot([S.ps(eA, "ptp", [128, 8, 128], BF16)])
            ps_o = Rot([S.ps(eA, "ps_o", [128, 8, 64], F32)])
            tmp = [Rot([S.sb(eA, f"rt{k}_{i}", [128, 4, 8], F32) for i in range(2)]) for k in range(4)]
            sm = Rot([S.sb(eA, f"sm{i}", [128, 2, 256], F32) for i in range(2)])
            Pb = Rot([S.sb(eA, f"Pb{i}", [128, 2, 256], BF16) for i in range(2)])
            PT = Rot([S.sb(eA, f"PT{i}", [128, 4, 128], BF16) for i in range(2)])
            negmx = Rot([S.sb(eA, f"negmx{i}", [128, 2], F32) for i in range(2)])
            rs = Rot([S.sb(eA, f"rs{i}", [128, 2], F32) for i in range(2)])
            lnrs = Rot([S.sb(eA, f"lnrs{i}", [128, 2], F32) for i in range(2)])
            o_sb = Rot([S.sb(eA, f"o_sb{i}", [128, 2, 64], F32) for i in range(3)])
            for g in range(3):
                d = DILS[g]
                tiles = group_tiles(g)
                ntile = len(tiles)
                qtiles = [i for i, tl in enumerate(tiles) if tl["q"]]
                nq = len(qtiles)
                Rq = tiles[qtiles[0]]["R"]
                for hp in range(8):
                    c0 = g * 1024 + hp * 128
                    w_q, w_k, w_v = wq.next(), wk.next(), wv.next()
                    S.dma("pool", w_q[:], w_qkv[:, c0:c0 + 128].rearrange("(kc p) n -> p kc n", p=128), reads=[w_qkv], writes=[w_q])
                    S.dma("pool", w_k[:], w_qkv[:, 3072 + c0:3072 + c0 + 128].rearrange("(kc p) n -> p kc n", p=128), reads=[w_qkv], writes=[w_k])
                    S.dma("pool", w_v[:], w_qkv[:, 6144 + c0:6144 + c0 + 128].rearrange("(kc p) n -> p kc n", p=128), reads=[w_qkv], writes=[w_v])
                    if stop == 'wload':
                        S.barrier(); raise _Stop()
                    for ti, tl in enumerate(tiles):
                        R, u0 = tl["R"], tl["u0"]
                        ps = pq.next()
                        stop_ = u0 + d * (R - 1) + 1
                        mats = [(0, w_k), (2, w_v)] + ([(1, w_q)] if tl["q"] else [])
                        first = True
                        for slot, ww in mats:
                            for kc in range(KC):
                                S.op("pe", lambda e, kc=kc, slot=slot, ww=ww: e.matmul(ps[0:R, slot, :], hT[:, kc, u0:stop_:d], ww[:, kc, :], start=(kc == 0), stop=(kc == KC - 1)),
                                     reads=[hT, ww], writes=[ps], acc=not first)
                                first = False
                        if stop == 'mm0' and ti == 0:
                            S.barrier(); raise _Stop()
                        if stop == 'proj_mm' and ti == 1:
                            S.barrier(); raise _Stop()
                        nx = 2 if tl["q"] else 1
                        src = ps[0:R, 0:nx, :].rearrange("p x (h d) -> p (x h) d", d=64)
                        dst = KQ[0:R, ti, 0:nx, :].rearrange("p x (h d) -> p (x h) d", d=64)
                        gti = GT_OFF[g] + ti
                        cb = cos_t[0:R, gti, :].unsqueeze(1).to_broadcast([R, 2 * nx, 8])
                        sb_ = sin_t[0:R, gti, :].unsqueeze(1).to_broadcast([R, 2 * nx, 8])
                        t1, t2, t3, t4 = (tmp[k].next() for k in range(4))
                        n2 = 2 * nx
                        S.op("dve", lambda e: e.tensor_tensor(out=t1[0:R, 0:n2, :], in0=src[:, :, 0:8], in1=cb, op=ALU.mult), reads=[ps, cos_t], writes=[t1])
                        if stop == 'rot0a' and ti == 0:
                            S.barrier(); raise _Stop()
                        S.op("dve", lambda e: e.tensor_tensor(out=t2[0:R, 0:n2, :], in0=src[:, :, 8:16], in1=sb_, op=ALU.mult), reads=[ps, sin_t], writes=[t2])
                        S.op("dve", lambda e: e.tensor_tensor(out=t3[0:R, 0:n2, :], in0=src[:, :, 8:16], in1=cb, op=ALU.mult), reads=[ps, cos_t], writes=[t3])
                        S.op("dve", lambda e: e.tensor_tensor(out=t4[0:R, 0:n2, :], in0=src[:, :, 0:8], in1=sb_, op=ALU.mult), reads=[ps, sin_t], writes=[t4])
                        S.op("dve", lambda e: e.tensor_tensor(out=dst[:, :, 0:8], in0=t1[0:R, 0:n2, :], in1=t2[0:R, 0:n2, :], op=ALU.subtract), reads=[t1, t2], writes=[KQ], acc=True)
                        S.op("dve", lambda e: e.tensor_tensor(out=dst[:, :, 8:16], in0=t3[0:R, 0:n2, :], in1=t4[0:R, 0:n2, :], op=ALU.add), reads=[t3, t4], writes=[KQ], acc=True)
                        if stop == 'rot0' and ti == 0:
                            S.barrier(); raise _Stop()
                        if stop == 'proj_rot' and ti == 1:
                            S.barrier(); raise _Stop()
                        S.op("dve", lambda e: e.tensor_copy(out=dst[:, :, 16:64], in_=src[:, :, 16:64]), reads=[ps], writes=[KQ], acc=True)
                        if stop == 'cp0' and ti == 0:
                            S.barrier(); raise _Stop()
                        S.op("dve", lambda e: e.tensor_copy(out=V[0:R, ti, :], in_=ps[0:R, 2, :]), reads=[ps], writes=[V], acc=True)
                        if stop == 'cp1' and ti == 0:
                            S.barrier(); raise _Stop()
                    if stop == 'proj':
                        S.barrier(); raise _Stop()
                    for hh in range(2):
                        i0 = 0
                        while i0 < ntile:
                            R = tiles[i0]["R"]
                            i1 = i0
                            while i1 < ntile and tiles[i1]["R"] == R:
                                i1 += 1
                            transpose_chunks(S, C, KQ, lambda c, i0=i0, R=R, hh=hh: KQ[0:R, i0 + c, 0, hh * 64:(hh + 1) * 64], i1 - i0, KT,
                                             lambda c0_, n, i0=i0, R=R, hh=hh: KT[:, hh, i0 + c0_:i0 + c0_ + n, 0:R], pst, R=R, NP=64)
                            i0 = i1
                        transpose_chunks(S, C, KQ, lambda s_, hh=hh: KQ[0:Rq, qtiles[s_], 1, hh * 64:(hh + 1) * 64], nq, QT,
                                         lambda c0_, n, hh=hh: QT[:, hh, c0_:c0_ + n, 0:Rq], pst, R=Rq, NP=64)
                    if stop == 'tr':
                        S.barrier(); raise _Stop()
                    for s, ti in enumerate(qtiles):
                        tl = tiles[ti]
                        pi = tl["prev"]
                        NK = 128 + Rq
                        mask = maskH[g] if tl["halo"] else maskP
                        pss = ps_s.next()
                        for hh in range(2):
                            pb = 64 * hh
                            if stop == 's_mm1' and s == 0 and hp == 0 and g == 0 and hh == 1:
                                S.barrier(); raise _Stop()
                            S.op("pe", lambda e, hh=hh, pb=pb: e.matmul(pss[0:Rq, hh, 0:128], QT[:, hh, s, 0:Rq], KT[:, hh, pi, 0:128], start=True, stop=True),
                                 reads=[QT, KT], writes=[pss], acc=(hh > 0))
                            if stop == 's_mm0' and s == 0 and hp == 0 and g == 0:
                                S.barrier(); raise _Stop()
                            S.op("pe", lambda e, hh=hh, pb=pb: e.matmul(pss[0:Rq, hh, 128:NK], QT[:, hh, s, 0:Rq], KT[:, hh, ti, 0:Rq], start=True, stop=True),
                                 reads=[QT, KT], writes=[pss], acc=True)
                        if stop == 's_mm' and s == 0 and hp == 0 and g == 0:
                            S.barrier(); raise _Stop()
                        smt = sm.next()
                        S.op("dve", lambda e: e.scalar_tensor_tensor(out=smt[0:Rq, :, 0:NK], in0=pss[0:Rq, :, 0:NK], scalar=0.125,
                                                                      in1=mask[0:Rq, 0:NK].unsqueeze(1).to_broadcast([Rq, 2, NK]), op0=ALU.mult, op1=ALU.add),
                             reads=[pss, mask], writes=[smt])
                        if stop == 's_sm' and s == 0 and hp == 0 and g == 0:
                            S.barrier(); raise _Stop()
                        nm = negmx.next()
                        S.op("dve", lambda e: e.tensor_reduce(out=nm[0:Rq, :], in_=smt[0:Rq, :, 0:NK], axis=AX.X, op=ALU.max, negate=True), reads=[smt], writes=[nm])
                        if stop == 's_max' and s == 0 and hp == 0 and g == 0:
                            S.barrier(); raise _Stop()
                        pbt = Pb.next()
                        rst = rs.next()
                        for hh in range(2):
                            S.op("act", lambda e, hh=hh: e.activation(out=pbt[0:Rq, hh, 0:NK], in_=smt[0:Rq, hh, 0:NK], func=AF.Exp, bias=nm[0:Rq, hh:hh + 1], scale=1.0, accum_out=rst[0:Rq, hh:hh + 1]),
                                 reads=[smt, nm], writes=[pbt, rst], acc=(hh > 0))
                        if stop == 's_exp' and s == 0 and hp == 0 and g == 0:
                            S.barrier(); raise _Stop()
                        ptt = ptp.next()
                        first = True
                        for hh in range(2):
                            S.op("pe", lambda e, hh=hh: e.transpose(ptt[0:128, 2 * hh, 0:Rq], pbt[0:Rq, hh, 0:128], C["ident16"][0:Rq, 0:Rq]), reads=[pbt, C["ident16"]], writes=[ptt], acc=not first)
                            first = False
                            S.op("pe", lambda e, hh=hh: e.transpose(ptt[0:Rq, 2 * hh + 1, 0:Rq], pbt[0:Rq, hh, 128:NK], C["ident16"][0:Rq, 0:Rq]), reads=[pbt, C["ident16"]], writes=[ptt], acc=True)
                        if stop == 's_pt' and s == 0 and hp == 0 and g == 0:
                            S.barrier(); raise _Stop()
                        PTt = PT.next()
                        S.op("act", lambda e: e.activation(out=PTt[:, :, 0:Rq], in_=ptt[:, 0:4, 0:Rq], func=AF.Copy), reads=[ptt], writes=[PTt])
                        if stop == 's_ptc' and s == 0 and hp == 0 and g == 0:
                            S.barrier(); raise _Stop()
                        pso = ps_o.next()
                        for hh in range(2):
                            S.op("pe", lambda e, hh=hh: e.matmul(pso[0:Rq, hh, :], PTt[0:128, 2 * hh, 0:Rq], V[0:128, pi, hh * 64:(hh + 1) * 64], start=True, stop=False),
                                 reads=[PTt, V], writes=[pso], acc=(hh > 0))
                            S.op("pe", lambda e, hh=hh: e.matmul(pso[0:Rq, hh, :], PTt[0:Rq, 2 * hh + 1, 0:Rq], V[0:Rq, ti, hh * 64:(hh + 1) * 64], start=False, stop=True),
                                 reads=[PTt, V], writes=[pso], acc=True)
                        if stop == 's_o' and s == 0 and hp == 0 and g == 0:
                            S.barrier(); raise _Stop()
                        lr = lnrs.next()
                        S.op("act", lambda e: e.activation(out=lr[0:Rq, :], in_=rst[0:Rq, :], func=AF.Ln), reads=[rst], writes=[lr])
                        S.op("dve", lambda e: e.tensor_tensor(out=lse_sb[0:Rq, s, 2 * hp:2 * hp + 2], in0=lr[0:Rq, :], in1=nm[0:Rq, :], op=ALU.subtract), reads=[lr, nm], writes=[lse_sb], acc=True)
                        S.op("dve", lambda e: e.reciprocal(out=rst[0:Rq, :], in_=rst[0:Rq, :]), reads=[rst, lr], writes=[rst])
                        if stop == 's_ln' and s == 0 and hp == 0 and g == 0:
                            S.barrier(); raise _Stop()
                        ob = o_sb.next()
                        S.op("dve", lambda e: e.tensor_tensor(out=ob[0:Rq, :, :], in0=pso[0:Rq, 0:2, :], in1=rst[0:Rq, :].unsqueeze(2).to_broadcast([Rq, 2, 64]), op=ALU.mult), reads=[pso, rst], writes=[ob])
                        if stop == 's_ob' and s == 0 and hp == 0 and g == 0:
                            S.barrier(); raise _Stop()
                        n0 = tl["u0"] - OWN0
                        ov = o_scr[g][:, :].rearrange("(m d) c -> d m c", d=d)
                        S.dma("sp", ov[n0 % d, n0 // d:n0 // d + Rq, hp * 128:(hp + 1) * 128], ob[0:Rq, :, :].rearrange("p h d -> p (h d)"), reads=[ob], writes=[o_scr[g]], acc=True)
                if stop == 'attn0':
                    S.barrier(); raise _Stop()
                for s, ti in enumerate(qtiles):
                    n0 = tiles[ti]["u0"] - OWN0
                    lv = lse_scr[g][:, :].rearrange("(m d) c -> d m c", d=d)
                    S.dma("sp", lv[n0 % d, n0 // d:n0 // d + Rq, :], lse_sb[0:Rq, s, :], reads=[lse_sb], writes=[lse_scr[g]], acc=True)
            S.barrier()
        if stop == 'attn':
            raise _Stop()
        with ExitStack() as eB:
            mixedT = S.sb(eB, "mixedTa", [128, 24, TOK], BF16)
            with ExitStack() as eB1:
                og = [Rot([S.sb(eB1, f"og{g}_{i}", [128, 1024], F32) for i in range(2)]) for g in range(3)]
                lg = [Rot([S.sb(eB1, f"lg{g}_{i}", [128, 16], F32) for i in range(2)]) for g in range(3)]
                mx = S.sb(eB1, "mx_m", [128, 16], F32)
                ssum = S.sb(eB1, "ssum_m", [128, 16], F32)
                mixed = Rot([S.sb(eB1, f"mixed{i}", [128, 3072], BF16) for i in range(2)])
                pstB = Rot([S.ps(eB1, f"pstB{i}", [128, 4, 128], BF16) for i in range(2)])
                for t in range(NT):
                    ogt = [og[g].next() for g in range(3)]
                    lgt = [lg[g].next() for g in range(3)]
                    for g in range(3):
                        S.dma("sp", ogt[g][:], o_scr[g][t * 128:(t + 1) * 128, :], reads=[o_scr[g]], writes=[ogt[g]])
                        S.dma("sp", lgt[g][:], lse_scr[g][t * 128:(t + 1) * 128, :], reads=[lse_scr[g]], writes=[lgt[g]])
                    S.op("dve", lambda e: e.tensor_tensor(out=mx[:], in0=lgt[0][:], in1=lgt[1][:], op=ALU.max), reads=[lgt[0], lgt[1]], writes=[mx])
                    S.op("dve", lambda e: e.tensor_tensor(out=mx[:], in0=mx[:], in1=lgt[2][:], op=ALU.max), reads=[mx, lgt[2]], writes=[mx])
                    for g in range(3):
                        S.op("dve", lambda e, g=g: e.tensor_tensor(out=lgt[g][:], in0=lgt[g][:], in1=mx[:], op=ALU.subtract), reads=[lgt[g], mx], writes=[lgt[g]])
                        S.op("act", lambda e, g=g: e.activation(out=lgt[g][:], in_=lgt[g][:], func=AF.Exp), reads=[lgt[g]], writes=[lgt[g]])
                    S.op("dve", lambda e: e.tensor_tensor(out=ssum[:], in0=lgt[0][:], in1=lgt[1][:], op=ALU.add), reads=[lgt[0], lgt[1]], writes=[ssum])
                    S.op("dve", lambda e: e.tensor_tensor(out=ssum[:], in0=ssum[:], in1=lgt[2][:], op=ALU.add), reads=[ssum, lgt[2]], writes=[ssum])
                    S.op("dve", lambda e: e.reciprocal(out=ssum[:], in_=ssum[:]), reads=[ssum], writes=[ssum])
                    mt = mixed.next()
                    for g in range(3):
                        S.op("dve", lambda e, g=g: e.tensor_tensor(out=lgt[g][:], in0=lgt[g][:], in1=ssum[:], op=ALU.mult), reads=[lgt[g], ssum], writes=[lgt[g]])
                        S.op("dve", lambda e, g=g: e.tensor_tensor(out=mt[:, g * 1024:(g + 1) * 1024].rearrange("p (s d) -> p s d", d=64),
                                                                    in0=ogt[g][:].rearrange("p (s d) -> p s d", d=64),
                                                                    in1=lgt[g][:, :].unsqueeze(2).to_broadcast([128, 16, 64]), op=ALU.mult),
                             reads=[ogt[g], lgt[g]], writes=[mt], acc=(g > 0))
                    transpose_chunks(S, C, mt, lambda c, mt=mt: mt[:, c * 128:(c + 1) * 128], 24, mixedT,
                                     lambda c0_, n, t=t: mixedT[:, c0_:c0_ + n, t * 128:(t + 1) * 128], pstB)
                S.barrier()
            wo = Rot([S.sb(eB, f"wo{i}", [128, 24, 512], BF16) for i in range(2)])
            psy = Rot([S.ps(eB, f"psyA{i}", [128, 512], F32) for i in range(2)])
            ysb = Rot([S.sb(eB, f"ysbA{i}", [128, 512], F32) for i in range(3)])
            for nch in range(D // 512):
                w = wo.next()
                S.dma("pool", w[:], w_o[:, nch * 512:(nch + 1) * 512].rearrange("(kc p) n -> p kc n", p=128), reads=[w_o], writes=[w])
                for t in range(NT):
                    ps = psy.next()
                    for kc in range(24):
                        S.op("pe", lambda e, kc=kc: e.matmul(ps[:], mixedT[:, kc, t * 128:(t + 1) * 128], w[:, kc, :], start=(kc == 0), stop=(kc == 23)),
                             reads=[mixedT, w], writes=[ps], acc=(kc > 0))
                    yb = ysb.next()
                    S.op("act", lambda e: e.activation(out=yb[:], in_=ps[:], func=AF.Copy), reads=[ps], writes=[yb])
                    S.dma("sp", y_scr[t * 128:(t + 1) * 128, nch * 512:(nch + 1) * 512], yb[:], reads=[yb], writes=[y_scr], acc=True)
            S.barrier()
        if stop == 'wo':
            raise _Stop()
        with ExitStack() as e3:
            g1 = S.sb(e3, "g1a", [128, D], F32)
            lng = S.sb(e3, "lnga", [128, D], F32)
            lnb = S.sb(e3, "lnba", [128, D], F32)
            load_bc(S, "sp", g1, vecs, (VR(2) if ctx else vecs[2:3, :]))
            plus_one(S, g1)
            load_bc(S, "sp", lng, vecs, (VR(3) if ctx else vecs[3:4, :]))
            load_bc(S, "sp", lnb, vecs, (VR(4) if ctx else vecs[4:5, :]))
            tail = Tail(S, C, e3, (VR(6) if ctx else vecs[6:7, :]), (VR(5) if ctx else vecs[5:6, :]), vecs, wr_d, br_d, x_out, h_out, idx_out, p_out, scat=(ctx.scat if ctx else None))
            ys = Rot([S.sb(e3, f"ya_{i}", [128, D], F32) for i in range(2)])
            xs_ = Rot([S.sb(e3, f"xa_{i}", [128, D], F32) for i in range(2)])
            outs = Rot([S.sb(e3, f"oa_{i}", [128, D], F32) for i in range(2)])
            st = S.sb(e3, "sta", [128, 4, 6], F32)
            mv3 = S.sb(e3, "mva", [128, 2], F32)
            rstd3 = S.sb(e3, "rstda", [128, 1], F32)
            for t in range(NT):
                y = ys.next()
                xx = xs_.next()
                o = outs.next()
                S.dma("sp", y[:], y_scr[t * 128:(t + 1) * 128, :], reads=[y_scr], writes=[y])
                S.dma("sp", xx[:], xext[OWN0 + t * 128:OWN0 + (t + 1) * 128, :], reads=[xext], writes=[xx])
                residual_ln(S, xx, y, g1, lng, lnb, o, st, mv3, rstd3)
                tail.run(t, o)
        if not ctx:
            S.finish([x_out, h_out, idx_out, p_out])
    except _Stop:
        pass
    return nc


def attn_core_inputs(x_b, pos_b, T0):
    xext = np.zeros((EXT, D), np.float32)
    pext = np.zeros((EXT,), np.int32)
    lo = T0 - OWN0
    s = max(lo, 0)
    xext[s - lo:] = x_b[s:T0 + TOK]
    pext[s - lo:] = pos_b[s:T0 + TOK]
    postab = np.zeros((128, NGT), np.int32)
    hbv = np.zeros((3, 128), np.float32)
    for g in range(3):
        d = DILS[g]
        for ti, tl in enumerate(group_tiles(g)):
            u = tl["u0"] + d * np.arange(tl["R"])
            postab[:tl["R"], GT_OFF[g] + ti] = pext[u]
        first_q = next(tl for tl in group_tiles(g) if tl["halo"])
        prev = group_tiles(g)[first_q["prev"]]
        u = prev["u0"] + d * np.arange(128)
        hbv[g] = np.where(u + lo >= 0, 0.0, NEG)
    return {"xext": xext, "postab": postab, "hb": hbv}


_NC_CACHE = {}
_DBG = None


def _get(name, fn):
    if name not in _NC_CACHE:
        _NC_CACHE[name] = fn()
    return _NC_CACHE[name]


def _run(nc, in_maps):
    res = run_bass_kernel_spmd(nc, in_maps, core_ids=list(range(NCORES)))
    return res.results


def _pk(v):
    return np.ascontiguousarray(np.asarray(v).reshape(-1, 128).T)


def moe_w_layout(w, nchunk):
    nl = w.shape[0]
    r = w.reshape(nl, 16, 128, nchunk, 256).transpose(0, 3, 2, 1, 4)
    return np.ascontiguousarray(r).reshape(nl * nchunk * 128, 16 * 256)


def _moe_launch(h_list, idx_list, p_list, w_in, b_in, w_out, b_out):
    nc = _get("moe", build_moe)
    h_all = np.concatenate(h_list, 0)
    idx_all = np.concatenate(idx_list, 0).view(np.int32)
    p_all = np.concatenate(p_list, 0)
    in_maps = []
    for i in range(NCORES):
        e0 = i * NLOC
        in_maps.append({
            "h_all": h_all, "idx_all": idx_all, "p_all": p_all,
            "eids": np.arange(e0, e0 + NLOC, dtype=np.float32).reshape(1, NLOC),
            "w_in": moe_w_layout(w_in[e0:e0 + NLOC], 16),
            "b_in": np.ascontiguousarray(b_in[e0:e0 + NLOC].reshape(NLOC, 2 * FF // 128, 128).transpose(0, 2, 1).reshape(NLOC * 128, 2 * FF // 128)),
            "w_out": moe_w_layout(w_out[e0:e0 + NLOC], 8),
            "b_out": np.ascontiguousarray(b_out[e0:e0 + NLOC]),
        })
    res = _run(nc, in_maps)
    partials = [r["partial"] for r in res]
    return [np.stack([partials[c][i * TOK:(i + 1) * TOK] for c in range(NCORES)], 0) for i in range(NCORES)]


def kernel_unfused(x, c, positions, cond_w, cond_b, ln_g, ln_b, attn_w_qkv, attn_w_o,
           sg_w_in, sg_b_in, sg_ln_g, sg_ln_b, sg_w_spatial, sg_b_spatial, sg_w_out,
           router_w, router_b, expert_w_in, expert_b_in, expert_w_out, expert_b_out):
    f32 = lambda a: np.asarray(a, dtype=np.float32)
    x, c, cond_w, cond_b, ln_g, ln_b = f32(x), f32(c), f32(cond_w), f32(cond_b), f32(ln_g), f32(ln_b)
    positions = np.asarray(positions).astype(np.int32)
    attn_w_qkv, attn_w_o = f32(attn_w_qkv), f32(attn_w_o)
    sg_w_in, sg_b_in, sg_ln_g, sg_ln_b = f32(sg_w_in), f32(sg_b_in), f32(sg_ln_g), f32(sg_ln_b)
    sg_w_spatial, sg_b_spatial, sg_w_out = f32(sg_w_spatial), f32(sg_b_spatial), f32(sg_w_out)
    router_w, router_b = f32(router_w), f32(router_b)
    expert_w_in, expert_b_in, expert_w_out, expert_b_out = f32(expert_w_in), f32(expert_b_in), f32(expert_w_out), f32(expert_b_out)
    B = x.shape[0]

    cT = np.ascontiguousarray(c.reshape(B, D // 128, 128).transpose(2, 1, 0))
    in_maps = [{"cT": cT, "cw": np.ascontiguousarray(cond_w[:, :, i * MODW:(i + 1) * MODW]),
                "cb": np.ascontiguousarray(cond_b[:, i * MODW:(i + 1) * MODW])} for i in range(NCORES)]
    res = _run(_get("cond", build_cond), in_maps)
    mod = np.concatenate([r["mod"] for r in res], axis=2).reshape(DEPTH, B, 6, D)
    zero = np.zeros((D,), np.float32)

    in_maps = []
    for i in range(NCORES):
        b, T0 = i // 4, (i % 4) * TOK
        m = attn_core_inputs(x[b], positions[b], T0)
        md = mod[0, b]
        m["vecs"] = np.stack([md[0], md[1], md[2], ln_g[0, 0], ln_b[0, 0], md[3], md[4], zero], 0)
        m.update({"w_qkv": attn_w_qkv[0], "w_o": attn_w_o[0], "wr": router_w[0], "br": router_b[0][None, :]})
        in_maps.append(m)
    res = _run(_get("attn", build_attn), in_maps)
    x1 = [r["x_out"] for r in res]
    if _DBG is not None:
        _DBG.update(mod=mod, x1=x1, h2=[r["h_out"] for r in res], idx2=[r["idx_out"] for r in res], p2=[r["p_out"] for r in res])
        if _DBG.get("stop") == 1:
            return None
    parts = _moe_launch([r["h_out"] for r in res], [r["idx_out"] for r in res], [r["p_out"] for r in res],
                        expert_w_in[0], expert_b_in[0], expert_w_out[0], expert_b_out[0])

    if _DBG is not None:
        _DBG.update(parts0=parts)
        if _DBG.get("stop") == 2:
            return None
    in_maps = []
    for i in range(NCORES):
        b = i // 4
        m0, m1 = mod[0, b], mod[1, b]
        vecs = np.stack([m0[5], ln_g[0, 1], ln_b[0, 1], m1[0], m1[1], m1[2], ln_g[1, 0], ln_b[1, 0], m1[3], m1[4]], 0)
        in_maps.append({
            "parts": parts[i], "x_in": x1[i], "vecs": vecs, "sg_w_in": sg_w_in[0],
            "bu_pk": _pk(sg_b_in[0, :SGW]), "bv": np.ascontiguousarray(sg_b_in[0, SGW:][None, :]),
            "lng_pk": _pk(sg_ln_g[0]), "lnb_pk": _pk(sg_ln_b[0]),
            "w_sp": sg_w_spatial[0], "b_sp": np.ascontiguousarray(sg_b_spatial[0].reshape(1, -1)),
            "sg_w_out": sg_w_out[0], "wr": router_w[1], "br": router_b[1][None, :],
        })
    res = _run(_get("mid", build_mid), in_maps)
    x3 = [r["x_out"] for r in res]
    if _DBG is not None:
        _DBG.update(x3=x3, h4=[r["h_out"] for r in res], idx4=[r["idx_out"] for r in res], p4=[r["p_out"] for r in res])
        if _DBG.get("stop") == 3:
            return None
    parts = _moe_launch([r["h_out"] for r in res], [r["idx_out"] for r in res], [r["p_out"] for r in res],
                        expert_w_in[1], expert_b_in[1], expert_w_out[1], expert_b_out[1])

    in_maps = []
    for i in range(NCORES):
        b = i // 4
        in_maps.append({"parts": parts[i], "x_in": x3[i], "vecs": np.stack([mod[1, b][5], ln_g[1, 1], ln_b[1, 1]], 0)})
    res = _run(_get("final", build_final), in_maps)
    out = np.concatenate([r["out"] for r in res], 0).reshape(B, SEQ, D)
    return out.astype(np.float32)


NF = 2
NPAIR = 2
NCF = NF * NPAIR
PTOK = NTOK // NPAIR
CTOK = PTOK // NF
NCHK = CTOK // TOK
NLOCF = NEXP // NF
NBLKF = 28 if NPAIR == 4 else (40 if NPAIR == 2 else 64)
MODF = 6 * D


class Ctx:
    pass


def xbarrier(S, nc, tok):
    S.barrier()
    nc.all_core_barrier()
    tp, ts, dsrc = tok
    S.op("pool", lambda e: e.memset(tp[:], 1.0), writes=[tp])
    S.dma("sp", ts[:], dsrc, writes=[ts])
    for e in ("pe", "dve", "act", "pool", "sp"):
        S._deps(e, [tp, ts], [], False)


def emit_cond_f(S, C, cT, cw, cb, modv):
    KC = D // 128
    with ExitStack() as es:
        ct = S.sb(es, "ct", [128, KC, 1], F32)
        S.dma("sp", ct[:], cT[:, :, :], writes=[ct])
        S.op("act", lambda e: e.activation(out=ct[:], in_=ct[:], func=AF.Silu), reads=[ct], writes=[ct])
        wbs = Rot([S.sb(es, f"cwb{i}", [128, KC, 512], F32) for i in range(3)])
        brs = Rot([S.sb(es, f"cbr{i}", [1, 512], F32) for i in range(2)])
        pss = Rot([S.ps(es, f"cps{i}", [1, 512], F32) for i in range(2)])
        obs = Rot([S.sb(es, f"cob{i}", [1, 512], F32) for i in range(2)])
        for l in range(DEPTH):
            for nch in range(MODF // 512):
                wbuf = wbs.next()
                S.dma("sp", wbuf[:], cw[l, :, nch * 512:(nch + 1) * 512].rearrange("(kc p) n -> p kc n", p=128), reads=[cw], writes=[wbuf])
                br = brs.next()
                S.dma("sp", br[:], cb[l:l + 1, nch * 512:(nch + 1) * 512], reads=[cb], writes=[br])
                ps = pss.next()
                for kc in range(KC):
                    S.op("pe", lambda e, kc=kc: e.matmul(ps[:], ct[:, kc, :], wbuf[:, kc, :], start=(kc == 0), stop=False), reads=[ct, wbuf], writes=[ps], acc=(kc > 0))
                S.op("pe", lambda e: e.matmul(ps[:], C["ones32"][0:1, 0:1], br[0:1, :], start=False, stop=True), reads=[C["ones32"], br], writes=[ps], acc=True)
                ob = obs.next()
                S.op("dve", lambda e: e.tensor_copy(out=ob[:], in_=ps[:]), reads=[ps], writes=[ob])
                S.dma("sp", modv[l:l + 1, nch * 512:(nch + 1) * 512], ob[:], reads=[ob], writes=[modv], acc=True)
        S.barrier()


class PartLoader:
    nparts = NF

    def __init__(self, PART, pidx, tile0, breg):
        self.PART, self.pidx, self.tile0, self.breg = PART, pidx, tile0, breg

    def __call__(self, S, c, t, dst):
        for hf in range(2):
            S.dma("pool", None, None, reads=[self.PART[hf], self.pidx], writes=[dst], acc=(hf > 0),
                  fn=lambda e, hf=hf: e.indirect_dma_start(out=dst[:, hf * (D // 2):(hf + 1) * (D // 2)], out_offset=None, in_=self.PART[hf][:, :],
                                                           in_offset=bass.IndirectOffsetOnAxis(ap=self.pidx[:, c, self.tile0 + t:self.tile0 + t + 1], axis=0),
                                                           bounds_check=self.breg, oob_is_err=False))


def build_fused():
    nc = bass.Bass("TRN2", target_bir_lowering=False, num_devices=NCF)
    with ExitStack() as es:
        S = Sched(nc, es)
        I = lambda name, shape, dt: S.dram(name, shape, dt, kind="ExternalInput")
        x_pad = I("x_pad", [OWN0 + CTOK, D], F32)
        postab = I("postab", [NCHK, 128, NGT], I32)
        hb = I("hb", [NCHK, 3, 128], F32)
        cT = I("cT", [128, D // 128, 1], F32)
        cond_w = I("cond_w", [DEPTH, D, MODF], F32)
        cond_b = I("cond_b", [DEPTH, MODF], F32)
        lnp = I("lnp", [8, D], F32)
        w_qkv = I("w_qkv", [D, 9216], F32)
        w_o = I("w_o", [3072, D], F32)
        wr = I("wr", [DEPTH, D, NEXP], F32)
        br = I("br", [DEPTH, NEXP], F32)
        sg_w_in = I("sg_w_in", [D, 2 * SGW], F32)
        bu_pk = I("bu_pk", [128, SGC], F32)
        bv = I("bv", [1, SGW], F32)
        lng_pk = I("lng_pk", [128, SGC], F32)
        lnb_pk = I("lnb_pk", [128, SGC], F32)
        w_sp = I("w_sp", [16, 128, 128], F32)
        b_sp = I("b_sp", [1, 16 * 128], F32)
        sg_w_out = I("sg_w_out", [SGW, D], F32)
        e_w_in = [I(f"e_w_in{l}", [NLOCF * 16 * 128, 4096], F32) for l in range(DEPTH)]
        e_b_in = [I(f"e_b_in{l}", [NLOCF * 128, 2 * FF // 128], F32) for l in range(DEPTH)]
        e_w_out = [I(f"e_w_out{l}", [NLOCF * 8 * 128, 4096], F32) for l in range(DEPTH)]
        e_b_out = [I(f"e_b_out{l}", [NLOCF, D], F32) for l in range(DEPTH)]
        eids = I("eids", [1, NLOCF], F32)
        rowidx_d = I("rowidx", [128, NCHK * 8], I32)
        prow_d = I("prow", [128, PTOK // 128], I32)
        pidx_d = I("pidx", [128, NF, NCHK * 8], I32)
        out = S.dram("out", [CTOK, D], F32, kind="ExternalOutput")
        modv = S.dram("modv", [DEPTH, MODF], F32)
        x1_all = S.dram("x1_all", [CTOK, D], F32)
        x3_all = S.dram("x3_all", [CTOK, D], F32)
        o_scr = [S.dram(f"o_scr{g}", [TOK, 1024], F32) for g in range(3)]
        lse_scr = [S.dram(f"lse_scr{g}", [TOK, 16], F32) for g in range(3)]
        y_scr_a = S.dram("y_scr_a", [TOK, D], F32)
        x2_scr = S.dram("x2_scr", [TOK, D], F32)
        y_scr = S.dram("y_scr", [TOK, D], F32)
        xs = S.dram("xs", [NBLKF * 512, D], BF16)
        ysd = [S.dram(f"ys{i}", [NBLKF * 512 + 128, D // 2], F32) for i in range(2)]
        SH = lambda name, shape, dt: T(nc.dram_tensor(name, shape, dt, addr_space="Shared").ap(), name)
        H = SH("H_sh", [PTOK, D], BF16)
        RT = SH("RT_sh", [PTOK, 8], F32)
        PART = [SH(f"PART{i}_sh", [NF * PTOK, D // 2], F32) for i in range(2)]

        C = make_consts(S, es)
        rowidx = S.sb(es, "rowidx", [128, NCHK * 8], I32)
        prow = S.sb(es, "prow", [128, PTOK // 128], I32)
        pidx = S.sb(es, "pidx", [128, NF, NCHK * 8], I32)
        S.dma("sp", rowidx[:], rowidx_d[:, :], writes=[rowidx])
        S.dma("sp", prow[:], prow_d[:, :], writes=[prow])
        S.dma("sp", pidx[:], pidx_d[:, :, :], writes=[pidx])
        tok = (S.sb(es, "tokp", [1, 8], F32), S.sb(es, "toks", [1, 8], F32), lnp[0:1, 0:8])
        breg_h = nc.gpsimd.to_reg(PTOK - 1)
        breg_part = nc.gpsimd.to_reg(NF * PTOK - 1)
        dummy = T(None, "dummy")

        def mk(**kw):
            c = Ctx()
            c.nc, c.S, c.C = nc, S, C
            c.t = {}
            c.sb = {"prow": prow}
            c.breg_part = breg_part
            c.scat = None
            c.part_loader = None
            c.vr = {}
            for k, v in kw.items():
                setattr(c, k, v)
            return c

        mrow = lambda l, j: modv[l:l + 1, j * D:(j + 1) * D]
        lrow = lambda k: lnp[k:k + 1, :]
        Tv = lambda ap, name: T(ap, name)

        emit_cond_f(S, C, cT, cond_w, cond_b, modv)

        for ch in range(NCHK):
            ctx = mk()
            ctx.t = {"xext": Tv(x_pad[ch * TOK:ch * TOK + EXT, :], "xext"), "vecs": dummy, "postab": Tv(postab[ch], "postab"), "hb": Tv(hb[ch], "hb"),
                     "w_qkv": w_qkv, "w_o": w_o, "wr": Tv(wr[0], "wr0"), "br": Tv(br[0:1, :], "br0"),
                     "x_out": Tv(x1_all[ch * TOK:(ch + 1) * TOK, :], "x1c"), "h_out": dummy, "idx_out": dummy, "p_out": dummy,
                     "o_scr0": o_scr[0], "o_scr1": o_scr[1], "o_scr2": o_scr[2], "lse_scr0": lse_scr[0], "lse_scr1": lse_scr[1], "lse_scr2": lse_scr[2],
                     "y_scr_a": y_scr_a}
            ctx.vr = {0: mrow(0, 0), 1: mrow(0, 1), 2: mrow(0, 2), 3: lrow(0), 4: lrow(1), 5: mrow(0, 3), 6: mrow(0, 4)}
            ctx.scat = {"H": H, "RT": RT, "rowidx": rowidx, "tile0": ch * 8, "breg": breg_h}
            build_attn(None, ctx)
            S.barrier()
        xbarrier(S, nc, tok)

        def moe(l):
            ctx = mk()
            ctx.t = {"h_all": H, "RT": RT, "idx_all": dummy, "p_all": dummy, "eids": eids, "w_in": e_w_in[l], "b_in": e_b_in[l],
                     "w_out": e_w_out[l], "b_out": e_b_out[l], "partial": dummy, "xs": xs, "ys0": ysd[0], "ys1": ysd[1],
                     "PART0": PART[0], "PART1": PART[1]}
            build_moe(PTOK, NLOCF, NBLKF, ctx)
            xbarrier(S, nc, tok)

        moe(0)

        for ch in range(NCHK):
            ctx = mk()
            ctx.t = {"parts": dummy, "x_in": Tv(x1_all[ch * TOK:(ch + 1) * TOK, :], "x1c"), "vecs": dummy, "sg_w_in": sg_w_in, "bu_pk": bu_pk, "bv": bv,
                     "lng_pk": lng_pk, "lnb_pk": lnb_pk, "w_sp": w_sp, "b_sp": b_sp, "sg_w_out": sg_w_out, "wr": Tv(wr[1], "wr1"), "br": Tv(br[1:2, :], "br1"),
                     "x_out": Tv(x3_all[ch * TOK:(ch + 1) * TOK, :], "x3c"), "h_out": dummy, "idx_out": dummy, "p_out": dummy, "x2_scr": x2_scr, "y_scr": y_scr}
            ctx.vr = {0: mrow(0, 5), 1: lrow(2), 2: lrow(3), 3: mrow(1, 0), 4: mrow(1, 1), 5: mrow(1, 2), 6: lrow(4), 7: lrow(5), 8: mrow(1, 3), 9: mrow(1, 4)}
            ctx.scat = {"H": H, "RT": RT, "rowidx": rowidx, "tile0": ch * 8, "breg": breg_h}
            ctx.part_loader = PartLoader(PART, pidx, ch * 8, breg_part)
            build_mid(ctx)
            S.barrier()
        xbarrier(S, nc, tok)

        moe(1)

        for ch in range(NCHK):
            ctx = mk()
            ctx.t = {"parts": dummy, "x_in": Tv(x3_all[ch * TOK:(ch + 1) * TOK, :], "x3c"), "vecs": dummy, "out": Tv(out[ch * TOK:(ch + 1) * TOK, :], "outc")}
            ctx.vr = {0: mrow(1, 5), 1: lrow(6), 2: lrow(7)}
            ctx.part_loader = PartLoader(PART, pidx, ch * 8, breg_part)
            build_final(ctx)
            S.barrier()
        S.barrier()
    return nc


def kernel(x, c, positions, cond_w, cond_b, ln_g, ln_b, attn_w_qkv, attn_w_o,
           sg_w_in, sg_b_in, sg_ln_g, sg_ln_b, sg_w_spatial, sg_b_spatial, sg_w_out,
           router_w, router_b, expert_w_in, expert_b_in, expert_w_out, expert_b_out):
    f32 = lambda a: np.asarray(a, dtype=np.float32)
    x, c, cond_w, cond_b, ln_g, ln_b = f32(x), f32(c), f32(cond_w), f32(cond_b), f32(ln_g), f32(ln_b)
    positions = np.asarray(positions).astype(np.int32)
    attn_w_qkv, attn_w_o = f32(attn_w_qkv), f32(attn_w_o)
    sg_w_in, sg_b_in, sg_ln_g, sg_ln_b = f32(sg_w_in), f32(sg_b_in), f32(sg_ln_g), f32(sg_ln_b)
    sg_w_spatial, sg_b_spatial, sg_w_out = f32(sg_w_spatial), f32(sg_b_spatial), f32(sg_w_out)
    router_w, router_b = f32(router_w), f32(router_b)
    expert_w_in, expert_b_in, expert_w_out, expert_b_out = f32(expert_w_in), f32(expert_b_in), f32(expert_w_out), f32(expert_b_out)
    lnp = np.ascontiguousarray(np.stack([ln_g[0, 0], ln_b[0, 0], ln_g[0, 1], ln_b[0, 1], ln_g[1, 0], ln_b[1, 0], ln_g[1, 1], ln_b[1, 1]], 0))
    p128 = np.arange(128, dtype=np.int32)[:, None]
    in_maps = []
    ppb = NPAIR // 2
    place = []
    wcache = {}
    for ci in range(NCF):
        q, i = ci // NF, ci % NF
        b = q // ppb
        Tc = (q % ppb) * PTOK + i * CTOK
        place.append((b, Tc))
        x_pad = np.zeros((OWN0 + CTOK, D), np.float32)
        lo = Tc - OWN0
        s0 = max(lo, 0)
        x_pad[s0 - lo:] = x[b, s0:Tc + CTOK]
        pt, hbv = [], []
        for ch in range(NCHK):
            m = attn_core_inputs(x[b], positions[b], Tc + ch * TOK)
            pt.append(m["postab"])
            hbv.append(m["hb"])
        e0 = i * NLOCF
        if i not in wcache:
            wcache[i] = {
                **{f"e_w_in{l}": moe_w_layout(expert_w_in[l, e0:e0 + NLOCF], 16) for l in range(DEPTH)},
                **{f"e_b_in{l}": np.ascontiguousarray(expert_b_in[l, e0:e0 + NLOCF].reshape(NLOCF, 2 * FF // 128, 128).transpose(0, 2, 1).reshape(NLOCF * 128, 2 * FF // 128)) for l in range(DEPTH)},
                **{f"e_w_out{l}": moe_w_layout(expert_w_out[l, e0:e0 + NLOCF], 8) for l in range(DEPTH)},
                **{f"e_b_out{l}": np.ascontiguousarray(expert_b_out[l, e0:e0 + NLOCF]) for l in range(DEPTH)},
            }
        in_maps.append({
            "x_pad": x_pad, "postab": np.stack(pt, 0), "hb": np.stack(hbv, 0),
            "cT": np.ascontiguousarray(c[b].reshape(D // 128, 128).T[:, :, None]),
            "cond_w": cond_w, "cond_b": cond_b, "lnp": lnp, "w_qkv": attn_w_qkv[0], "w_o": attn_w_o[0],
            "wr": router_w, "br": router_b,
            "sg_w_in": sg_w_in[0], "bu_pk": _pk(sg_b_in[0, :SGW]), "bv": np.ascontiguousarray(sg_b_in[0, SGW:][None, :]),
            "lng_pk": _pk(sg_ln_g[0]), "lnb_pk": _pk(sg_ln_b[0]), "w_sp": sg_w_spatial[0],
            "b_sp": np.ascontiguousarray(sg_b_spatial[0].reshape(1, -1)), "sg_w_out": sg_w_out[0],
            **wcache[i],
            "eids": np.arange(e0, e0 + NLOCF, dtype=np.float32).reshape(1, NLOCF),
            "rowidx": (i * CTOK + np.arange(NCHK * 8, dtype=np.int32)[None, :] * 128 + p128).astype(np.int32),
            "prow": (i * PTOK + np.arange(PTOK // 128, dtype=np.int32)[None, :] * 128 + p128).astype(np.int32),
            "pidx": np.stack([(cc * PTOK + i * CTOK + np.arange(NCHK * 8, dtype=np.int32)[None, :] * 128 + p128) for cc in range(NF)], 1).astype(np.int32),
        })
    nc = _get("fused", build_fused)
    res = run_bass_kernel_spmd(nc, in_maps, core_ids=list(range(NCF)))
    out = np.zeros((x.shape[0], SEQ, D), np.float32)
    for ci, (b, Tc) in enumerate(place):
        out[b, Tc:Tc + CTOK] = res.results[ci]["out"]
    return out
```

```python
import numpy as np
from contextlib import ExitStack
import concourse.bass as bass
import concourse.mybir as mybir
from concourse.bass_utils import run_bass_kernel_spmd

F32 = mybir.dt.float32
BF16 = mybir.dt.bfloat16
I32 = mybir.dt.int32
U32 = mybir.dt.uint32
AF = mybir.ActivationFunctionType
ALU = mybir.AluOpType
AX = mybir.AxisListType

D = 2048
NCORES = 8
TOK = 1024
NTOK = 8192
SEQ = 4096
DEPTH = 2
ALPHA = float((2 * DEPTH) ** 0.25)
LN_EPS = 1e-5
NEXP = 32
NLOC = 4
NBLK = 20
FF = 2048
BIGIDX = 1.0e6


class T:
    def __init__(self, t, name=""):
        self.t = t
        self.name = name
        self.w = {}
        self.r = {}

    def __getitem__(self, idx):
        return self.t[idx]


class Sched:
    NDMA = 20

    def __init__(self, nc, es):
        self.nc = nc
        self.es = es
        self.eng = {"pe": nc.tensor, "dve": nc.vector, "act": nc.scalar, "pool": nc.gpsimd, "sp": nc.sync}
        self.sem = {}
        self.cnt = {}
        self.known = {}
        for k in self.eng:
            self.sem[k] = es.enter_context(nc.semaphore("sem_" + k))
            self.cnt[k] = 0
            self.known[k] = {}
        self.dsem = {}
        self.dcnt = {}
        self.dptr = {}
        for q in ("sp", "pool", "act"):
            self.dsem[q] = [es.enter_context(nc.semaphore(f"dma_{q}_{i}")) for i in range(self.NDMA)]
            self.dcnt[q] = [0] * self.NDMA
            self.dptr[q] = 0
        self.ntensors = 0

    def sb(self, es, name, shape, dt):
        self.ntensors += 1
        return T(es.enter_context(self.nc.sbuf_tensor(f"{name}_{self.ntensors}", list(shape), dt)), name)

    def ps(self, es, name, shape, dt=F32):
        self.ntensors += 1
        return T(es.enter_context(self.nc.psum_tensor(f"{name}_{self.ntensors}", list(shape), dt)), name)

    def dram(self, name, shape, dt, kind="Internal"):
        h = self.nc.dram_tensor(name, list(shape), dt, kind=kind)
        return T(h.ap(), name)

    def _wait(self, e, sem, val):
        k = self.known[e]
        key = id(sem)
        if k.get(key, 0) >= val:
            return
        self.eng[e].wait_ge(sem, val)
        k[key] = val

    def _deps(self, e, reads, writes, acc):
        own = id(self.sem[e]) if e == "pe" else None
        for b in list(reads) + list(writes):
            if acc and any(b is x for x in writes):
                continue
            for key, (sem, val) in b.w.items():
                if key == own:
                    continue
                self._wait(e, sem, val)
        for b in writes:
            for key, (sem, val) in b.r.items():
                if key == own:
                    continue
                self._wait(e, sem, val)

    def _update(self, sem, val, reads, writes, acc):
        key = id(sem)
        for b in writes:
            if not acc:
                b.w = {}
                b.r = {}
            b.w[key] = (sem, val)
        for b in reads:
            if any(b is x for x in writes):
                continue
            b.r[key] = (sem, val)

    def op(self, e, fn, reads=(), writes=(), acc=False):
        self._deps(e, reads, writes, acc)
        ins = fn(self.eng[e])
        self.cnt[e] += 1
        ins.then_inc(self.sem[e], 1)
        self._update(self.sem[e], self.cnt[e], reads, writes, acc)
        return ins

    def dma(self, q, out, in_, reads=(), writes=(), acc=False, fn=None):
        i = self.dptr[q] % self.NDMA
        self.dptr[q] += 1
        sem = self.dsem[q][i]
        if self.dcnt[q][i] > 0:
            self._wait(q, sem, 16 * self.dcnt[q][i])
        self._deps(q, reads, writes, acc)
        if fn is None:
            ins = self.eng[q].dma_start(out=out, in_=in_)
        else:
            ins = fn(self.eng[q])
        ins.then_inc(sem, 16)
        self.dcnt[q][i] += 1
        self._update(sem, 16 * self.dcnt[q][i], reads, writes, acc)
        return ins

    def barrier(self):
        evs = []
        for k in self.eng:
            if self.cnt[k] > 0:
                evs.append((self.sem[k], self.cnt[k]))
        for q in self.dsem:
            for i in range(self.NDMA):
                if self.dcnt[q][i] > 0:
                    evs.append((self.dsem[q][i], 16 * self.dcnt[q][i]))
        for e in self.eng:
            for sem, val in evs:
                if e == "pe" and sem is self.sem["pe"]:
                    continue
                self._wait(e, sem, val)

    def finish(self, outs):
        for b in outs:
            for key, (sem, val) in b.w.items():
                self._wait("sp", sem, val)


class Rot:
    def __init__(self, items):
        self.items = items
        self.i = 0

    def next(self):
        x = self.items[self.i % len(self.items)]
        self.i += 1
        return x


def new_nc():
    return bass.Bass("TRN2", target_bir_lowering=False)


def make_consts(S, es):
    C = {}
    io_f_i = S.sb(es, "io_f_i", [128, 128], I32)
    io_p_i = S.sb(es, "io_p_i", [128, 1], I32)
    C["io_f"] = S.sb(es, "io_f", [128, 128], F32)
    C["io_p"] = S.sb(es, "io_p", [128, 1], F32)
    S.op("pool", lambda e: e.iota(io_f_i[:], pattern=[[1, 128]], base=0, channel_multiplier=0), writes=[io_f_i])
    S.op("pool", lambda e: e.iota(io_p_i[:], pattern=[[0, 1]], base=0, channel_multiplier=1), writes=[io_p_i])
    S.op("dve", lambda e: e.tensor_copy(out=C["io_f"][:], in_=io_f_i[:]), reads=[io_f_i], writes=[C["io_f"]])
    S.op("dve", lambda e: e.tensor_copy(out=C["io_p"][:], in_=io_p_i[:]), reads=[io_p_i], writes=[C["io_p"]])
    C["ident32"] = S.sb(es, "ident32", [128, 128], F32)
    C["ident16"] = S.sb(es, "ident16", [128, 128], BF16)
    S.op("dve", lambda e: e.tensor_scalar(out=C["ident32"][:], in0=C["io_f"][:], scalar1=C["io_p"][:, 0:1], scalar2=None, op0=ALU.is_equal),
         reads=[C["io_f"], C["io_p"]], writes=[C["ident32"]])
    S.op("dve", lambda e: e.tensor_copy(out=C["ident16"][:], in_=C["ident32"][:]), reads=[C["ident32"]], writes=[C["ident16"]])
    C["ones16"] = S.sb(es, "ones16", [128, 128], BF16)
    S.op("dve", lambda e: e.memset(C["ones16"][:], 1.0), writes=[C["ones16"]])
    C["ones32"] = S.sb(es, "ones32", [128, 128], F32)
    S.op("dve", lambda e: e.memset(C["ones32"][:], 1.0), writes=[C["ones32"]])
    return C


def transpose_chunks(S, C, src, src_ap_fn, nchunks, dst, dst_ap_fn, psrot, R=128, dt16=True, evac="act", NP=128):
    ident = C["ident16"] if dt16 else C["ident32"]
    for c0 in range(0, nchunks, 4):
        n = min(4, nchunks - c0)
        ps = psrot.next()
        for i in range(n):
            S.op("pe", lambda e, i=i: e.transpose(ps[0:NP, i, 0:R], src_ap_fn(c0 + i), ident[0:R, 0:R]), reads=[src, ident], writes=[ps], acc=(i > 0))
        if evac == "act":
            S.op("act", lambda e: e.activation(out=dst_ap_fn(c0, n), in_=ps[0:NP, 0:n, 0:R], func=AF.Copy), reads=[ps], writes=[dst], acc=True)
        else:
            S.op("dve", lambda e: e.tensor_copy(out=dst_ap_fn(c0, n), in_=ps[0:NP, 0:n, 0:R]), reads=[ps], writes=[dst], acc=True)


def build_moe(ntok=NTOK, nloc=NLOC, nblk=NBLK, ctx=None):
    NT = ntok // 128
    KC = D // 128
    FC = FF // 128
    BR = 512
    POOL = nblk * BR
    ZROW = POOL
    nc = ctx.nc if ctx else new_nc()
    with ExitStack() as es:
        S = ctx.S if ctx else Sched(nc, es)
        DR = (lambda name, shape, dt, kind="Internal": ctx.t[name]) if ctx else S.dram
        VR = (lambda k: ctx.vr[k]) if ctx else None
        h_all = DR("h_all", [ntok, D], BF16, kind="ExternalInput")
        idx_all = DR("idx_all", [ntok, 4], I32, kind="ExternalInput")
        p_all = DR("p_all", [ntok, 4], F32, kind="ExternalInput")
        eids = DR("eids", [1, nloc], F32, kind="ExternalInput")
        w_in = DR("w_in", [nloc * 16 * 128, 4096], F32, kind="ExternalInput")
        b_in = DR("b_in", [nloc * 128, 2 * FC], F32, kind="ExternalInput")
        w_out = DR("w_out", [nloc * 8 * 128, 4096], F32, kind="ExternalInput")
        b_out = DR("b_out", [nloc, D], F32, kind="ExternalInput")
        partial = DR("partial", [ntok, D], F32, kind="ExternalOutput")
        xs = DR("xs", [POOL, D], BF16)
        ysh = [DR(f"ys{i}", [POOL + 128, D // 2], F32) for i in range(2)]

        C = ctx.C if ctx else make_consts(S, es)
        breg_sc = nc.gpsimd.to_reg(POOL - 1)
        breg_ga = nc.gpsimd.to_reg(POOL + 127)
        dest_sc = S.sb(es, "dest_sc", [128, NT, 4], I32)
        dest_ga = S.sb(es, "dest_ga", [128, NT, 4], I32)
        G = S.sb(es, "G", [128, NT, 4], F32)
        widx_in = S.sb(es, "widx_in", [128, nblk, 16], I32)
        widx_out = S.sb(es, "widx_out", [128, nblk, 8], I32)
        bidx_in = S.sb(es, "bidx_in", [128, nblk], I32)
        bidx_out = S.sb(es, "bidx_out", [128, nblk], I32)
        breg_w = nc.gpsimd.to_reg(nloc * 16 * 128 - 1)

        with ExitStack() as e1:
            idx_i = S.sb(e1, "idx_i", [128, NT, 4], I32)
            idx_f = S.sb(e1, "idx_f", [128, NT, 4], F32)
            p_sb = S.sb(e1, "p_sb", [128, NT, 4], F32)
            eid_bc = S.sb(e1, "eid_bc", [128, nloc], F32)
            eq = S.sb(e1, "eq", [128, NT, 4], F32)
            M = S.sb(e1, "M", [128, NT, nloc], F32)
            M16 = S.sb(e1, "M16", [128, NT, nloc], BF16)
            Ls = S.sb(e1, "Ls", [128, 128], BF16)
            within = S.sb(e1, "within", [128, NT, nloc], F32)
            colsum = S.sb(e1, "colsum", [128, NT, nloc], F32)
            off = S.sb(e1, "off", [128, NT, nloc], F32)
            dest = S.sb(e1, "dest", [128, NT, nloc], F32)
            tmp = S.sb(e1, "tmp", [128, NT, nloc], F32)
            cnt = S.sb(e1, "cnt", [128, nloc], F32)
            nb = S.sb(e1, "nb", [128, nloc], F32)
            nbi = S.sb(e1, "nbi", [128, nloc], I32)
            bstart = S.sb(e1, "bstart", [128, nloc], F32)
            bend = S.sb(e1, "bend", [128, nloc], F32)
            base = S.sb(e1, "base", [128, nloc], F32)
            eblk_f = S.sb(e1, "eblk_f", [128, nblk], F32)
            etmp = S.sb(e1, "etmp", [128, nblk], F32)
            NPC = (NT * nloc + 511) // 512
            psA = S.ps(e1, "psA", [128, NPC, 512], F32)
            psB = S.ps(e1, "psB", [128, NPC, 512], F32)
            zero16 = S.sb(e1, "zero16", [128, D], BF16)
            zero32 = S.sb(e1, "zero32", [128, D // 2], F32)
            S.op("dve", lambda e: e.memset(zero16[:], 0.0), writes=[zero16])
            S.op("dve", lambda e: e.memset(zero32[:], 0.0), writes=[zero32])
            for r in range(POOL // 128):
                S.dma("sp", xs[r * 128:(r + 1) * 128, :], zero16[:], reads=[zero16], writes=[xs], acc=True)
            for i in range(2):
                S.dma("sp", ysh[i][ZROW:ZROW + 128, :], zero32[:], reads=[zero32], writes=[ysh[i]], acc=True)
            if ctx:
                rt_sb = S.sb(e1, "rt_sb", [128, NT, 8], F32)
                S.dma("sp", rt_sb[:], ctx.t["RT"][:, :].rearrange("(t p) k -> p t k", p=128), reads=[ctx.t["RT"]], writes=[rt_sb])
                S.op("dve", lambda e: e.tensor_copy(out=p_sb[:], in_=rt_sb[:, :, 4:8]), reads=[rt_sb], writes=[p_sb])
            else:
                S.dma("sp", idx_i[:], idx_all[:, :].rearrange("(t p) k -> p t k", p=128), writes=[idx_i])
                S.dma("sp", p_sb[:], p_all[:, :].rearrange("(t p) k -> p t k", p=128), writes=[p_sb])
            S.dma("sp", eid_bc[:], eids[0:1, :].partition_broadcast(128), writes=[eid_bc])
            if ctx:
                S.op("dve", lambda e: e.tensor_copy(out=idx_f[:], in_=rt_sb[:, :, 0:4]), reads=[rt_sb], writes=[idx_f])
            else:
                S.op("dve", lambda e: e.tensor_copy(out=idx_f[:], in_=idx_i[:]), reads=[idx_i], writes=[idx_f])
            S.op("dve", lambda e: e.tensor_scalar(out=Ls[:], in0=C["io_f"][:], scalar1=C["io_p"][:, 0:1], scalar2=None, op0=ALU.is_gt),
                 reads=[C["io_f"], C["io_p"]], writes=[Ls])
            for j in range(nloc):
                S.op("dve", lambda e, j=j: e.tensor_scalar(out=eq[:], in0=idx_f[:], scalar1=eid_bc[:, j:j + 1], scalar2=None, op0=ALU.is_equal),
                     reads=[idx_f, eid_bc], writes=[eq])
                S.op("dve", lambda e, j=j: e.tensor_reduce(out=M[:, :, j], in_=eq[:], axis=AX.X, op=ALU.add), reads=[eq], writes=[M], acc=True)
            S.op("dve", lambda e: e.tensor_copy(out=M16[:], in_=M[:]), reads=[M], writes=[M16])
            Mf = M16[:].rearrange("p t j -> p (t j)")
            W_ = NT * nloc
            for pc in range(NPC):
                a0, a1 = pc * 512, min(W_, (pc + 1) * 512)
                S.op("pe", lambda e: e.matmul(psA[:, pc, 0:a1 - a0], Ls[:], Mf[:, a0:a1], start=True, stop=True), reads=[Ls, M16], writes=[psA], acc=(pc > 0))
                S.op("pe", lambda e: e.matmul(psB[:, pc, 0:a1 - a0], C["ones16"][:], Mf[:, a0:a1], start=True, stop=True), reads=[C["ones16"], M16], writes=[psB], acc=(pc > 0))
                S.op("dve", lambda e: e.tensor_copy(out=within[:].rearrange("p t j -> p (t j)")[:, a0:a1], in_=psA[:, pc, 0:a1 - a0]), reads=[psA], writes=[within], acc=(pc > 0))
                S.op("dve", lambda e: e.tensor_copy(out=colsum[:].rearrange("p t j -> p (t j)")[:, a0:a1], in_=psB[:, pc, 0:a1 - a0]), reads=[psB], writes=[colsum], acc=(pc > 0))
            S.op("dve", lambda e: e.memset(off[:, 0, :], 0.0), writes=[off])
            for t in range(1, NT):
                S.op("dve", lambda e, t=t: e.tensor_tensor(out=off[:, t, :], in0=off[:, t - 1, :], in1=colsum[:, t - 1, :], op=ALU.add),
                     reads=[off, colsum], writes=[off])
            S.op("dve", lambda e: e.tensor_tensor(out=cnt[:], in0=off[:, NT - 1, :], in1=colsum[:, NT - 1, :], op=ALU.add), reads=[off, colsum], writes=[cnt])
            S.op("dve", lambda e: e.tensor_scalar(out=nb[:], in0=cnt[:], scalar1=1.0 / BR, scalar2=(BR - 1.0) / BR - 0.5 + 0.5 / BR, op0=ALU.mult, op1=ALU.add), reads=[cnt], writes=[nb])
            S.op("dve", lambda e: e.tensor_copy(out=nbi[:], in_=nb[:]), reads=[nb], writes=[nbi])
            S.op("dve", lambda e: e.tensor_copy(out=nb[:], in_=nbi[:]), reads=[nbi], writes=[nb])
            S.op("dve", lambda e: e.memset(bstart[:, 0:1], 0.0), writes=[bstart])
            for j in range(1, nloc):
                S.op("dve", lambda e, j=j: e.tensor_tensor(out=bstart[:, j:j + 1], in0=bstart[:, j - 1:j], in1=nb[:, j - 1:j], op=ALU.add), reads=[bstart, nb], writes=[bstart])
            S.op("dve", lambda e: e.tensor_tensor(out=bend[:], in0=bstart[:], in1=nb[:], op=ALU.add), reads=[bstart, nb], writes=[bend])
            S.op("dve", lambda e: e.tensor_scalar(out=base[:], in0=bstart[:], scalar1=float(BR), scalar2=None, op0=ALU.mult), reads=[bstart], writes=[base])
            S.op("dve", lambda e: e.memset(eblk_f[:], 0.0), writes=[eblk_f])
            for j in range(nloc - 1):
                S.op("dve", lambda e, j=j: e.tensor_scalar(out=etmp[:], in0=C["io_f"][:, 0:nblk], scalar1=bend[:, j:j + 1], scalar2=None, op0=ALU.is_ge), reads=[C["io_f"], bend], writes=[etmp])
                S.op("dve", lambda e: e.tensor_tensor(out=eblk_f[:], in0=eblk_f[:], in1=etmp[:], op=ALU.add), reads=[eblk_f, etmp], writes=[eblk_f])
            cpi = S.sb(e1, "cpi", [128, 16], F32)
            wtmp = S.sb(e1, "wtmp", [128, nblk, 16], F32)
            btmp = S.sb(e1, "btmp", [128, nblk], F32)
            for c in range(16):
                S.op("dve", lambda e, c=c: e.tensor_scalar(out=cpi[:, c:c + 1], in0=C["io_p"][:, 0:1], scalar1=float(c * 128), scalar2=None, op0=ALU.add), reads=[C["io_p"]], writes=[cpi], acc=(c > 0))
            for rb in range(nblk):
                S.op("dve", lambda e, rb=rb: e.scalar_tensor_tensor(out=wtmp[:, rb, :], in0=eblk_f[:, rb:rb + 1].to_broadcast([128, 16]), scalar=2048.0, in1=cpi[:], op0=ALU.mult, op1=ALU.add),
                     reads=[eblk_f, cpi], writes=[wtmp], acc=(rb > 0))
            usedf = S.sb(e1, "usedf", [128, nblk], F32)
            S.op("dve", lambda e: e.tensor_scalar(out=usedf[:], in0=C["io_f"][:, 0:nblk], scalar1=bend[:, nloc - 1:nloc], scalar2=None, op0=ALU.is_lt), reads=[C["io_f"], bend], writes=[usedf])
            S.op("dve", lambda e: e.tensor_tensor(out=wtmp[:], in0=wtmp[:], in1=usedf[:].unsqueeze(2).to_broadcast([128, nblk, 16]), op=ALU.mult), reads=[wtmp, usedf], writes=[wtmp])
            S.op("dve", lambda e: e.tensor_copy(out=widx_in[:], in_=wtmp[:]), reads=[wtmp], writes=[widx_in])
            for rb in range(nblk):
                S.op("dve", lambda e, rb=rb: e.scalar_tensor_tensor(out=wtmp[:, rb, 0:8], in0=eblk_f[:, rb:rb + 1].to_broadcast([128, 8]), scalar=1024.0, in1=cpi[:, 0:8], op0=ALU.mult, op1=ALU.add),
                     reads=[eblk_f, cpi], writes=[wtmp], acc=(rb > 0))
            S.op("dve", lambda e: e.tensor_tensor(out=wtmp[:, :, 0:8], in0=wtmp[:, :, 0:8], in1=usedf[:].unsqueeze(2).to_broadcast([128, nblk, 8]), op=ALU.mult), reads=[wtmp, usedf], writes=[wtmp])
            S.op("dve", lambda e: e.tensor_copy(out=widx_out[:], in_=wtmp[:, :, 0:8]), reads=[wtmp], writes=[widx_out])
            S.op("dve", lambda e: e.scalar_tensor_tensor(out=btmp[:], in0=eblk_f[:], scalar=128.0, in1=C["io_p"][:, 0:1].to_broadcast([128, nblk]), op0=ALU.mult, op1=ALU.add), reads=[eblk_f, C["io_p"]], writes=[btmp])
            S.op("dve", lambda e: e.tensor_copy(out=bidx_in[:], in_=btmp[:]), reads=[btmp], writes=[bidx_in])
            S.op("dve", lambda e: e.tensor_copy(out=bidx_out[:], in_=eblk_f[:]), reads=[eblk_f], writes=[bidx_out])
            S.op("dve", lambda e: e.tensor_tensor(out=dest[:], in0=within[:], in1=off[:], op=ALU.add), reads=[within, off], writes=[dest])
            for j in range(nloc):
                S.op("dve", lambda e, j=j: e.tensor_scalar(out=dest[:, :, j], in0=dest[:, :, j], scalar1=base[:, j:j + 1], scalar2=float(ZROW), op0=ALU.add, op1=ALU.min),
                     reads=[dest, base], writes=[dest])
            lk = S.sb(e1, "lk", [128, NT, 4], F32)
            dk = S.sb(e1, "dk", [128, NT, 4], F32)
            tk = S.sb(e1, "tk", [128, NT, 4], F32)
            S.op("dve", lambda e: e.memset(lk[:], 0.0), writes=[lk])
            S.op("dve", lambda e: e.memset(dk[:], 0.0), writes=[dk])
            for j in range(nloc):
                S.op("dve", lambda e, j=j: e.tensor_scalar(out=eq[:], in0=idx_f[:], scalar1=eid_bc[:, j:j + 1], scalar2=None, op0=ALU.is_equal),
                     reads=[idx_f, eid_bc], writes=[eq])
                S.op("dve", lambda e: e.tensor_tensor(out=lk[:], in0=lk[:], in1=eq[:], op=ALU.add), reads=[lk, eq], writes=[lk])
                S.op("dve", lambda e, j=j: e.tensor_tensor(out=tk[:], in0=eq[:], in1=dest[:, :, j:j + 1].to_broadcast([128, NT, 4]), op=ALU.mult), reads=[eq, dest], writes=[tk])
                S.op("dve", lambda e: e.tensor_tensor(out=dk[:], in0=dk[:], in1=tk[:], op=ALU.add), reads=[dk, tk], writes=[dk])
            S.op("dve", lambda e: e.tensor_tensor(out=G[:], in0=lk[:], in1=p_sb[:], op=ALU.mult), reads=[lk, p_sb], writes=[G])
            S.op("dve", lambda e: e.scalar_tensor_tensor(out=tk[:], in0=dk[:], scalar=-BIGIDX, in1=lk[:], op0=ALU.add, op1=ALU.mult), reads=[dk, lk], writes=[tk])
            S.op("dve", lambda e: e.tensor_scalar(out=tk[:], in0=tk[:], scalar1=BIGIDX, scalar2=None, op0=ALU.add), reads=[tk], writes=[tk])
            S.op("dve", lambda e: e.tensor_copy(out=dest_sc[:], in_=tk[:]), reads=[tk], writes=[dest_sc])
            S.op("dve", lambda e: e.scalar_tensor_tensor(out=tk[:], in0=dk[:], scalar=-float(ZROW), in1=lk[:], op0=ALU.add, op1=ALU.mult), reads=[dk, lk], writes=[tk])
            S.op("dve", lambda e: e.tensor_scalar(out=tk[:], in0=tk[:], scalar1=float(ZROW), scalar2=None, op0=ALU.add), reads=[tk], writes=[tk])
            S.op("dve", lambda e: e.tensor_copy(out=dest_ga[:], in_=tk[:]), reads=[tk], writes=[dest_ga])
            hbufs = Rot([S.sb(e1, f"hb{i}", [128, D], BF16) for i in range(3)])
            for t in range(NT):
                hb = hbufs.next()
                S.dma("sp", hb[:], h_all[t * 128:(t + 1) * 128, :], writes=[hb])
                for j in range(4):
                    S.dma("pool", None, None, reads=[hb, dest_sc], writes=[xs], acc=not (t == 0 and j == 0),
                          fn=lambda e, t=t, j=j, hb=hb: e.indirect_dma_start(
                              out=xs[:, :], out_offset=bass.IndirectOffsetOnAxis(ap=dest_sc[:, t, j:j + 1], axis=0),
                              in_=hb[:, :], in_offset=None, bounds_check=breg_sc, oob_is_err=False))
            S.barrier()

        with ExitStack() as e2:
            xsT = Rot([S.sb(e2, f"xsT{i}", [128, KC, BR], BF16) for i in range(2)])
            hidT = Rot([S.sb(e2, f"hidT{i}", [128, FC, BR], BF16) for i in range(2)])
            wst = Rot([S.sb(e2, f"wst{i}", [128, 16, 256], F32) for i in range(3)])
            wb = Rot([S.sb(e2, f"wb{i}", [128, 16, 256], BF16) for i in range(6)])
            xrow = Rot([S.sb(e2, f"xrow{i}", [128, D], BF16) for i in range(2)])
            bblk = Rot([S.sb(e2, f"bblk{i}", [128, 2 * FC], F32) for i in range(2)])
            bout = Rot([S.sb(e2, f"bout{i}", [128, D], F32) for i in range(1)])
            pst = Rot([S.ps(e2, f"pst{i}", [128, 4, 128], BF16) for i in range(2)])
            psg = Rot([S.ps(e2, f"psg{i}", [128, 512], F32) for i in range(2)])
            psu = Rot([S.ps(e2, f"psu{i}", [128, 512], F32) for i in range(2)])
            psy = Rot([S.ps(e2, f"psy{i}", [128, 512], F32) for i in range(2)])
            g_sb = Rot([S.sb(e2, f"g_sb{i}", [128, 512], F32) for i in range(2)])
            s_sb = Rot([S.sb(e2, f"s_sb{i}", [128, 512], F32) for i in range(2)])
            u_sb = Rot([S.sb(e2, f"u_sb{i}", [128, 512], F32) for i in range(2)])
            y_sb = Rot([S.sb(e2, f"y_sb{i}", [128, 256], F32) for i in range(3)])
            ncast = [0]

            def wchunk(src, idx_ap, idx_T):
                st = wst.next()
                S.dma("pool", None, None, reads=[src, idx_T], writes=[st],
                      fn=lambda e: e.indirect_dma_start(out=st[:].rearrange("p k n -> p (k n)"), out_offset=None, in_=src[:, :],
                                                        in_offset=bass.IndirectOffsetOnAxis(ap=idx_ap, axis=0), bounds_check=breg_w, oob_is_err=False))
                w = wb.next()
                ncast[0] += 1
                if ncast[0] % 3 == 0:
                    S.op("dve", lambda e: e.tensor_copy(out=w[:], in_=st[:]), reads=[st], writes=[w])
                else:
                    S.op("act", lambda e: e.activation(out=w[:], in_=st[:], func=AF.Copy), reads=[st], writes=[w])
                return w

            for rb in range(nblk):
                bb = bblk.next()
                S.dma("pool", None, None, reads=[b_in, bidx_in], writes=[bb],
                      fn=lambda e, bb=bb: e.indirect_dma_start(out=bb[:, :], out_offset=None, in_=b_in[:, :],
                                                               in_offset=bass.IndirectOffsetOnAxis(ap=bidx_in[:, rb:rb + 1], axis=0), bounds_check=breg_w, oob_is_err=False))
                bo = bout.next()
                S.dma("pool", None, None, reads=[b_out, bidx_out], writes=[bo],
                      fn=lambda e, bo=bo: e.indirect_dma_start(out=bo[:, :], out_offset=None, in_=b_out[:, :],
                                                               in_offset=bass.IndirectOffsetOnAxis(ap=bidx_out[:, rb:rb + 1], axis=0), bounds_check=breg_w, oob_is_err=False))
                xt = xsT.next()
                ht = hidT.next()
                for rt in range(BR // 128):
                    xr = xrow.next()
                    S.dma("sp", xr[:], xs[rb * BR + rt * 128:rb * BR + (rt + 1) * 128, :], reads=[xs], writes=[xr])
                    transpose_chunks(S, C, xr, lambda c, xr=xr: xr[:, c * 128:(c + 1) * 128], KC, xt,
                                     lambda c0, n, rt=rt: xt[:, c0:c0 + n, rt * 128:(rt + 1) * 128], pst)
                for cp in range(8):
                    wg = wchunk(w_in, widx_in[:, rb, cp:cp + 1], widx_in)
                    wu = wchunk(w_in, widx_in[:, rb, 8 + cp:8 + cp + 1], widx_in)
                    for sub in range(2):
                        fc = cp * 2 + sub
                        pg = psg.next()
                        pu = psu.next()
                        for kc in range(KC):
                            S.op("pe", lambda e, kc=kc: e.matmul(pg[:], wg[:, kc, sub * 128:(sub + 1) * 128], xt[:, kc, :], start=(kc == 0), stop=(kc == KC - 1)),
                                 reads=[wg, xt], writes=[pg], acc=(kc > 0))
                        for kc in range(KC):
                            S.op("pe", lambda e, kc=kc: e.matmul(pu[:], wu[:, kc, sub * 128:(sub + 1) * 128], xt[:, kc, :], start=(kc == 0), stop=(kc == KC - 1)),
                                 reads=[wu, xt], writes=[pu], acc=(kc > 0))
                        g = g_sb.next()
                        s_ = s_sb.next()
                        u = u_sb.next()
                        S.op("dve", lambda e: e.tensor_scalar(out=g[:], in0=pg[:], scalar1=bb[:, fc:fc + 1], scalar2=7.0, op0=ALU.add, op1=ALU.min), reads=[pg, bb], writes=[g])
                        S.op("act", lambda e: e.activation(out=s_[:], in_=g[:], func=AF.Silu, scale=1.702), reads=[g], writes=[s_])
                        S.op("dve", lambda e: e.tensor_scalar(out=u[:], in0=pu[:], scalar1=bb[:, FC + fc:FC + fc + 1], scalar2=7.0, op0=ALU.add, op1=ALU.min), reads=[pu, bb], writes=[u])
                        S.op("dve", lambda e: e.tensor_scalar(out=u[:], in0=u[:], scalar1=-7.0, scalar2=1.0, op0=ALU.max, op1=ALU.add), reads=[u], writes=[u])
                        S.op("dve", lambda e: e.scalar_tensor_tensor(out=ht[:, fc, :], in0=s_[:], scalar=1.0 / 1.702, in1=u[:], op0=ALU.mult, op1=ALU.mult),
                             reads=[s_, u], writes=[ht], acc=True)
                for nch in range(8):
                    wo = wchunk(w_out, widx_out[:, rb, nch:nch + 1], widx_out)
                    for rt in range(BR // 128):
                        py = psy.next()
                        for fc in range(FC):
                            S.op("pe", lambda e, fc=fc: e.matmul(py[:, 0:256], ht[:, fc, rt * 128:(rt + 1) * 128], wo[:, fc, :], start=(fc == 0), stop=(fc == FC - 1)),
                                 reads=[ht, wo], writes=[py], acc=(fc > 0))
                        yb = y_sb.next()
                        S.op("dve", lambda e: e.tensor_tensor(out=yb[:], in0=py[:, 0:256], in1=bo[:, nch * 256:(nch + 1) * 256], op=ALU.add), reads=[py, bo], writes=[yb])
                        r0 = rb * BR + rt * 128
                        S.dma("sp", ysh[nch // 4][r0:r0 + 128, (nch % 4) * 256:(nch % 4 + 1) * 256], yb[:], reads=[yb], writes=[ysh[nch // 4]], acc=True)
            S.barrier()

        with ExitStack() as e3:
            H = D // 2
            gts = Rot([[S.sb(e3, f"gt{i}_{j}", [128, H], F32) for j in range(4)] for i in range(2)])
            accs = Rot([S.sb(e3, f"acc{i}", [128, H], F32) for i in range(3)])
            for t in range(NT):
                for hf in range(2):
                    gt = gts.next()
                    for j in range(4):
                        S.dma("pool", None, None, reads=[ysh[hf], dest_ga], writes=[gt[j]],
                              fn=lambda e, t=t, j=j, gt=gt, hf=hf: e.indirect_dma_start(
                                  out=gt[j][:, :], out_offset=None, in_=ysh[hf][:, :],
                                  in_offset=bass.IndirectOffsetOnAxis(ap=dest_ga[:, t, j:j + 1], axis=0),
                                  bounds_check=breg_ga, oob_is_err=False))
                    acc = accs.next()
                    S.op("dve", lambda e: e.tensor_scalar(out=acc[:], in0=gt[0][:], scalar1=G[:, t, 0:1], scalar2=None, op0=ALU.mult), reads=[gt[0], G], writes=[acc])
                    for j in range(1, 4):
                        S.op("dve", lambda e, j=j: e.scalar_tensor_tensor(out=acc[:], in0=gt[j][:], scalar=G[:, t, j:j + 1], in1=acc[:], op0=ALU.mult, op1=ALU.add),
                             reads=[gt[j], G, acc], writes=[acc])
                    if ctx:
                        S.dma("pool", None, None, reads=[acc, ctx.sb["prow"]], writes=[ctx.t["PART%d" % hf]], acc=True,
                              fn=lambda e, t=t, hf=hf, acc=acc: e.indirect_dma_start(
                                  out=ctx.t["PART%d" % hf][:, :], out_offset=bass.IndirectOffsetOnAxis(ap=ctx.sb["prow"][:, t:t + 1], axis=0),
                                  in_=acc[:, :], in_offset=None, bounds_check=ctx.breg_part, oob_is_err=False))
                    else:
                        S.dma("sp", partial[t * 128:(t + 1) * 128, hf * H:(hf + 1) * H], acc[:], reads=[acc], writes=[partial], acc=True)
        if not ctx:
            S.finish([partial])
    return nc


def load_bc(S, q, dst, src, row_ap):
    S.dma(q, dst[:], row_ap.partition_broadcast(128), reads=[src], writes=[dst])


def plus_one(S, t):
    S.op("dve", lambda e: e.tensor_scalar(out=t[:], in0=t[:], scalar1=1.0, scalar2=None, op0=ALU.add), reads=[t], writes=[t])


def ln_tile(S, v, g_bc, b_bc, out, st, mv, rstd):
    for c in range(4):
        S.op("dve", lambda e, c=c: e.bn_stats(out=st[:, c, :], in_=v[:, c * 512:(c + 1) * 512]), reads=[v], writes=[st], acc=(c > 0))
    S.op("dve", lambda e: e.bn_aggr(out=mv[:], in_=st[:].rearrange("p c s -> p (c s)")), reads=[st], writes=[mv])
    S.op("act", lambda e: e.activation(out=rstd[:], in_=mv[:, 1:2], func=AF.Sqrt, bias=LN_EPS, scale=1.0), reads=[mv], writes=[rstd])
    S.op("dve", lambda e: e.reciprocal(out=rstd[:], in_=rstd[:]), reads=[rstd], writes=[rstd])
    S.op("dve", lambda e: e.tensor_scalar(out=v[:], in0=v[:], scalar1=mv[:, 0:1], scalar2=rstd[:, 0:1], op0=ALU.subtract, op1=ALU.mult), reads=[v, mv, rstd], writes=[v])
    S.op("dve", lambda e: e.tensor_tensor(out=v[:], in0=v[:], in1=g_bc[:], op=ALU.mult), reads=[v, g_bc], writes=[v])
    S.op("dve", lambda e: e.tensor_tensor(out=out[:], in0=v[:], in1=b_bc[:], op=ALU.add), reads=[v, b_bc], writes=[out])


def residual_ln(S, x, y, g1_bc, lng_bc, lnb_bc, out, st, mv, rstd):
    S.op("dve", lambda e: e.tensor_tensor(out=y[:], in0=y[:], in1=g1_bc[:], op=ALU.mult), reads=[y, g1_bc], writes=[y])
    S.op("dve", lambda e: e.scalar_tensor_tensor(out=y[:], in0=x[:], scalar=ALPHA, in1=y[:], op0=ALU.mult, op1=ALU.add), reads=[x, y], writes=[y])
    ln_tile(S, y, lng_bc, lnb_bc, out, st, mv, rstd)


class Tail:
    def __init__(self, S, C, es, sc_row, sh_row, vecs, wr_d, br_d, x_out, h_out, idx_out, p_out, scat=None):
        self.scat = scat
        self.S, self.C = S, C
        self.sc1 = S.sb(es, "t_sc1", [128, D], F32)
        self.sh = S.sb(es, "t_sh", [128, D], F32)
        load_bc(S, "sp", self.sc1, vecs, sc_row)
        plus_one(S, self.sc1)
        load_bc(S, "sp", self.sh, vecs, sh_row)
        self.wr = S.sb(es, "t_wr", [128, D // 128, NEXP], F32)
        S.dma("sp", self.wr[:], wr_d[:, :].rearrange("(kc p) e -> p kc e", p=128), writes=[self.wr])
        self.br = S.sb(es, "t_br", [1, NEXP], F32)
        S.dma("sp", self.br[:], br_d[0:1, :], writes=[self.br])
        self.h32 = Rot([S.sb(es, f"t_h32_{i}", [128, D], F32) for i in range(2)])
        self.h16 = Rot([S.sb(es, f"t_h16_{i}", [128, D], BF16) for i in range(2)])
        self.hT = S.sb(es, "t_hT", [128, D // 128, 128], F32)
        self.pst = Rot([S.ps(es, f"t_pst{i}", [128, 4, 128], F32) for i in range(2)])
        self.pl = S.ps(es, "t_pl", [128, NEXP], F32)
        self.lg = S.sb(es, "t_lg", [128, NEXP], F32)
        self.top = S.sb(es, "t_top", [128, 8], F32)
        self.topi = S.sb(es, "t_topi", [128, 8], U32)
        self.negm = S.sb(es, "t_negm", [128, 1], F32)
        self.ex = S.sb(es, "t_ex", [128, 4], F32)
        self.sm = S.sb(es, "t_sm", [128, 1], F32)
        self.x_out, self.h_out, self.idx_out, self.p_out = x_out, h_out, idx_out, p_out
        self.rt = Rot([S.sb(es, f"t_rt{i}", [128, 8], F32) for i in range(2)])

    def run(self, t, xn):
        S, C = self.S, self.C
        r0 = t * 128
        S.dma("sp", self.x_out[r0:r0 + 128, :], xn[:], reads=[xn], writes=[self.x_out], acc=True)
        h32 = self.h32.next()
        h16 = self.h16.next()
        S.op("dve", lambda e: e.tensor_tensor(out=h32[:], in0=xn[:], in1=self.sc1[:], op=ALU.mult), reads=[xn, self.sc1], writes=[h32])
        S.op("dve", lambda e: e.tensor_tensor(out=h32[:], in0=h32[:], in1=self.sh[:], op=ALU.add), reads=[h32, self.sh], writes=[h32])
        S.op("act", lambda e: e.activation(out=h16[:], in_=h32[:], func=AF.Copy), reads=[h32], writes=[h16])
        if self.scat is None:
            S.dma("sp", self.h_out[r0:r0 + 128, :], h16[:], reads=[h16], writes=[self.h_out], acc=True)
        else:
            sc = self.scat
            S.dma("pool", None, None, reads=[h16, sc["rowidx"]], writes=[sc["H"]], acc=True,
                  fn=lambda e: e.indirect_dma_start(out=sc["H"][:, :], out_offset=bass.IndirectOffsetOnAxis(ap=sc["rowidx"][:, sc["tile0"] + t:sc["tile0"] + t + 1], axis=0),
                                                    in_=h16[:, :], in_offset=None, bounds_check=sc["breg"], oob_is_err=False))
        transpose_chunks(S, C, h32, lambda c: h32[:, c * 128:(c + 1) * 128], D // 128, self.hT,
                         lambda c0, n: self.hT[:, c0:c0 + n, :], self.pst, dt16=False, evac="act")
        KC = D // 128
        for kc in range(KC):
            S.op("pe", lambda e, kc=kc: e.matmul(self.pl[:], self.hT[:, kc, :], self.wr[:, kc, :], start=(kc == 0), stop=False),
                 reads=[self.hT, self.wr], writes=[self.pl], acc=(kc > 0))
        S.op("pe", lambda e: e.matmul(self.pl[:], C["ones32"][0:1, :], self.br[0:1, :], start=False, stop=True), reads=[C["ones32"], self.br], writes=[self.pl], acc=True)
        S.op("dve", lambda e: e.tensor_copy(out=self.lg[:], in_=self.pl[:]), reads=[self.pl], writes=[self.lg])
        S.op("dve", lambda e: e.max(out=self.top[:], in_=self.lg[:]), reads=[self.lg], writes=[self.top])
        S.op("dve", lambda e: e.max_index(out=self.topi[:], in_max=self.top[:], in_values=self.lg[:]), reads=[self.lg, self.top], writes=[self.topi])
        S.op("dve", lambda e: e.tensor_scalar(out=self.negm[:], in0=self.top[:, 0:1], scalar1=-1.0, scalar2=None, op0=ALU.mult), reads=[self.top], writes=[self.negm])
        S.op("act", lambda e: e.activation(out=self.ex[:], in_=self.top[:, 0:4], func=AF.Exp, bias=self.negm[:, 0:1], scale=1.0, accum_out=self.sm[:]),
             reads=[self.top, self.negm], writes=[self.ex, self.sm])
        S.op("dve", lambda e: e.reciprocal(out=self.sm[:], in_=self.sm[:]), reads=[self.sm], writes=[self.sm])
        S.op("dve", lambda e: e.tensor_scalar(out=self.ex[:], in0=self.ex[:], scalar1=self.sm[:, 0:1], scalar2=None, op0=ALU.mult), reads=[self.ex, self.sm], writes=[self.ex])
        if self.scat is None:
            S.dma("sp", self.idx_out[r0:r0 + 128, :], self.topi[:, 0:4], reads=[self.topi], writes=[self.idx_out], acc=True)
            S.dma("sp", self.p_out[r0:r0 + 128, :], self.ex[:], reads=[self.ex], writes=[self.p_out], acc=True)
        else:
            sc = self.scat
            rt = self.rt.next()
            S.op("dve", lambda e: e.tensor_copy(out=rt[:, 0:4], in_=self.topi[:, 0:4]), reads=[self.topi], writes=[rt])
            S.op("dve", lambda e: e.tensor_copy(out=rt[:, 4:8], in_=self.ex[:]), reads=[self.ex], writes=[rt], acc=True)
            S.dma("pool", None, None, reads=[rt, sc["rowidx"]], writes=[sc["RT"]], acc=True,
                  fn=lambda e: e.indirect_dma_start(out=sc["RT"][:, :], out_offset=bass.IndirectOffsetOnAxis(ap=sc["rowidx"][:, sc["tile0"] + t:sc["tile0"] + t + 1], axis=0),
                                                    in_=rt[:, :], in_offset=None, bounds_check=sc["breg"], oob_is_err=False))


class PartSum:
    def __init__(self, S, es, parts, x_in, vecs, gate_row, lng_row, lnb_row, loader=None):
        self.S = S
        self.loader = loader
        self.parts, self.x_in = parts, x_in
        self.g1 = S.sb(es, "ps_g1", [128, D], F32)
        self.lng = S.sb(es, "ps_lng", [128, D], F32)
        self.lnb = S.sb(es, "ps_lnb", [128, D], F32)
        load_bc(S, "sp", self.g1, vecs, gate_row)
        plus_one(S, self.g1)
        load_bc(S, "sp", self.lng, vecs, lng_row)
        load_bc(S, "sp", self.lnb, vecs, lnb_row)
        self.pb = Rot([S.sb(es, f"ps_pb{i}", [128, D], F32) for i in range(3)])
        self.acc = Rot([S.sb(es, f"ps_acc{i}", [128, D], F32) for i in range(2)])
        self.xb = Rot([S.sb(es, f"ps_xb{i}", [128, D], F32) for i in range(2)])
        self.out = Rot([S.sb(es, f"ps_out{i}", [128, D], F32) for i in range(2)])
        self.st = S.sb(es, "ps_st", [128, 4, 6], F32)
        self.mv = S.sb(es, "ps_mv", [128, 2], F32)
        self.rstd = S.sb(es, "ps_rstd", [128, 1], F32)

    def run(self, t):
        S = self.S
        r0 = t * 128
        acc = self.acc.next()
        if self.loader is None:
            S.dma("sp", acc[:], self.parts[0, r0:r0 + 128, :], reads=[self.parts], writes=[acc])
            for c in range(1, NCORES):
                pb = self.pb.next()
                S.dma("act" if c % 2 else "sp", pb[:], self.parts[c, r0:r0 + 128, :], reads=[self.parts], writes=[pb])
                S.op("dve", lambda e: e.tensor_tensor(out=acc[:], in0=acc[:], in1=pb[:], op=ALU.add), reads=[acc, pb], writes=[acc])
        else:
            self.loader(S, 0, t, acc)
            for c in range(1, self.loader.nparts):
                pb = self.pb.next()
                self.loader(S, c, t, pb)
                S.op("dve", lambda e: e.tensor_tensor(out=acc[:], in0=acc[:], in1=pb[:], op=ALU.add), reads=[acc, pb], writes=[acc])
        xb = self.xb.next()
        S.dma("sp", xb[:], self.x_in[r0:r0 + 128, :], reads=[self.x_in], writes=[xb])
        out = self.out.next()
        residual_ln(S, xb, acc, self.g1, self.lng, self.lnb, out, self.st, self.mv, self.rstd)
        return out


def build_final(ctx=None):
    nc = ctx.nc if ctx else new_nc()
    with ExitStack() as es:
        S = ctx.S if ctx else Sched(nc, es)
        DR = (lambda name, shape, dt, kind="Internal": ctx.t[name]) if ctx else S.dram
        VR = (lambda k: ctx.vr[k]) if ctx else None
        parts = DR("parts", [NCORES, TOK, D], F32, kind="ExternalInput")
        x_in = DR("x_in", [TOK, D], F32, kind="ExternalInput")
        vecs = DR("vecs", [3, D], F32, kind="ExternalInput")
        out = DR("out", [TOK, D], F32, kind="ExternalOutput")
        P = PartSum(S, es, parts, x_in, vecs, (VR(0) if ctx else vecs[0:1, :]), (VR(1) if ctx else vecs[1:2, :]), (VR(2) if ctx else vecs[2:3, :]), loader=(ctx.part_loader if ctx else None))
        for t in range(TOK // 128):
            o = P.run(t)
            S.dma("sp", out[t * 128:(t + 1) * 128, :], o[:], reads=[o], writes=[out], acc=True)
        if not ctx:
            S.finish([out])
    return nc


MODW = 6 * D // NCORES


def build_cond():
    nc = new_nc()
    KC = D // 128
    with ExitStack() as es:
        S = Sched(nc, es)
        cT = S.dram("cT", [128, KC, 2], F32, kind="ExternalInput")
        cw = S.dram("cw", [DEPTH, D, MODW], F32, kind="ExternalInput")
        cb = S.dram("cb", [DEPTH, MODW], F32, kind="ExternalInput")
        out = S.dram("mod", [DEPTH, 2, MODW], F32, kind="ExternalOutput")
        C = make_consts(S, es)
        ct = S.sb(es, "ct", [128, KC, 2], F32)
        S.dma("sp", ct[:], cT[:, :, :], writes=[ct])
        S.op("act", lambda e: e.activation(out=ct[:], in_=ct[:], func=AF.Silu), reads=[ct], writes=[ct])
        wbs = Rot([S.sb(es, f"cwb{i}", [128, KC, 512], F32) for i in range(3)])
        brs = Rot([S.sb(es, f"cbr{i}", [1, 512], F32) for i in range(2)])
        pss = Rot([S.ps(es, f"cps{i}", [2, 512], F32) for i in range(2)])
        obs = Rot([S.sb(es, f"cob{i}", [2, 512], F32) for i in range(2)])
        for l in range(DEPTH):
            for nch in range(MODW // 512):
                wbuf = wbs.next()
                S.dma("sp" if nch % 2 == 0 else "act", wbuf[:], cw[l, :, nch * 512:(nch + 1) * 512].rearrange("(kc p) n -> p kc n", p=128), reads=[cw], writes=[wbuf])
                br = brs.next()
                S.dma("sp", br[:], cb[l:l + 1, nch * 512:(nch + 1) * 512], reads=[cb], writes=[br])
                ps = pss.next()
                for kc in range(KC):
                    S.op("pe", lambda e, kc=kc: e.matmul(ps[:], ct[:, kc, :], wbuf[:, kc, :], start=(kc == 0), stop=False), reads=[ct, wbuf], writes=[ps], acc=(kc > 0))
                S.op("pe", lambda e: e.matmul(ps[:], C["ones32"][0:1, 0:2], br[0:1, :], start=False, stop=True), reads=[C["ones32"], br], writes=[ps], acc=True)
                ob = obs.next()
                S.op("dve", lambda e: e.tensor_copy(out=ob[:], in_=ps[:]), reads=[ps], writes=[ob])
                S.dma("sp", out[l, :, nch * 512:(nch + 1) * 512], ob[:], reads=[ob], writes=[out], acc=True)
        S.finish([out])
    return nc


SGW = 4096
SGC = SGW // 128


def build_mid(ctx=None):
    nc = ctx.nc if ctx else new_nc()
    KC = D // 128
    NT = TOK // 128
    with ExitStack() as es:
        S = ctx.S if ctx else Sched(nc, es)
        DR = (lambda name, shape, dt, kind="Internal": ctx.t[name]) if ctx else S.dram
        VR = (lambda k: ctx.vr[k]) if ctx else None
        parts = DR("parts", [NCORES, TOK, D], F32, kind="ExternalInput")
        x_in = DR("x_in", [TOK, D], F32, kind="ExternalInput")
        vecs = DR("vecs", [10, D], F32, kind="ExternalInput")
        sg_w_in = DR("sg_w_in", [D, 2 * SGW], F32, kind="ExternalInput")
        bu_pk = DR("bu_pk", [128, SGC], F32, kind="ExternalInput")
        bv = DR("bv", [1, SGW], F32, kind="ExternalInput")
        lng_pk = DR("lng_pk", [128, SGC], F32, kind="ExternalInput")
        lnb_pk = DR("lnb_pk", [128, SGC], F32, kind="ExternalInput")
        w_sp = DR("w_sp", [16, 128, 128], F32, kind="ExternalInput")
        b_sp = DR("b_sp", [1, 16 * 128], F32, kind="ExternalInput")
        sg_w_out = DR("sg_w_out", [SGW, D], F32, kind="ExternalInput")
        wr_d = DR("wr", [D, NEXP], F32, kind="ExternalInput")
        br_d = DR("br", [1, NEXP], F32, kind="ExternalInput")
        x_out = DR("x_out", [TOK, D], F32, kind="ExternalOutput")
        h_out = DR("h_out", [TOK, D], BF16, kind="ExternalOutput")
        idx_out = DR("idx_out", [TOK, 4], U32, kind="ExternalOutput")
        p_out = DR("p_out", [TOK, 4], F32, kind="ExternalOutput")
        x2_scr = DR("x2_scr", [TOK, D], F32)
        y_scr = DR("y_scr", [TOK, D], F32)

        C = ctx.C if ctx else make_consts(S, es)
        hT = S.sb(es, "hT", [128, KC, TOK], BF16)
        with ExitStack() as e1:
            P = PartSum(S, e1, parts, x_in, vecs, (VR(0) if ctx else vecs[0:1, :]), (VR(1) if ctx else vecs[1:2, :]), (VR(2) if ctx else vecs[2:3, :]), loader=(ctx.part_loader if ctx else None))
            sc1 = S.sb(e1, "sc1m", [128, D], F32)
            sh = S.sb(e1, "shm", [128, D], F32)
            load_bc(S, "sp", sc1, vecs, (VR(4) if ctx else vecs[4:5, :]))
            plus_one(S, sc1)
            load_bc(S, "sp", sh, vecs, (VR(3) if ctx else vecs[3:4, :]))
            h16s = Rot([S.sb(e1, f"h16_{i}", [128, D], BF16) for i in range(2)])
            htmp = S.sb(e1, "htmp", [128, D], F32)
            pst = Rot([S.ps(e1, f"pst1_{i}", [128, 4, 128], BF16) for i in range(2)])
            for t in range(NT):
                x2 = P.run(t)
                S.dma("sp", x2_scr[t * 128:(t + 1) * 128, :], x2[:], reads=[x2], writes=[x2_scr], acc=True)
                h16 = h16s.next()
                S.op("dve", lambda e: e.tensor_tensor(out=htmp[:], in0=x2[:], in1=sc1[:], op=ALU.mult), reads=[x2, sc1], writes=[htmp])
                S.op("dve", lambda e: e.tensor_tensor(out=h16[:], in0=htmp[:], in1=sh[:], op=ALU.add), reads=[htmp, sh], writes=[h16])
                transpose_chunks(S, C, h16, lambda c, h16=h16: h16[:, c * 128:(c + 1) * 128], KC, hT,
                                 lambda c0, n, t=t: hT[:, c0:c0 + n, t * 128:(t + 1) * 128], pst)
            S.barrier()
        with ExitStack() as e2:
            WcT = S.sb(e2, "WcT", [128, 16, 128], BF16)
            addend = S.sb(e2, "addend", [128, SGC, 128], F32)
            lngp = S.sb(e2, "lngp", [128, SGC], F32)
            lnbp = S.sb(e2, "lnbp", [128, SGC], F32)
            bup = S.sb(e2, "bup", [128, SGC], F32)
            S.dma("sp", lngp[:], lng_pk[:, :], writes=[lngp])
            S.dma("sp", lnbp[:], lnb_pk[:, :], writes=[lnbp])
            S.dma("sp", bup[:], bu_pk[:, :], writes=[bup])
            pst = Rot([S.ps(e2, f"pst2_{i}", [128, 4, 128], BF16) for i in range(2)])
            psm = Rot([S.ps(e2, f"psm{i}", [128, 512], F32) for i in range(3)])
            pss = Rot([S.ps(e2, f"pss{i}", [128, 4, 128], F32) for i in range(2)])
            with ExitStack() as e2a:
                wsp32 = S.sb(e2a, "wsp32", [128, 16, 128], F32)
                wsp16 = S.sb(e2a, "wsp16", [128, 16, 128], BF16)
                tril = S.sb(e2a, "tril", [128, 128], F32)
                rsw = S.sb(e2a, "rsw", [128, 16, 128], F32)
                bsp = S.sb(e2a, "bsp", [128, 16, 128], F32)
                S.dma("sp", wsp32[:], w_sp[:, :, :].rearrange("g t s -> t g s"), writes=[wsp32])
                S.dma("sp", bsp[:].rearrange("p g t -> p (g t)"), b_sp[0:1, :].partition_broadcast(128), writes=[bsp])
                S.op("dve", lambda e: e.tensor_scalar(out=tril[:], in0=C["io_f"][:], scalar1=C["io_p"][:, 0:1], scalar2=None, op0=ALU.is_le),
                     reads=[C["io_f"], C["io_p"]], writes=[tril])
                for g in range(16):
                    S.op("dve", lambda e, g=g: e.tensor_tensor(out=wsp16[:, g, :], in0=wsp32[:, g, :], in1=tril[:], op=ALU.mult), reads=[wsp32, tril], writes=[wsp16], acc=(g > 0))
                transpose_chunks(S, C, wsp16, lambda c: wsp16[:, c, :], 16, WcT, lambda c0, n: WcT[:, c0:c0 + n, :], pst)
                for q in range(4):
                    ps = psm.next()
                    S.op("pe", lambda e, q=q: e.matmul(ps[:], C["ones16"][:], WcT[:, 4 * q:4 * q + 4, :].rearrange("p g t -> p (g t)"), start=True, stop=True),
                         reads=[C["ones16"], WcT], writes=[ps])
                    S.op("dve", lambda e, q=q: e.tensor_copy(out=rsw[:, 4 * q:4 * q + 4, :].rearrange("p g t -> p (g t)"), in_=ps[:]), reads=[ps], writes=[rsw], acc=(q > 0))
                for fc in range(SGC):
                    g = fc // 2
                    S.op("dve", lambda e, fc=fc, g=g: e.scalar_tensor_tensor(out=addend[:, fc, :], in0=rsw[:, g, :], scalar=lnbp[:, fc:fc + 1], in1=bsp[:, g, :], op0=ALU.mult, op1=ALU.add),
                         reads=[rsw, lnbp, bsp], writes=[addend], acc=(fc > 0))
                S.barrier()
            v16 = [S.sb(e2, f"v16_{i}", [128, SGW], BF16) for i in range(2)]
            mixedT = S.sb(e2, "mixedT", [128, SGC, 512], BF16)
            wb = Rot([S.sb(e2, f"wbm{i}", [128, 16, 512], BF16) for i in range(4)])
            brow = Rot([S.sb(e2, f"browm{i}", [1, 512], BF16) for i in range(2)])
            u_sb = Rot([S.sb(e2, f"u_sb{i}", [128, 512], F32) for i in range(2)])
            y_sb = Rot([S.sb(e2, f"y_sbm{i}", [128, 512], F32) for i in range(3)])
            st8 = S.sb(e2, "st8", [128, 8, 6], F32)
            mv = S.sb(e2, "mvm", [128, 2], F32)
            rstd = S.sb(e2, "rstdm", [128, 1], F32)
            for half in range(2):
                for pair in range(2):
                    for nch in range(SGW // 512):
                        w = wb.next()
                        S.dma("pool", w[:], sg_w_in[:, SGW + nch * 512:SGW + (nch + 1) * 512].rearrange("(kc p) n -> p kc n", p=128), reads=[sg_w_in], writes=[w])
                        br = brow.next()
                        S.dma("pool", br[:], bv[0:1, nch * 512:(nch + 1) * 512], reads=[bv], writes=[br])
                        for tl in range(2):
                            t = half * 4 + pair * 2 + tl
                            ps = psm.next()
                            for kc in range(KC):
                                S.op("pe", lambda e, kc=kc: e.matmul(ps[:], hT[:, kc, t * 128:(t + 1) * 128], w[:, kc, :], start=(kc == 0), stop=False),
                                     reads=[hT, w], writes=[ps], acc=(kc > 0))
                            S.op("pe", lambda e: e.matmul(ps[:], C["ones16"][0:1, :], br[0:1, :], start=False, stop=True), reads=[C["ones16"], br], writes=[ps], acc=True)
                            S.op("act", lambda e, tl=tl: e.activation(out=v16[tl][:, nch * 512:(nch + 1) * 512], in_=ps[:], func=AF.Gelu), reads=[ps], writes=[v16[tl]], acc=True)
                    for tl in range(2):
                        tloc = pair * 2 + tl
                        v = v16[tl]
                        for c in range(8):
                            S.op("dve", lambda e, c=c: e.bn_stats(out=st8[:, c, :], in_=v[:, c * 512:(c + 1) * 512]), reads=[v], writes=[st8], acc=(c > 0))
                        S.op("dve", lambda e: e.bn_aggr(out=mv[:], in_=st8[:].rearrange("p c s -> p (c s)")), reads=[st8], writes=[mv])
                        S.op("act", lambda e: e.activation(out=rstd[:], in_=mv[:, 1:2], func=AF.Sqrt, bias=LN_EPS, scale=1.0), reads=[mv], writes=[rstd])
                        S.op("dve", lambda e: e.reciprocal(out=rstd[:], in_=rstd[:]), reads=[rstd], writes=[rstd])
                        S.op("dve", lambda e: e.tensor_scalar(out=v[:], in0=v[:], scalar1=mv[:, 0:1], scalar2=rstd[:, 0:1], op0=ALU.subtract, op1=ALU.mult), reads=[v, mv, rstd], writes=[v])
                        for fc0 in range(0, SGC, 4):
                            ps = pss.next()
                            for i in range(4):
                                fc = fc0 + i
                                S.op("pe", lambda e, i=i, fc=fc: e.matmul(ps[:, i, :], v[:, fc * 128:(fc + 1) * 128], WcT[:, fc // 2, :], start=True, stop=True),
                                     reads=[v, WcT], writes=[ps], acc=(i > 0))
                            for i in range(4):
                                fc = fc0 + i
                                S.op("dve", lambda e, i=i, fc=fc: e.scalar_tensor_tensor(out=mixedT[:, fc, tloc * 128:(tloc + 1) * 128], in0=ps[:, i, :], scalar=lngp[:, fc:fc + 1], in1=addend[:, fc, :], op0=ALU.mult, op1=ALU.add),
                                     reads=[ps, lngp, addend], writes=[mixedT], acc=True)
                for nch in range(SGW // 512):
                    w = wb.next()
                    S.dma("pool", w[:], sg_w_in[:, nch * 512:(nch + 1) * 512].rearrange("(kc p) n -> p kc n", p=128), reads=[sg_w_in], writes=[w])
                    for sub in range(4):
                        fc = nch * 4 + sub
                        ps = psm.next()
                        for kc in range(KC):
                            S.op("pe", lambda e, kc=kc: e.matmul(ps[:], w[:, kc, sub * 128:(sub + 1) * 128], hT[:, kc, half * 512:(half + 1) * 512], start=(kc == 0), stop=(kc == KC - 1)),
                                 reads=[w, hT], writes=[ps], acc=(kc > 0))
                        u = u_sb.next()
                        S.op("act", lambda e: e.activation(out=u[:], in_=ps[:], func=AF.Gelu, bias=bup[:, fc:fc + 1], scale=1.0), reads=[ps, bup], writes=[u])
                        S.op("dve", lambda e: e.tensor_tensor(out=mixedT[:, fc, :], in0=u[:], in1=mixedT[:, fc, :], op=ALU.mult), reads=[u, mixedT], writes=[mixedT])
                for nch in range(D // 512):
                    wa = wb.next()
                    S.dma("pool", wa[:], sg_w_out[0:2048, nch * 512:(nch + 1) * 512].rearrange("(kc p) n -> p kc n", p=128), reads=[sg_w_out], writes=[wa])
                    wbb = wb.next()
                    S.dma("pool", wbb[:], sg_w_out[2048:4096, nch * 512:(nch + 1) * 512].rearrange("(kc p) n -> p kc n", p=128), reads=[sg_w_out], writes=[wbb])
                    for tloc in range(4):
                        ps = psm.next()
                        for fc in range(SGC):
                            ww = wa if fc < 16 else wbb
                            S.op("pe", lambda e, fc=fc, ww=ww: e.matmul(ps[:], mixedT[:, fc, tloc * 128:(tloc + 1) * 128], ww[:, fc % 16, :], start=(fc == 0), stop=(fc == SGC - 1)),
                                 reads=[mixedT, ww], writes=[ps], acc=(fc > 0))
                        yb = y_sb.next()
                        S.op("act", lambda e: e.activation(out=yb[:], in_=ps[:], func=AF.Copy), reads=[ps], writes=[yb])
                        r0 = (half * 4 + tloc) * 128
                        S.dma("sp", y_scr[r0:r0 + 128, nch * 512:(nch + 1) * 512], yb[:], reads=[yb], writes=[y_scr], acc=True)
            S.barrier()
        with ExitStack() as e3:
            g1 = S.sb(e3, "g1m", [128, D], F32)
            lng = S.sb(e3, "lng3", [128, D], F32)
            lnb = S.sb(e3, "lnb3", [128, D], F32)
            load_bc(S, "sp", g1, vecs, (VR(5) if ctx else vecs[5:6, :]))
            plus_one(S, g1)
            load_bc(S, "sp", lng, vecs, (VR(6) if ctx else vecs[6:7, :]))
            load_bc(S, "sp", lnb, vecs, (VR(7) if ctx else vecs[7:8, :]))
            tail = Tail(S, C, e3, (VR(9) if ctx else vecs[9:10, :]), (VR(8) if ctx else vecs[8:9, :]), vecs, wr_d, br_d, x_out, h_out, idx_out, p_out, scat=(ctx.scat if ctx else None))
            ys = Rot([S.sb(e3, f"y3_{i}", [128, D], F32) for i in range(2)])
            xs_ = Rot([S.sb(e3, f"x3_{i}", [128, D], F32) for i in range(2)])
            outs = Rot([S.sb(e3, f"o3_{i}", [128, D], F32) for i in range(2)])
            st = S.sb(e3, "st3", [128, 4, 6], F32)
            mv3 = S.sb(e3, "mv3", [128, 2], F32)
            rstd3 = S.sb(e3, "rstd3", [128, 1], F32)
            for t in range(NT):
                y = ys.next()
                xx = xs_.next()
                o = outs.next()
                S.dma("sp", y[:], y_scr[t * 128:(t + 1) * 128, :], reads=[y_scr], writes=[y])
                S.dma("act", xx[:], x2_scr[t * 128:(t + 1) * 128, :], reads=[x2_scr], writes=[xx])
                residual_ln(S, xx, y, g1, lng, lnb, o, st, mv3, rstd3)
                tail.run(t, o)
        if not ctx:
            S.finish([x_out, h_out, idx_out, p_out])
    return nc


EXT = 3072
OWN0 = 2048
DILS = (1, 4, 16)
NEG = -30000.0
ROPE_THETA = 500000.0


def group_tiles(g):
    tiles = []
    if g == 0:
        for j in range(9):
            tiles.append(dict(u0=1920 + 128 * j, R=128, q=(j >= 1), prev=j - 1, halo=(j == 1)))
    elif g == 1:
        for rho in range(4):
            for k in range(3):
                tiles.append(dict(u0=4 * (384 + 128 * k) + rho, R=128, q=(k >= 1), prev=rho * 3 + k - 1, halo=(k == 1)))
    else:
        for rho in range(16):
            tiles.append(dict(u0=rho, R=128, q=False, prev=None, halo=False))
        for rho in range(16):
            tiles.append(dict(u0=16 * 128 + rho, R=64, q=True, prev=rho, halo=True))
    return tiles


GT_OFF = (0, 9, 21)
NGT = 53


class _Stop(Exception):
    pass


def build_attn(stop=None, ctx=None):
    nc = ctx.nc if ctx else new_nc()
    KC = D // 128
    NT = TOK // 128
    try:
      with ExitStack() as es:
        S = ctx.S if ctx else Sched(nc, es)
        DR = (lambda name, shape, dt, kind="Internal": ctx.t[name]) if ctx else S.dram
        VR = (lambda k: ctx.vr[k]) if ctx else None
        xext = DR("xext", [EXT, D], F32, kind="ExternalInput")
        vecs = DR("vecs", [8, D], F32, kind="ExternalInput")
        postab = DR("postab", [128, NGT], I32, kind="ExternalInput")
        hb = DR("hb", [3, 128], F32, kind="ExternalInput")
        w_qkv = DR("w_qkv", [D, 9216], F32, kind="ExternalInput")
        w_o = DR("w_o", [3072, D], F32, kind="ExternalInput")
        wr_d = DR("wr", [D, NEXP], F32, kind="ExternalInput")
        br_d = DR("br", [1, NEXP], F32, kind="ExternalInput")
        x_out = DR("x_out", [TOK, D], F32, kind="ExternalOutput")
        h_out = DR("h_out", [TOK, D], BF16, kind="ExternalOutput")
        idx_out = DR("idx_out", [TOK, 4], U32, kind="ExternalOutput")
        p_out = DR("p_out", [TOK, 4], F32, kind="ExternalOutput")
        o_scr = [DR(f"o_scr{g}", [TOK, 1024], F32) for g in range(3)]
        lse_scr = [DR(f"lse_scr{g}", [TOK, 16], F32) for g in range(3)]
        y_scr = DR("y_scr_a", [TOK, D], F32)

        C = ctx.C if ctx else make_consts(S, es)
        with ExitStack() as eA:
            hT = S.sb(eA, "hTx", [128, KC, EXT], BF16)
            if stop == 'consts':
                S.barrier(); raise _Stop()
            maskP = S.sb(eA, "maskP", [128, 256], F32)
            maskH = [S.sb(eA, f"maskH{g}", [128, 256], F32) for g in range(3)]
            S.op("dve", lambda e: e.tensor_scalar(out=maskP[:, 0:128], in0=C["io_f"][:], scalar1=C["io_p"][:, 0:1], scalar2=None, op0=ALU.is_ge),
                 reads=[C["io_f"], C["io_p"]], writes=[maskP])
            S.op("dve", lambda e: e.tensor_scalar(out=maskP[:, 128:256], in0=C["io_f"][:], scalar1=C["io_p"][:, 0:1], scalar2=None, op0=ALU.is_le),
                 reads=[C["io_f"], C["io_p"]], writes=[maskP])
            S.op("dve", lambda e: e.tensor_scalar(out=maskP[:], in0=maskP[:], scalar1=-1.0, scalar2=-NEG, op0=ALU.add, op1=ALU.mult), reads=[maskP], writes=[maskP])
            for g in range(3):
                S.dma("sp", maskH[g][:, 0:128], hb[g:g + 1, :].partition_broadcast(128), writes=[maskH[g]])
                S.op("dve", lambda e, g=g: e.tensor_tensor(out=maskH[g][:, 0:128], in0=maskH[g][:, 0:128], in1=maskP[:, 0:128], op=ALU.add), reads=[maskH[g], maskP], writes=[maskH[g]])
                S.op("dve", lambda e, g=g: e.tensor_copy(out=maskH[g][:, 128:256], in_=maskP[:, 128:256]), reads=[maskP], writes=[maskH[g]], acc=True)
            if stop == 'masks':
                S.barrier(); raise _Stop()
            cos_t = S.sb(eA, "cos_t", [128, NGT, 8], F32)
            sin_t = S.sb(eA, "sin_t", [128, NGT, 8], F32)
            with ExitStack() as e0:
                pos_i = S.sb(e0, "pos_i", [128, NGT], I32)
                pos_f = S.sb(e0, "pos_f", [128, NGT], F32)
                ang = S.sb(e0, "ang", [128, NGT, 8], F32)
                kf = S.sb(e0, "kf", [128, NGT, 8], F32)
                ki = S.sb(e0, "ki", [128, NGT, 8], I32)
                rr = S.sb(e0, "rr", [128, NGT, 8], F32)
                S.dma("sp", pos_i[:], postab[:, :], writes=[pos_i])
                S.op("dve", lambda e: e.tensor_copy(out=pos_f[:], in_=pos_i[:]), reads=[pos_i], writes=[pos_f])
                for f in range(8):
                    inv = float(np.float32(ROPE_THETA) ** np.float32(-f / 8.0))
                    S.op("dve", lambda e, f=f, inv=inv: e.tensor_scalar(out=ang[:, :, f], in0=pos_f[:], scalar1=inv, scalar2=None, op0=ALU.mult), reads=[pos_f], writes=[ang], acc=(f > 0))
                if stop == 'ang':
                    S.barrier(); raise _Stop()
                TWO_PI = 2.0 * float(np.pi)
                for which, dst in ((0, sin_t), (1, cos_t)):
                    if which == 1:
                        S.op("dve", lambda e: e.tensor_scalar(out=ang[:], in0=ang[:], scalar1=float(np.pi) / 2, scalar2=None, op0=ALU.add), reads=[ang], writes=[ang])
                    S.op("dve", lambda e: e.tensor_scalar(out=kf[:], in0=ang[:], scalar1=1.0 / TWO_PI, scalar2=None, op0=ALU.mult), reads=[ang], writes=[kf])
                    S.op("dve", lambda e: e.tensor_copy(out=ki[:], in_=kf[:]), reads=[kf], writes=[ki])
                    S.op("dve", lambda e: e.tensor_copy(out=kf[:], in_=ki[:]), reads=[ki], writes=[kf])
                    S.op("dve", lambda e: e.scalar_tensor_tensor(out=rr[:], in0=kf[:], scalar=-TWO_PI, in1=ang[:], op0=ALU.mult, op1=ALU.add), reads=[kf, ang], writes=[rr])
                    S.op("dve", lambda e: e.tensor_scalar(out=rr[:], in0=rr[:], scalar1=-3.14159, scalar2=3.14159, op0=ALU.max, op1=ALU.min), reads=[rr], writes=[rr])
                    S.op("act", lambda e, dst=dst: e.activation(out=dst[:], in_=rr[:], func=AF.Sin), reads=[rr], writes=[dst])
                if stop == 'tables':
                    S.barrier(); raise _Stop()
                sc1 = S.sb(e0, "sc1a", [128, D], F32)
                sh = S.sb(e0, "sha", [128, D], F32)
                load_bc(S, "sp", sc1, vecs, (VR(1) if ctx else vecs[1:2, :]))
                plus_one(S, sc1)
                load_bc(S, "sp", sh, vecs, (VR(0) if ctx else vecs[0:1, :]))
                xts = Rot([S.sb(e0, f"xta{i}", [128, D], F32) for i in range(3)])
                h16s = Rot([S.sb(e0, f"h16a{i}", [128, D], BF16) for i in range(2)])
                pst0 = Rot([S.ps(e0, f"pst0_{i}", [128, 4, 128], BF16) for i in range(2)])
                for t in range(EXT // 128):
                    xt = xts.next()
                    S.dma("sp", xt[:], xext[t * 128:(t + 1) * 128, :], reads=[xext], writes=[xt])
                    h16 = h16s.next()
                    S.op("dve", lambda e: e.tensor_tensor(out=xt[:], in0=xt[:], in1=sc1[:], op=ALU.mult), reads=[xt, sc1], writes=[xt])
                    S.op("dve", lambda e: e.tensor_tensor(out=h16[:], in0=xt[:], in1=sh[:], op=ALU.add), reads=[xt, sh], writes=[h16])
                    transpose_chunks(S, C, h16, lambda c, h16=h16: h16[:, c * 128:(c + 1) * 128], KC, hT,
                                     lambda c0, n, t=t: hT[:, c0:c0 + n, t * 128:(t + 1) * 128], pst0)
                S.barrier()
            if stop == 'hT':
                raise _Stop()
            KQ = S.sb(eA, "KQ", [128, 32, 2, 128], BF16)
            V = S.sb(eA, "V", [128, 32, 128], BF16)
            KT = S.sb(eA, "KT", [64, 2, 32, 128], BF16)
            QT = S.sb(eA, "QT", [64, 2, 16, 128], BF16)
            lse_sb = S.sb(eA, "lse_sb", [128, 16, 16], F32)
            wq = Rot([S.sb(eA, f"wq{i}", [128, KC, 128], BF16) for i in range(2)])
            wk = Rot([S.sb(eA, f"wk{i}", [128, KC, 128], BF16) for i in range(2)])
            wv = Rot([S.sb(eA, f"wv{i}", [128, KC, 128], BF16) for i in range(2)])
            pq = Rot([S.ps(eA, f"pq{i}", [128, 4, 128], F32) for i in range(3)])
            pst = Rot([S.ps(eA, "pstA", [128, 8, 128], BF16)])
            ps_s = Rot([S.ps(eA, f"ps_s{i}", [128, 2, 256], F32) for i in range(2)])
            ptp = Rot([S.ps(eA, "ptp", [128, 8, 128], BF16)])
            ps_o = Rot([S.ps(eA, "ps_o", [128, 8, 64], F32)])
            tmp = [Rot([S.sb(eA, f"rt{k}_{i}", [128, 4, 8], F32) for i in range(3)]) for k in range(4)]
            sm = Rot([S.sb(eA, f"sm{i}", [128, 2, 256], F32) for i in range(3)])
            Pb = Rot([S.sb(eA, f"Pb{i}", [128, 2, 256], BF16) for i in range(3)])
            PT = Rot([S.sb(eA, f"PT{i}", [128, 4, 128], BF16) for i in range(3)])
            negmx = Rot([S.sb(eA, f"negmx{i}", [128, 2], F32) for i in range(3)])
            rs = Rot([S.sb(eA, f"rs{i}", [128, 2], F32) for i in range(3)])
            lnrs = Rot([S.sb(eA, f"lnrs{i}", [128, 2], F32) for i in range(3)])
            o_sb = Rot([S.sb(eA, f"o_sb{i}", [128, 2, 64], F32) for i in range(3)])
            for g in range(3):
                d = DILS[g]
                tiles = group_tiles(g)
                ntile = len(tiles)
                qtiles = [i for i, tl in enumerate(tiles) if tl["q"]]
                nq = len(qtiles)
                Rq = tiles[qtiles[0]]["R"]
                for hp in range(8):
                    c0 = g * 1024 + hp * 128
                    w_q, w_k, w_v = wq.next(), wk.next(), wv.next()
                    S.dma("pool", w_q[:], w_qkv[:, c0:c0 + 128].rearrange("(kc p) n -> p kc n", p=128), reads=[w_qkv], writes=[w_q])
                    S.dma("pool", w_k[:], w_qkv[:, 3072 + c0:3072 + c0 + 128].rearrange("(kc p) n -> p kc n", p=128), reads=[w_qkv], writes=[w_k])
                    S.dma("pool", w_v[:], w_qkv[:, 6144 + c0:6144 + c0 + 128].rearrange("(kc p) n -> p kc n", p=128), reads=[w_qkv], writes=[w_v])
                    if stop == 'wload':
                        S.barrier(); raise _Stop()
                    for ti, tl in enumerate(tiles):
                        R, u0 = tl["R"], tl["u0"]
                        ps = pq.next()
                        stop_ = u0 + d * (R - 1) + 1
                        mats = [(0, w_k), (2, w_v)] + ([(1, w_q)] if tl["q"] else [])
                        first = True
                        for slot, ww in mats:
                            for kc in range(KC):
                                S.op("pe", lambda e, kc=kc, slot=slot, ww=ww: e.matmul(ps[0:R, slot, :], hT[:, kc, u0:stop_:d], ww[:, kc, :], start=(kc == 0), stop=(kc == KC - 1)),
                                     reads=[hT, ww], writes=[ps], acc=not first)
                                first = False
                        if stop == 'mm0' and ti == 0:
                            S.barrier(); raise _Stop()
                        if stop == 'proj_mm' and ti == 1:
                            S.barrier(); raise _Stop()
                        nx = 2 if tl["q"] else 1
                        src = ps[0:R, 0:nx, :].rearrange("p x (h d) -> p (x h) d", d=64)
                        dst = KQ[0:R, ti, 0:nx, :].rearrange("p x (h d) -> p (x h) d", d=64)
                        gti = GT_OFF[g] + ti
                        cb = cos_t[0:R, gti, :].unsqueeze(1).to_broadcast([R, 2 * nx, 8])
                        sb_ = sin_t[0:R, gti, :].unsqueeze(1).to_broadcast([R, 2 * nx, 8])
                        t1, t2, t3, t4 = (tmp[k].next() for k in range(4))
                        n2 = 2 * nx
                        S.op("dve", lambda e: e.tensor_tensor(out=t1[0:R, 0:n2, :], in0=src[:, :, 0:8], in1=cb, op=ALU.mult), reads=[ps, cos_t], writes=[t1])
                        if stop == 'rot0a' and ti == 0:
                            S.barrier(); raise _Stop()
                        S.op("dve", lambda e: e.tensor_tensor(out=t2[0:R, 0:n2, :], in0=src[:, :, 8:16], in1=sb_, op=ALU.mult), reads=[ps, sin_t], writes=[t2])
                        S.op("dve", lambda e: e.tensor_tensor(out=t3[0:R, 0:n2, :], in0=src[:, :, 8:16], in1=cb, op=ALU.mult), reads=[ps, cos_t], writes=[t3])
                        S.op("dve", lambda e: e.tensor_tensor(out=t4[0:R, 0:n2, :], in0=src[:, :, 0:8], in1=sb_, op=ALU.mult), reads=[ps, sin_t], writes=[t4])
                        S.op("dve", lambda e: e.tensor_tensor(out=dst[:, :, 0:8], in0=t1[0:R, 0:n2, :], in1=t2[0:R, 0:n2, :], op=ALU.subtract), reads=[t1, t2], writes=[KQ], acc=True)
                        S.op("dve", lambda e: e.tensor_tensor(out=dst[:, :, 8:16], in0=t3[0:R, 0:n2, :], in1=t4[0:R, 0:n2, :], op=ALU.add), reads=[t3, t4], writes=[KQ], acc=True)
                        if stop == 'rot0' and ti == 0:
                            S.barrier(); raise _Stop()
                        if stop == 'proj_rot' and ti == 1:
                            S.barrier(); raise _Stop()
                        S.op("dve", lambda e: e.tensor_copy(out=dst[:, :, 16:64], in_=src[:, :, 16:64]), reads=[ps], writes=[KQ], acc=True)
                        if stop == 'cp0' and ti == 0:
                            S.barrier(); raise _Stop()
                        S.op("dve", lambda e: e.tensor_copy(out=V[0:R, ti, :], in_=ps[0:R, 2, :]), reads=[ps], writes=[V], acc=True)
                        if stop == 'cp1' and ti == 0:
                            S.barrier(); raise _Stop()
                    if stop == 'proj':
                        S.barrier(); raise _Stop()
                    for hh in range(2):
                        i0 = 0
                        while i0 < ntile:
                            R = tiles[i0]["R"]
                            i1 = i0
                            while i1 < ntile and tiles[i1]["R"] == R:
                                i1 += 1
                            transpose_chunks(S, C, KQ, lambda c, i0=i0, R=R, hh=hh: KQ[0:R, i0 + c, 0, hh * 64:(hh + 1) * 64], i1 - i0, KT,
                                             lambda c0_, n, i0=i0, R=R, hh=hh: KT[:, hh, i0 + c0_:i0 + c0_ + n, 0:R], pst, R=R, NP=64)
                            i0 = i1
                        transpose_chunks(S, C, KQ, lambda s_, hh=hh: KQ[0:Rq, qtiles[s_], 1, hh * 64:(hh + 1) * 64], nq, QT,
                                         lambda c0_, n, hh=hh: QT[:, hh, c0_:c0_ + n, 0:Rq], pst, R=Rq, NP=64)
                    if stop == 'tr':
                        S.barrier(); raise _Stop()
                    for s, ti in enumerate(qtiles):
                        tl = tiles[ti]
                        pi = tl["prev"]
                        NK = 128 + Rq
                        mask = maskH[g] if tl["halo"] else maskP
                        pss = ps_s.next()
                        for hh in range(2):
                            pb = 64 * hh
                            if stop == 's_mm1' and s == 0 and hp == 0 and g == 0 and hh == 1:
                                S.barrier(); raise _Stop()
                            S.op("pe", lambda e, hh=hh, pb=pb: e.matmul(pss[0:Rq, hh, 0:128], QT[:, hh, s, 0:Rq], KT[:, hh, pi, 0:128], start=True, stop=True),
                                 reads=[QT, KT], writes=[pss], acc=(hh > 0))
                            if stop == 's_mm0' and s == 0 and hp == 0 and g == 0:
                                S.barrier(); raise _Stop()
                            S.op("pe", lambda e, hh=hh, pb=pb: e.matmul(pss[0:Rq, hh, 128:NK], QT[:, hh, s, 0:Rq], KT[:, hh, ti, 0:Rq], start=True, stop=True),
                                 reads=[QT, KT], writes=[pss], acc=True)
                        if stop == 's_mm' and s == 0 and hp == 0 and g == 0:
                            S.barrier(); raise _Stop()
                        smt = sm.next()
                        S.op("dve", lambda e: e.scalar_tensor_tensor(out=smt[0:Rq, :, 0:NK], in0=pss[0:Rq, :, 0:NK], scalar=0.125,
                                                                      in1=mask[0:Rq, 0:NK].unsqueeze(1).to_broadcast([Rq, 2, NK]), op0=ALU.mult, op1=ALU.add),
                             reads=[pss, mask], writes=[smt])
                        if stop == 's_sm' and s == 0 and hp == 0 and g == 0:
                            S.barrier(); raise _Stop()
                        nm = negmx.next()
                        S.op("dve", lambda e: e.tensor_reduce(out=nm[0:Rq, :], in_=smt[0:Rq, :, 0:NK], axis=AX.X, op=ALU.max, negate=True), reads=[smt], writes=[nm])
                        if stop == 's_max' and s == 0 and hp == 0 and g == 0:
                            S.barrier(); raise _Stop()
                        pbt = Pb.next()
                        rst = rs.next()
                        for hh in range(2):
                            S.op("act", lambda e, hh=hh: e.activation(out=pbt[0:Rq, hh, 0:NK], in_=smt[0:Rq, hh, 0:NK], func=AF.Exp, bias=nm[0:Rq, hh:hh + 1], scale=1.0, accum_out=rst[0:Rq, hh:hh + 1]),
                                 reads=[smt, nm], writes=[pbt, rst], acc=(hh > 0))
                        if stop == 's_exp' and s == 0 and hp == 0 and g == 0:
                            S.barrier(); raise _Stop()
                        ptt = ptp.next()
                        first = True
                        for hh in range(2):
                            S.op("pe", lambda e, hh=hh: e.transpose(ptt[0:128, 2 * hh, 0:Rq], pbt[0:Rq, hh, 0:128], C["ident16"][0:Rq, 0:Rq]), reads=[pbt, C["ident16"]], writes=[ptt], acc=not first)
                            first = False
                            S.op("pe", lambda e, hh=hh: e.transpose(ptt[0:Rq, 2 * hh + 1, 0:Rq], pbt[0:Rq, hh, 128:NK], C["ident16"][0:Rq, 0:Rq]), reads=[pbt, C["ident16"]], writes=[ptt], acc=True)
                        if stop == 's_pt' and s == 0 and hp == 0 and g == 0:
                            S.barrier(); raise _Stop()
                        PTt = PT.next()
                        S.op("act", lambda e: e.activation(out=PTt[:, :, 0:Rq], in_=ptt[:, 0:4, 0:Rq], func=AF.Copy), reads=[ptt], writes=[PTt])
                        if stop == 's_ptc' and s == 0 and hp == 0 and g == 0:
                            S.barrier(); raise _Stop()
                        pso = ps_o.next()
                        for hh in range(2):
                            S.op("pe", lambda e, hh=hh: e.matmul(pso[0:Rq, hh, :], PTt[0:128, 2 * hh, 0:Rq], V[0:128, pi, hh * 64:(hh + 1) * 64], start=True, stop=False),
                                 reads=[PTt, V], writes=[pso], acc=(hh > 0))
                            S.op("pe", lambda e, hh=hh: e.matmul(pso[0:Rq, hh, :], PTt[0:Rq, 2 * hh + 1, 0:Rq], V[0:Rq, ti, hh * 64:(hh + 1) * 64], start=False, stop=True),
                                 reads=[PTt, V], writes=[pso], acc=True)
                        if stop == 's_o' and s == 0 and hp == 0 and g == 0:
                            S.barrier(); raise _Stop()
                        lr = lnrs.next()
                        S.op("act", lambda e: e.activation(out=lr[0:Rq, :], in_=rst[0:Rq, :], func=AF.Ln), reads=[rst], writes=[lr])
                        S.op("dve", lambda e: e.tensor_tensor(out=lse_sb[0:Rq, s, 2 * hp:2 * hp + 2], in0=lr[0:Rq, :], in1=nm[0:Rq, :], op=ALU.subtract), reads=[lr, nm], writes=[lse_sb], acc=True)
                        S.op("dve", lambda e: e.reciprocal(out=rst[0:Rq, :], in_=rst[0:Rq, :]), reads=[rst, lr], writes=[rst])
                        if stop == 's_ln' and s == 0 and hp == 0 and g == 0:
                            S.barrier(); raise _Stop()
                        ob = o_sb.next()
                        S.op("dve", lambda e: e.tensor_tensor(out=ob[0:Rq, :, :], in0=pso[0:Rq, 0:2, :], in1=rst[0:Rq, :].unsqueeze(2).to_broadcast([Rq, 2, 64]), op=ALU.mult), reads=[pso, rst], writes=[ob])
                        if stop == 's_ob' and s == 0 and hp == 0 and g == 0:
                            S.barrier(); raise _Stop()
                        n0 = tl["u0"] - OWN0
                        ov = o_scr[g][:, :].rearrange("(m d) c -> d m c", d=d)
                        S.dma("sp", ov[n0 % d, n0 // d:n0 // d + Rq, hp * 128:(hp + 1) * 128], ob[0:Rq, :, :].rearrange("p h d -> p (h d)"), reads=[ob], writes=[o_scr[g]], acc=True)
                if stop == 'attn0':
                    S.barrier(); raise _Stop()
                for s, ti in enumerate(qtiles):
                    n0 = tiles[ti]["u0"] - OWN0
                    lv = lse_scr[g][:, :].rearrange("(m d) c -> d m c", d=d)
                    S.dma("sp", lv[n0 % d, n0 // d:n0 // d + Rq, :], lse_sb[0:Rq, s, :], reads=[lse_sb], writes=[lse_scr[g]], acc=True)
            S.barrier()
        if stop == 'attn':
            raise _Stop()
        with ExitStack() as eB:
            mixedT = S.sb(eB, "mixedTa", [128, 24, TOK], BF16)
            with ExitStack() as eB1:
                og = [Rot([S.sb(eB1, f"og{g}_{i}", [128, 1024], F32) for i in range(2)]) for g in range(3)]
                lg = [Rot([S.sb(eB1, f"lg{g}_{i}", [128, 16], F32) for i in range(2)]) for g in range(3)]
                mx = S.sb(eB1, "mx_m", [128, 16], F32)
                ssum = S.sb(eB1, "ssum_m", [128, 16], F32)
                mixed = Rot([S.sb(eB1, f"mixed{i}", [128, 3072], BF16) for i in range(2)])
                pstB = Rot([S.ps(eB1, f"pstB{i}", [128, 4, 128], BF16) for i in range(2)])
                for t in range(NT):
                    ogt = [og[g].next() for g in range(3)]
                    lgt = [lg[g].next() for g in range(3)]
                    for g in range(3):
                        S.dma("sp", ogt[g][:], o_scr[g][t * 128:(t + 1) * 128, :], reads=[o_scr[g]], writes=[ogt[g]])
                        S.dma("sp", lgt[g][:], lse_scr[g][t * 128:(t + 1) * 128, :], reads=[lse_scr[g]], writes=[lgt[g]])
                    S.op("dve", lambda e: e.tensor_tensor(out=mx[:], in0=lgt[0][:], in1=lgt[1][:], op=ALU.max), reads=[lgt[0], lgt[1]], writes=[mx])
                    S.op("dve", lambda e: e.tensor_tensor(out=mx[:], in0=mx[:], in1=lgt[2][:], op=ALU.max), reads=[mx, lgt[2]], writes=[mx])
                    for g in range(3):
                        S.op("dve", lambda e, g=g: e.tensor_tensor(out=lgt[g][:], in0=lgt[g][:], in1=mx[:], op=ALU.subtract), reads=[lgt[g], mx], writes=[lgt[g]])
                        S.op("act", lambda e, g=g: e.activation(out=lgt[g][:], in_=lgt[g][:], func=AF.Exp), reads=[lgt[g]], writes=[lgt[g]])
                    S.op("dve", lambda e: e.tensor_tensor(out=ssum[:], in0=lgt[0][:], in1=lgt[1][:], op=ALU.add), reads=[lgt[0], lgt[1]], writes=[ssum])
                    S.op("dve", lambda e: e.tensor_tensor(out=ssum[:], in0=ssum[:], in1=lgt[2][:], op=ALU.add), reads=[ssum, lgt[2]], writes=[ssum])
                    S.op("dve", lambda e: e.reciprocal(out=ssum[:], in_=ssum[:]), reads=[ssum], writes=[ssum])
                    mt = mixed.next()
                    for g in range(3):
                        S.op("dve", lambda e, g=g: e.tensor_tensor(out=lgt[g][:], in0=lgt[g][:], in1=ssum[:], op=ALU.mult), reads=[lgt[g], ssum], writes=[lgt[g]])
                        S.op("dve", lambda e, g=g: e.tensor_tensor(out=mt[:, g * 1024:(g + 1) * 1024].rearrange("p (s d) -> p s d", d=64),
                                                                    in0=ogt[g][:].rearrange("p (s d) -> p s d", d=64),
                                                                    in1=lgt[g][:, :].unsqueeze(2).to_broadcast([128, 16, 64]), op=ALU.mult),
                             reads=[ogt[g], lgt[g]], writes=[mt], acc=(g > 0))
                    transpose_chunks(S, C, mt, lambda c, mt=mt: mt[:, c * 128:(c + 1) * 128], 24, mixedT,
                                     lambda c0_, n, t=t: mixedT[:, c0_:c0_ + n, t * 128:(t + 1) * 128], pstB)
                S.barrier()
            wo = Rot([S.sb(eB, f"wo{i}", [128, 24, 512], BF16) for i in range(2)])
            psy = Rot([S.ps(eB, f"psyA{i}", [128, 512], F32) for i in range(2)])
            ysb = Rot([S.sb(eB, f"ysbA{i}", [128, 512], F32) for i in range(3)])
            for nch in range(D // 512):
                w = wo.next()
                S.dma("pool", w[:], w_o[:, nch * 512:(nch + 1) * 512].rearrange("(kc p) n -> p kc n", p=128), reads=[w_o], writes=[w])
                for t in range(NT):
                    ps = psy.next()
                    for kc in range(24):
                        S.op("pe", lambda e, kc=kc: e.matmul(ps[:], mixedT[:, kc, t * 128:(t + 1) * 128], w[:, kc, :], start=(kc == 0), stop=(kc == 23)),
                             reads=[mixedT, w], writes=[ps], acc=(kc > 0))
                    yb = ysb.next()
                    S.op("act", lambda e: e.activation(out=yb[:], in_=ps[:], func=AF.Copy), reads=[ps], writes=[yb])
                    S.dma("sp", y_scr[t * 128:(t + 1) * 128, nch * 512:(nch + 1) * 512], yb[:], reads=[yb], writes=[y_scr], acc=True)
            S.barrier()
        if stop == 'wo':
            raise _Stop()
        with ExitStack() as e3:
            g1 = S.sb(e3, "g1a", [128, D], F32)
            lng = S.sb(e3, "lnga", [128, D], F32)
            lnb = S.sb(e3, "lnba", [128, D], F32)
            load_bc(S, "sp", g1, vecs, (VR(2) if ctx else vecs[2:3, :]))
            plus_one(S, g1)
            load_bc(S, "sp", lng, vecs, (VR(3) if ctx else vecs[3:4, :]))
            load_bc(S, "sp", lnb, vecs, (VR(4) if ctx else vecs[4:5, :]))
            tail = Tail(S, C, e3, (VR(6) if ctx else vecs[6:7, :]), (VR(5) if ctx else vecs[5:6, :]), vecs, wr_d, br_d, x_out, h_out, idx_out, p_out, scat=(ctx.scat if ctx else None))
            ys = Rot([S.sb(e3, f"ya_{i}", [128, D], F32) for i in range(2)])
            xs_ = Rot([S.sb(e3, f"xa_{i}", [128, D], F32) for i in range(2)])
            outs = Rot([S.sb(e3, f"oa_{i}", [128, D], F32) for i in range(2)])
            st = S.sb(e3, "sta", [128, 4, 6], F32)
            mv3 = S.sb(e3, "mva", [128, 2], F32)
            rstd3 = S.sb(e3, "rstda", [128, 1], F32)
            for t in range(NT):
                y = ys.next()
                xx = xs_.next()
                o = outs.next()
                S.dma("sp", y[:], y_scr[t * 128:(t + 1) * 128, :], reads=[y_scr], writes=[y])
                S.dma("sp", xx[:], xext[OWN0 + t * 128:OWN0 + (t + 1) * 128, :], reads=[xext], writes=[xx])
                residual_ln(S, xx, y, g1, lng, lnb, o, st, mv3, rstd3)
                tail.run(t, o)
        if not ctx:
            S.finish([x_out, h_out, idx_out, p_out])
    except _Stop:
        pass
    return nc


def attn_core_inputs(x_b, pos_b, T0):
    xext = np.zeros((EXT, D), np.float32)
    pext = np.zeros((EXT,), np.int32)
    lo = T0 - OWN0
    s = max(lo, 0)
    xext[s - lo:] = x_b[s:T0 + TOK]
    pext[s - lo:] = pos_b[s:T0 + TOK]
    postab = np.zeros((128, NGT), np.int32)
    hbv = np.zeros((3, 128), np.float32)
    for g in range(3):
        d = DILS[g]
        for ti, tl in enumerate(group_tiles(g)):
            u = tl["u0"] + d * np.arange(tl["R"])
            postab[:tl["R"], GT_OFF[g] + ti] = pext[u]
        first_q = next(tl for tl in group_tiles(g) if tl["halo"])
        prev = group_tiles(g)[first_q["prev"]]
        u = prev["u0"] + d * np.arange(128)
        hbv[g] = np.where(u + lo >= 0, 0.0, NEG)
    return {"xext": xext, "postab": postab, "hb": hbv}


_NC_CACHE = {}
_DBG = None


def _get(name, fn):
    if name not in _NC_CACHE:
        _NC_CACHE[name] = fn()
    return _NC_CACHE[name]


def _run(nc, in_maps):
    res = run_bass_kernel_spmd(nc, in_maps, core_ids=list(range(NCORES)))
    return res.results


def _pk(v):
    return np.ascontiguousarray(np.asarray(v).reshape(-1, 128).T)


def moe_w_layout(w, nchunk):
    nl = w.shape[0]
    r = w.reshape(nl, 16, 128, nchunk, 256).transpose(0, 3, 2, 1, 4)
    return np.ascontiguousarray(r).reshape(nl * nchunk * 128, 16 * 256)


def _moe_launch(h_list, idx_list, p_list, w_in, b_in, w_out, b_out):
    nc = _get("moe", build_moe)
    h_all = np.concatenate(h_list, 0)
    idx_all = np.concatenate(idx_list, 0).view(np.int32)
    p_all = np.concatenate(p_list, 0)
    in_maps = []
    for i in range(NCORES):
        e0 = i * NLOC
        in_maps.append({
            "h_all": h_all, "idx_all": idx_all, "p_all": p_all,
            "eids": np.arange(e0, e0 + NLOC, dtype=np.float32).reshape(1, NLOC),
            "w_in": moe_w_layout(w_in[e0:e0 + NLOC], 16),
            "b_in": np.ascontiguousarray(b_in[e0:e0 + NLOC].reshape(NLOC, 2 * FF // 128, 128).transpose(0, 2, 1).reshape(NLOC * 128, 2 * FF // 128)),
            "w_out": moe_w_layout(w_out[e0:e0 + NLOC], 8),
            "b_out": np.ascontiguousarray(b_out[e0:e0 + NLOC]),
        })
    res = _run(nc, in_maps)
    partials = [r["partial"] for r in res]
    return [np.stack([partials[c][i * TOK:(i + 1) * TOK] for c in range(NCORES)], 0) for i in range(NCORES)]


def kernel_unfused(x, c, positions, cond_w, cond_b, ln_g, ln_b, attn_w_qkv, attn_w_o,
           sg_w_in, sg_b_in, sg_ln_g, sg_ln_b, sg_w_spatial, sg_b_spatial, sg_w_out,
           router_w, router_b, expert_w_in, expert_b_in, expert_w_out, expert_b_out):
    f32 = lambda a: np.asarray(a, dtype=np.float32)
    x, c, cond_w, cond_b, ln_g, ln_b = f32(x), f32(c), f32(cond_w), f32(cond_b), f32(ln_g), f32(ln_b)
    positions = np.asarray(positions).astype(np.int32)
    attn_w_qkv, attn_w_o = f32(attn_w_qkv), f32(attn_w_o)
    sg_w_in, sg_b_in, sg_ln_g, sg_ln_b = f32(sg_w_in), f32(sg_b_in), f32(sg_ln_g), f32(sg_ln_b)
    sg_w_spatial, sg_b_spatial, sg_w_out = f32(sg_w_spatial), f32(sg_b_spatial), f32(sg_w_out)
    router_w, router_b = f32(router_w), f32(router_b)
    expert_w_in, expert_b_in, expert_w_out, expert_b_out = f32(expert_w_in), f32(expert_b_in), f32(expert_w_out), f32(expert_b_out)
    B = x.shape[0]

    cT = np.ascontiguousarray(c.reshape(B, D // 128, 128).transpose(2, 1, 0))
    in_maps = [{"cT": cT, "cw": np.ascontiguousarray(cond_w[:, :, i * MODW:(i + 1) * MODW]),
                "cb": np.ascontiguousarray(cond_b[:, i * MODW:(i + 1) * MODW])} for i in range(NCORES)]
    res = _run(_get("cond", build_cond), in_maps)
    mod = np.concatenate([r["mod"] for r in res], axis=2).reshape(DEPTH, B, 6, D)
    zero = np.zeros((D,), np.float32)

    in_maps = []
    for i in range(NCORES):
        b, T0 = i // 4, (i % 4) * TOK
        m = attn_core_inputs(x[b], positions[b], T0)
        md = mod[0, b]
        m["vecs"] = np.stack([md[0], md[1], md[2], ln_g[0, 0], ln_b[0, 0], md[3], md[4], zero], 0)
        m.update({"w_qkv": attn_w_qkv[0], "w_o": attn_w_o[0], "wr": router_w[0], "br": router_b[0][None, :]})
        in_maps.append(m)
    res = _run(_get("attn", build_attn), in_maps)
    x1 = [r["x_out"] for r in res]
    if _DBG is not None:
        _DBG.update(mod=mod, x1=x1, h2=[r["h_out"] for r in res], idx2=[r["idx_out"] for r in res], p2=[r["p_out"] for r in res])
        if _DBG.get("stop") == 1:
            return None
    parts = _moe_launch([r["h_out"] for r in res], [r["idx_out"] for r in res], [r["p_out"] for r in res],
                        expert_w_in[0], expert_b_in[0], expert_w_out[0], expert_b_out[0])

    if _DBG is not None:
        _DBG.update(parts0=parts)
        if _DBG.get("stop") == 2:
            return None
    in_maps = []
    for i in range(NCORES):
        b = i // 4
        m0, m1 = mod[0, b], mod[1, b]
        vecs = np.stack([m0[5], ln_g[0, 1], ln_b[0, 1], m1[0], m1[1], m1[2], ln_g[1, 0], ln_b[1, 0], m1[3], m1[4]], 0)
        in_maps.append({
            "parts": parts[i], "x_in": x1[i], "vecs": vecs, "sg_w_in": sg_w_in[0],
            "bu_pk": _pk(sg_b_in[0, :SGW]), "bv": np.ascontiguousarray(sg_b_in[0, SGW:][None, :]),
            "lng_pk": _pk(sg_ln_g[0]), "lnb_pk": _pk(sg_ln_b[0]),
            "w_sp": sg_w_spatial[0], "b_sp": np.ascontiguousarray(sg_b_spatial[0].reshape(1, -1)),
            "sg_w_out": sg_w_out[0], "wr": router_w[1], "br": router_b[1][None, :],
        })
    res = _run(_get("mid", build_mid), in_maps)
    x3 = [r["x_out"] for r in res]
    if _DBG is not None:
        _DBG.update(x3=x3, h4=[r["h_out"] for r in res], idx4=[r["idx_out"] for r in res], p4=[r["p_out"] for r in res])
        if _DBG.get("stop") == 3:
            return None
    parts = _moe_launch([r["h_out"] for r in res], [r["idx_out"] for r in res], [r["p_out"] for r in res],
                        expert_w_in[1], expert_b_in[1], expert_w_out[1], expert_b_out[1])

    in_maps = []
    for i in range(NCORES):
        b = i // 4
        in_maps.append({"parts": parts[i], "x_in": x3[i], "vecs": np.stack([mod[1, b][5], ln_g[1, 1], ln_b[1, 1]], 0)})
    res = _run(_get("final", build_final), in_maps)
    out = np.concatenate([r["out"] for r in res], 0).reshape(B, SEQ, D)
    return out.astype(np.float32)


NF = 2
NPAIR = 2
NCF = NF * NPAIR
PTOK = NTOK // NPAIR
CTOK = PTOK // NF
NCHK = CTOK // TOK
NLOCF = NEXP // NF
NBLKF = 28 if NPAIR == 4 else (40 if NPAIR == 2 else 64)
MODF = 6 * D


class Ctx:
    pass


def xbarrier(S, nc, tok):
    S.barrier()
    nc.all_core_barrier()
    tp, ts, dsrc = tok
    S.op("pool", lambda e: e.memset(tp[:], 1.0), writes=[tp])
    S.dma("sp", ts[:], dsrc, writes=[ts])
    for e in ("pe", "dve", "act", "pool", "sp"):
        S._deps(e, [tp, ts], [], False)


def emit_cond_f(S, C, cT, cw, cb, modv):
    KC = D // 128
    with ExitStack() as es:
        ct = S.sb(es, "ct", [128, KC, 1], F32)
        S.dma("sp", ct[:], cT[:, :, :], writes=[ct])
        S.op("act", lambda e: e.activation(out=ct[:], in_=ct[:], func=AF.Silu), reads=[ct], writes=[ct])
        wbs = Rot([S.sb(es, f"cwb{i}", [128, KC, 512], F32) for i in range(3)])
        brs = Rot([S.sb(es, f"cbr{i}", [1, 512], F32) for i in range(2)])
        pss = Rot([S.ps(es, f"cps{i}", [1, 512], F32) for i in range(2)])
        obs = Rot([S.sb(es, f"cob{i}", [1, 512], F32) for i in range(2)])
        for l in range(DEPTH):
            for nch in range(MODF // 512):
                wbuf = wbs.next()
                S.dma("sp", wbuf[:], cw[l, :, nch * 512:(nch + 1) * 512].rearrange("(kc p) n -> p kc n", p=128), reads=[cw], writes=[wbuf])
                br = brs.next()
                S.dma("sp", br[:], cb[l:l + 1, nch * 512:(nch + 1) * 512], reads=[cb], writes=[br])
                ps = pss.next()
                for kc in range(KC):
                    S.op("pe", lambda e, kc=kc: e.matmul(ps[:], ct[:, kc, :], wbuf[:, kc, :], start=(kc == 0), stop=False), reads=[ct, wbuf], writes=[ps], acc=(kc > 0))
                S.op("pe", lambda e: e.matmul(ps[:], C["ones32"][0:1, 0:1], br[0:1, :], start=False, stop=True), reads=[C["ones32"], br], writes=[ps], acc=True)
                ob = obs.next()
                S.op("dve", lambda e: e.tensor_copy(out=ob[:], in_=ps[:]), reads=[ps], writes=[ob])
                S.dma("sp", modv[l:l + 1, nch * 512:(nch + 1) * 512], ob[:], reads=[ob], writes=[modv], acc=True)
        S.barrier()


class PartLoader:
    nparts = NF

    def __init__(self, PART, pidx, tile0, breg):
        self.PART, self.pidx, self.tile0, self.breg = PART, pidx, tile0, breg

    def __call__(self, S, c, t, dst):
        for hf in range(2):
            S.dma("pool", None, None, reads=[self.PART[hf], self.pidx], writes=[dst], acc=(hf > 0),
                  fn=lambda e, hf=hf: e.indirect_dma_start(out=dst[:, hf * (D // 2):(hf + 1) * (D // 2)], out_offset=None, in_=self.PART[hf][:, :],
                                                           in_offset=bass.IndirectOffsetOnAxis(ap=self.pidx[:, c, self.tile0 + t:self.tile0 + t + 1], axis=0),
                                                           bounds_check=self.breg, oob_is_err=False))


def build_fused():
    nc = bass.Bass("TRN2", target_bir_lowering=False, num_devices=NCF)
    with ExitStack() as es:
        S = Sched(nc, es)
        I = lambda name, shape, dt: S.dram(name, shape, dt, kind="ExternalInput")
        x_pad = I("x_pad", [OWN0 + CTOK, D], F32)
        postab = I("postab", [NCHK, 128, NGT], I32)
        hb = I("hb", [NCHK, 3, 128], F32)
        cT = I("cT", [128, D // 128, 1], F32)
        cond_w = I("cond_w", [DEPTH, D, MODF], F32)
        cond_b = I("cond_b", [DEPTH, MODF], F32)
        lnp = I("lnp", [8, D], F32)
        w_qkv = I("w_qkv", [D, 9216], F32)
        w_o = I("w_o", [3072, D], F32)
        wr = I("wr", [DEPTH, D, NEXP], F32)
        br = I("br", [DEPTH, NEXP], F32)
        sg_w_in = I("sg_w_in", [D, 2 * SGW], F32)
        bu_pk = I("bu_pk", [128, SGC], F32)
        bv = I("bv", [1, SGW], F32)
        lng_pk = I("lng_pk", [128, SGC], F32)
        lnb_pk = I("lnb_pk", [128, SGC], F32)
        w_sp = I("w_sp", [16, 128, 128], F32)
        b_sp = I("b_sp", [1, 16 * 128], F32)
        sg_w_out = I("sg_w_out", [SGW, D], F32)
        e_w_in = [I(f"e_w_in{l}", [NLOCF * 16 * 128, 4096], F32) for l in range(DEPTH)]
        e_b_in = [I(f"e_b_in{l}", [NLOCF * 128, 2 * FF // 128], F32) for l in range(DEPTH)]
        e_w_out = [I(f"e_w_out{l}", [NLOCF * 8 * 128, 4096], F32) for l in range(DEPTH)]
        e_b_out = [I(f"e_b_out{l}", [NLOCF, D], F32) for l in range(DEPTH)]
        eids = I("eids", [1, NLOCF], F32)
        rowidx_d = I("rowidx", [128, NCHK * 8], I32)
        prow_d = I("prow", [128, PTOK // 128], I32)
        pidx_d = I("pidx", [128, NF, NCHK * 8], I32)
        out = S.dram("out", [CTOK, D], F32, kind="ExternalOutput")
        modv = S.dram("modv", [DEPTH, MODF], F32)
        x1_all = S.dram("x1_all", [CTOK, D], F32)
        x3_all = S.dram("x3_all", [CTOK, D], F32)
        o_scr = [S.dram(f"o_scr{g}", [TOK, 1024], F32) for g in range(3)]
        lse_scr = [S.dram(f"lse_scr{g}", [TOK, 16], F32) for g in range(3)]
        y_scr_a = S.dram("y_scr_a", [TOK, D], F32)
        x2_scr = S.dram("x2_scr", [TOK, D], F32)
        y_scr = S.dram("y_scr", [TOK, D], F32)
        xs = S.dram("xs", [NBLKF * 512, D], BF16)
        ysd = [S.dram(f"ys{i}", [NBLKF * 512 + 128, D // 2], F32) for i in range(2)]
        SH = lambda name, shape, dt: T(nc.dram_tensor(name, shape, dt, addr_space="Shared").ap(), name)
        H = SH("H_sh", [PTOK, D], BF16)
        RT = SH("RT_sh", [PTOK, 8], F32)
        PART = [SH(f"PART{i}_sh", [NF * PTOK, D // 2], F32) for i in range(2)]

        C = make_consts(S, es)
        rowidx = S.sb(es, "rowidx", [128, NCHK * 8], I32)
        prow = S.sb(es, "prow", [128, PTOK // 128], I32)
        pidx = S.sb(es, "pidx", [128, NF, NCHK * 8], I32)
        S.dma("sp", rowidx[:], rowidx_d[:, :], writes=[rowidx])
        S.dma("sp", prow[:], prow_d[:, :], writes=[prow])
        S.dma("sp", pidx[:], pidx_d[:, :, :], writes=[pidx])
        tok = (S.sb(es, "tokp", [1, 8], F32), S.sb(es, "toks", [1, 8], F32), lnp[0:1, 0:8])
        breg_h = nc.gpsimd.to_reg(PTOK - 1)
        breg_part = nc.gpsimd.to_reg(NF * PTOK - 1)
        dummy = T(None, "dummy")

        def mk(**kw):
            c = Ctx()
            c.nc, c.S, c.C = nc, S, C
            c.t = {}
            c.sb = {"prow": prow}
            c.breg_part = breg_part
            c.scat = None
            c.part_loader = None
            c.vr = {}
            for k, v in kw.items():
                setattr(c, k, v)
            return c

        mrow = lambda l, j: modv[l:l + 1, j * D:(j + 1) * D]
        lrow = lambda k: lnp[k:k + 1, :]
        Tv = lambda ap, name: T(ap, name)

        emit_cond_f(S, C, cT, cond_w, cond_b, modv)

        for ch in range(NCHK):
            ctx = mk()
            ctx.t = {"xext": Tv(x_pad[ch * TOK:ch * TOK + EXT, :], "xext"), "vecs": dummy, "postab": Tv(postab[ch], "postab"), "hb": Tv(hb[ch], "hb"),
                     "w_qkv": w_qkv, "w_o": w_o, "wr": Tv(wr[0], "wr0"), "br": Tv(br[0:1, :], "br0"),
                     "x_out": Tv(x1_all[ch * TOK:(ch + 1) * TOK, :], "x1c"), "h_out": dummy, "idx_out": dummy, "p_out": dummy,
                     "o_scr0": o_scr[0], "o_scr1": o_scr[1], "o_scr2": o_scr[2], "lse_scr0": lse_scr[0], "lse_scr1": lse_scr[1], "lse_scr2": lse_scr[2],
                     "y_scr_a": y_scr_a}
            ctx.vr = {0: mrow(0, 0), 1: mrow(0, 1), 2: mrow(0, 2), 3: lrow(0), 4: lrow(1), 5: mrow(0, 3), 6: mrow(0, 4)}
            ctx.scat = {"H": H, "RT": RT, "rowidx": rowidx, "tile0": ch * 8, "breg": breg_h}
            build_attn(None, ctx)
            S.barrier()
        xbarrier(S, nc, tok)

        def moe(l):
            ctx = mk()
            ctx.t = {"h_all": H, "RT": RT, "idx_all": dummy, "p_all": dummy, "eids": eids, "w_in": e_w_in[l], "b_in": e_b_in[l],
                     "w_out": e_w_out[l], "b_out": e_b_out[l], "partial": dummy, "xs": xs, "ys0": ysd[0], "ys1": ysd[1],
                     "PART0": PART[0], "PART1": PART[1]}
            build_moe(PTOK, NLOCF, NBLKF, ctx)
            xbarrier(S, nc, tok)

        moe(0)

        for ch in range(NCHK):
            ctx = mk()
            ctx.t = {"parts": dummy, "x_in": Tv(x1_all[ch * TOK:(ch + 1) * TOK, :], "x1c"), "vecs": dummy, "sg_w_in": sg_w_in, "bu_pk": bu_pk, "bv": bv,
                     "lng_pk": lng_pk, "lnb_pk": lnb_pk, "w_sp": w_sp, "b_sp": b_sp, "sg_w_out": sg_w_out, "wr": Tv(wr[1], "wr1"), "br": Tv(br[1:2, :], "br1"),
                     "x_out": Tv(x3_all[ch * TOK:(ch + 1) * TOK, :], "x3c"), "h_out": dummy, "idx_out": dummy, "p_out": dummy, "x2_scr": x2_scr, "y_scr": y_scr}
            ctx.vr = {0: mrow(0, 5), 1: lrow(2), 2: lrow(3), 3: mrow(1, 0), 4: mrow(1, 1), 5: mrow(1, 2), 6: lrow(4), 7: lrow(5), 8: mrow(1, 3), 9: mrow(1, 4)}
            ctx.scat = {"H": H, "RT": RT, "rowidx": rowidx, "tile0": ch * 8, "breg": breg_h}
            ctx.part_loader = PartLoader(PART, pidx, ch * 8, breg_part)
            build_mid(ctx)
            S.barrier()
        xbarrier(S, nc, tok)

        moe(1)

        for ch in range(NCHK):
            ctx = mk()
            ctx.t = {"parts": dummy, "x_in": Tv(x3_all[ch * TOK:(ch + 1) * TOK, :], "x3c"), "vecs": dummy, "out": Tv(out[ch * TOK:(ch + 1) * TOK, :], "outc")}
            ctx.vr = {0: mrow(1, 5), 1: lrow(6), 2: lrow(7)}
            ctx.part_loader = PartLoader(PART, pidx, ch * 8, breg_part)
            build_final(ctx)
            S.barrier()
        S.barrier()
    return nc


def kernel(x, c, positions, cond_w, cond_b, ln_g, ln_b, attn_w_qkv, attn_w_o,
           sg_w_in, sg_b_in, sg_ln_g, sg_ln_b, sg_w_spatial, sg_b_spatial, sg_w_out,
           router_w, router_b, expert_w_in, expert_b_in, expert_w_out, expert_b_out):
    f32 = lambda a: np.asarray(a, dtype=np.float32)
    x, c, cond_w, cond_b, ln_g, ln_b = f32(x), f32(c), f32(cond_w), f32(cond_b), f32(ln_g), f32(ln_b)
    positions = np.asarray(positions).astype(np.int32)
    attn_w_qkv, attn_w_o = f32(attn_w_qkv), f32(attn_w_o)
    sg_w_in, sg_b_in, sg_ln_g, sg_ln_b = f32(sg_w_in), f32(sg_b_in), f32(sg_ln_g), f32(sg_ln_b)
    sg_w_spatial, sg_b_spatial, sg_w_out = f32(sg_w_spatial), f32(sg_b_spatial), f32(sg_w_out)
    router_w, router_b = f32(router_w), f32(router_b)
    expert_w_in, expert_b_in, expert_w_out, expert_b_out = f32(expert_w_in), f32(expert_b_in), f32(expert_w_out), f32(expert_b_out)
    lnp = np.ascontiguousarray(np.stack([ln_g[0, 0], ln_b[0, 0], ln_g[0, 1], ln_b[0, 1], ln_g[1, 0], ln_b[1, 0], ln_g[1, 1], ln_b[1, 1]], 0))
    p128 = np.arange(128, dtype=np.int32)[:, None]
    in_maps = []
    ppb = NPAIR // 2
    place = []
    wcache = {}
    for ci in range(NCF):
        q, i = ci // NF, ci % NF
        b = q // ppb
        Tc = (q % ppb) * PTOK + i * CTOK
        place.append((b, Tc))
        x_pad = np.zeros((OWN0 + CTOK, D), np.float32)
        lo = Tc - OWN0
        s0 = max(lo, 0)
        x_pad[s0 - lo:] = x[b, s0:Tc + CTOK]
        pt, hbv = [], []
        for ch in range(NCHK):
            m = attn_core_inputs(x[b], positions[b], Tc + ch * TOK)
            pt.append(m["postab"])
            hbv.append(m["hb"])
        e0 = i * NLOCF
        if i not in wcache:
            wcache[i] = {
                **{f"e_w_in{l}": moe_w_layout(expert_w_in[l, e0:e0 + NLOCF], 16) for l in range(DEPTH)},
                **{f"e_b_in{l}": np.ascontiguousarray(expert_b_in[l, e0:e0 + NLOCF].reshape(NLOCF, 2 * FF // 128, 128).transpose(0, 2, 1).reshape(NLOCF * 128, 2 * FF // 128)) for l in range(DEPTH)},
                **{f"e_w_out{l}": moe_w_layout(expert_w_out[l, e0:e0 + NLOCF], 8) for l in range(DEPTH)},
                **{f"e_b_out{l}": np.ascontiguousarray(expert_b_out[l, e0:e0 + NLOCF]) for l in range(DEPTH)},
            }
        in_maps.append({
            "x_pad": x_pad, "postab": np.stack(pt, 0), "hb": np.stack(hbv, 0),
            "cT": np.ascontiguousarray(c[b].reshape(D // 128, 128).T[:, :, None]),
            "cond_w": cond_w, "cond_b": cond_b, "lnp": lnp, "w_qkv": attn_w_qkv[0], "w_o": attn_w_o[0],
            "wr": router_w, "br": router_b,
            "sg_w_in": sg_w_in[0], "bu_pk": _pk(sg_b_in[0, :SGW]), "bv": np.ascontiguousarray(sg_b_in[0, SGW:][None, :]),
            "lng_pk": _pk(sg_ln_g[0]), "lnb_pk": _pk(sg_ln_b[0]), "w_sp": sg_w_spatial[0],
            "b_sp": np.ascontiguousarray(sg_b_spatial[0].reshape(1, -1)), "sg_w_out": sg_w_out[0],
            **wcache[i],
            "eids": np.arange(e0, e0 + NLOCF, dtype=np.float32).reshape(1, NLOCF),
            "rowidx": (i * CTOK + np.arange(NCHK * 8, dtype=np.int32)[None, :] * 128 + p128).astype(np.int32),
            "prow": (i * PTOK + np.arange(PTOK // 128, dtype=np.int32)[None, :] * 128 + p128).astype(np.int32),
            "pidx": np.stack([(cc * PTOK + i * CTOK + np.arange(NCHK * 8, dtype=np.int32)[None, :] * 128 + p128) for cc in range(NF)], 1).astype(np.int32),
        })
    nc = _get("fused", build_fused)
    res = run_bass_kernel_spmd(nc, in_maps, core_ids=list(range(NCF)))
    out = np.zeros((x.shape[0], SEQ, D), np.float32)
    for ci, (b, Tc) in enumerate(place):
        out[b, Tc:Tc + CTOK] = res.results[ci]["out"]
    return out
```
